# Optimizing a Trainium2 kernel written in Bass

```python
import math
import jax, jax.numpy as jnp
from jax import lax
import numpy as np

D_MODEL = 1024
BATCH = 16
SEQ = 2048
DEPTH = 4

GRID_W = 64
CTX_LEN = 256
F32 = jnp.float32

N_MOD = 6
DN_ALPHA = (2 * DEPTH) ** 0.25
DN_BETA = (8 * DEPTH) ** -0.25
NORM_EPS = 1e-6
ROPE_THETA = 10000.0
CHUNK = 64
VEC_CHUNK = 16
LB_FLOOR = 1e-30
MIX_W = D_MODEL // 2
N_EVEN = (DEPTH + 1) // 2
N_ODD = DEPTH // 2

HG_DK = 128
HG_DV = 128
HG_HEADS = MIX_W // HG_DV
HG_KW = HG_HEADS * HG_DK
HG_VW = HG_HEADS * HG_DV
AT_DH = 128
AT_HEADS = MIX_W // AT_DH
AT_KV_HEADS = AT_HEADS // 2
AT_QW = AT_HEADS * AT_DH
AT_KVW = AT_KV_HEADS * AT_DH
ROPE_PAIRS = AT_DH // 4
Q_BLOCK = 128
GLA_DV = 128
GLA_HEADS = MIX_W // GLA_DV
GLA_DK = GLA_DV // 2
GLA_KW = GLA_HEADS * GLA_DK
GLA_VW = GLA_HEADS * GLA_DV
GLA_GATE_RANK = 16
GLA_GATE_NORM = 16.0
SSD_DH = 64
SSD_HEADS = MIX_W // SSD_DH
SSD_INNER = SSD_HEADS * SSD_DH
SSD_GROUPS = 2
SSD_STATE = 128
SSD_BCW = SSD_GROUPS * SSD_STATE
SSD_CONV = 5
SSD_CONV_DIM = SSD_INNER + 2 * SSD_BCW
PEER_HEADS = 8
PEER_NKEYS = 128
PEER_EXPERTS = PEER_NKEYS * PEER_NKEYS
PEER_DQ = 256
PEER_TOPK = 16
PEER_TOKEN_BLOCK = 128

EVEN_SIZES = (HG_KW, HG_VW, HG_KW, HG_KW, HG_VW, AT_QW, AT_KVW, AT_KVW)
EVEN_IN = sum(EVEN_SIZES)
EVEN_OUT = HG_VW + AT_QW
ODD_SIZES = (GLA_KW, GLA_KW, GLA_VW, GLA_VW, GLA_GATE_RANK, GLA_GATE_RANK,
             SSD_INNER, SSD_INNER, SSD_BCW, SSD_BCW, SSD_HEADS, SSD_HEADS)
ODD_IN = sum(ODD_SIZES)
ODD_OUT = GLA_VW + SSD_INNER

kernel_name = "hybrid_hgrn2_gqa_gla_ssd_peer_diffusion"


def split_cols(y, sizes):
    return jnp.split(y, np.cumsum(sizes)[:-1].tolist(), axis=-1)


def to_heads(y, n_heads):
    b, n, w = y.shape
    return y.reshape(b, n, n_heads, w // n_heads).transpose(0, 2, 1, 3)


def from_heads(o):
    b, h, n, d = o.shape
    return o.transpose(0, 2, 1, 3).reshape(b, n, h * d)


def layer_norm(x, g, b):
    xf = x.astype(F32)
    mu = jnp.mean(xf, -1, keepdims=True)
    xc = xf - mu
    var = jnp.mean(xc * xc, -1, keepdims=True)
    return (xc * lax.rsqrt(var + NORM_EPS) * g + b).astype(x.dtype)


def rms_norm(x, g):
    xf = x.astype(F32)
    return (xf * lax.rsqrt(jnp.mean(xf * xf, -1, keepdims=True) + NORM_EPS) * g).astype(x.dtype)


def modulation(cvec, w, b):
    m = jax.nn.silu(cvec) @ w + b
    return tuple(t[:, None, :] for t in jnp.split(m, N_MOD, axis=-1))


def axial_rope_tables(rows):
    row = jnp.repeat(jnp.arange(rows), GRID_W).astype(F32)
    col = jnp.tile(jnp.arange(GRID_W), rows).astype(F32)
    inv = ROPE_THETA ** (-jnp.arange(ROPE_PAIRS, dtype=F32) / ROPE_PAIRS)
    ang = jnp.concatenate([row[:, None] * inv, col[:, None] * inv], axis=-1)
    return jnp.cos(ang), jnp.sin(ang)


def apply_rope(x, cos, sin):
    xf = x.astype(F32).reshape(x.shape[:-1] + (x.shape[-1] // 2, 2))
    x0, x1 = xf[..., 0], xf[..., 1]
    c, s = cos[:, None, :], sin[:, None, :]
    return jnp.stack([x0 * c - x1 * s, x0 * s + x1 * c], -1).reshape(x.shape).astype(x.dtype)


def blocked_gqa(q, k, v):
    b, n, h, dh = q.shape
    g = k.shape[2]
    qb = q.reshape(b, n // Q_BLOCK, Q_BLOCK, g, h // g, dh).transpose(1, 0, 2, 3, 4, 5)
    scale = dh ** -0.5

    def one_block(qblk):
        s = jnp.einsum('bqgrd,bkgd->bgrqk', qblk, k, preferred_element_type=F32) * scale
        p = jax.nn.softmax(s, axis=-1).astype(v.dtype)
        return jnp.einsum('bgrqk,bkgd->bqgrd', p, v)

    o = lax.map(one_block, qb)
    return o.transpose(1, 0, 2, 3, 4, 5).reshape(b, n, h, dh)


def chunked_vector_scan(q, k, v, log_f, s0):
    b, h, t, dk = q.shape
    dv = v.shape[-1]
    nc = t // VEC_CHUNK
    qc, kc, fc = (a.astype(F32).reshape(b, h, nc, VEC_CHUNK, dk) for a in (q, k, log_f))
    vc = v.astype(F32).reshape(b, h, nc, VEC_CHUNK, dv)
    if s0 is None:
        s0 = jnp.zeros((b, h, dk, dv), F32)
    cum = jnp.cumsum(fc, axis=3)
    ref = cum[:, :, :, :1]
    causal = jnp.tril(jnp.ones((VEC_CHUNK, VEC_CHUNK), dtype=bool))
    att = jnp.einsum('bhcid,bhcjd->bhcij', qc * jnp.exp(cum - ref), kc * jnp.exp(ref - cum))
    att = jnp.where(causal, att, 0.0)
    o = jnp.einsum('bhcij,bhcjv->bhciv', att, vc)
    last = cum[:, :, :, -1:]
    kv = jnp.einsum('bhcjd,bhcjv->bhcdv', kc * jnp.exp(last - cum), vc)
    decay = jnp.exp(last[:, :, :, 0])

    def step(s, inp):
        kv_c, d_c = inp
        return s * d_c[..., None] + kv_c, s

    s_t, s_in = lax.scan(step, s0, (jnp.moveaxis(kv, 2, 0), jnp.moveaxis(decay, 2, 0)))
    s_in = jnp.moveaxis(s_in, 0, 2)
    o = o + jnp.einsum('bhcid,bhcdv->bhciv', qc * jnp.exp(cum), s_in)
    return o.reshape(b, h, t, dv), s_t


def vector_final_state(_q, k, v, log_f):
    cum = jnp.cumsum(log_f.astype(F32), axis=2)
    w = jnp.exp(cum[:, :, -1:] - cum)
    return jnp.einsum('bhtd,bhtv->bhdv', k.astype(F32) * w, v.astype(F32))


def chunked_ssd_scan(xdt, bm, cm, log_a, s0):
    b, t, h, p = xdt.shape
    g, n = bm.shape[2], bm.shape[3]
    r = h // g
    nc = t // CHUNK
    xc = xdt.astype(F32).reshape(b, nc, CHUNK, g, r, p)
    bc = bm.astype(F32).reshape(b, nc, CHUNK, g, n)
    cc = cm.astype(F32).reshape(b, nc, CHUNK, g, n)
    if s0 is None:
        s0 = jnp.zeros((b, g, r, p, n), F32)
    cum = jnp.cumsum(log_a.astype(F32).reshape(b, nc, CHUNK, g, r), axis=2)
    causal = jnp.tril(jnp.ones((CHUNK, CHUNK), dtype=bool))[None, None, :, :, None, None]
    seg = cum[:, :, :, None] - cum[:, :, None]
    decay = jnp.where(causal, jnp.exp(jnp.where(causal, seg, 0.0)), 0.0)
    scores = jnp.einsum('bcign,bcjgn->bcijg', cc, bc)
    y = jnp.einsum('bcijgr,bcjgrp->bcigrp', scores[..., None] * decay, xc)
    last = cum[:, :, -1:]
    st = jnp.einsum('bcjgn,bcjgrp->bcgrpn', bc, xc * jnp.exp(last - cum)[..., None])
    chunk_decay = jnp.exp(last[:, :, 0])

    def step(s, inp):
        st_c, d_c = inp
        return s * d_c[..., None, None] + st_c, s

    s_t, s_in = lax.scan(step, s0, (jnp.moveaxis(st, 1, 0), jnp.moveaxis(chunk_decay, 1, 0)))
    s_in = jnp.moveaxis(s_in, 0, 1)
    y = y + jnp.einsum('bcign,bcgrpn->bcigrp', cc, s_in) * jnp.exp(cum)[..., None]
    return y.reshape(b, t, h, p), s_t


def ssd_final_state(xdt, bm, _cm, log_a):
    b, t, h, p = xdt.shape
    g = bm.shape[2]
    cum = jnp.cumsum(log_a.astype(F32), axis=1)
    w = jnp.exp(cum[:, -1:] - cum)
    xw = (xdt.astype(F32) * w[..., None]).reshape(b, t, g, h // g, p)
    return jnp.einsum('btgn,btgrp->bgrpn', bm.astype(F32), xw)


def bidir_prefix_scan(scan_fn, final_fn, axis, ctx_dirs, lat_dirs, need_ctx_out):
    o_ctx = None
    o_lat = None
    for d in range(2):
        flip = (lambda a: jnp.flip(a, axis)) if d == 1 else (lambda a: a)
        c_args = [flip(a) for a in ctx_dirs[d]]
        l_args = [flip(a) for a in lat_dirs[d]]
        if need_ctx_out:
            oc, s_ctx = scan_fn(*c_args, None)
            oc = flip(oc)
            o_ctx = oc if o_ctx is None else o_ctx + oc
        else:
            s_ctx = final_fn(*c_args)
        ol = flip(scan_fn(*l_args, s_ctx)[0])
        o_lat = ol if o_lat is None else o_lat + ol
    return o_ctx, o_lat


def depthwise_conv_centred(x, w, bias):
    y = lax.conv_general_dilated(x, w[:, None, :].astype(x.dtype), window_strides=(1,),
                                 padding=((SSD_CONV // 2, SSD_CONV // 2),),
                                 dimension_numbers=('NWC', 'WIO', 'NWC'),
                                 feature_group_count=x.shape[-1])
    return y + bias


def even_mixer(u_ctx, u_lat, cos, sin, w_in, w_out, lb, hg_norm_g, q_norm_g, k_norm_g, need_ctx_out):
    def prep(u):
        b, n, _ = u.shape
        q, i, zf, zb, g, aq, ak, av = split_cols(u @ w_in, EVEN_SIZES)
        qh = to_heads(jax.nn.silu(q.astype(F32)), HG_HEADS) * HG_DK ** -0.5
        vh = to_heads(i.astype(F32), HG_HEADS)
        dirs = []
        for d, zraw in enumerate((zf, zb)):
            z = zraw.astype(F32)
            log_f = jnp.logaddexp(jnp.log(jnp.maximum(lb[d], LB_FLOOR)),
                                  jnp.log1p(-lb[d]) + jax.nn.log_sigmoid(z))
            k = (1.0 - lb[d]) * jax.nn.sigmoid(-z)
            dirs.append((qh, to_heads(k, HG_HEADS), vh, to_heads(log_f, HG_HEADS)))
        aq = rms_norm(aq.reshape(b, n, AT_HEADS, AT_DH), q_norm_g)
        ak = rms_norm(ak.reshape(b, n, AT_KV_HEADS, AT_DH), k_norm_g)
        av = av.reshape(b, n, AT_KV_HEADS, AT_DH)
        return dirs, g, aq, ak, av

    c_dirs, c_g, c_q, c_k, c_v = prep(u_ctx)
    l_dirs, l_g, l_q, l_k, l_v = prep(u_lat)
    hg_c, hg_l = bidir_prefix_scan(chunked_vector_scan, vector_final_state, 2, c_dirs, l_dirs, need_ctx_out)
    at_l = blocked_gqa(apply_rope(l_q, cos, sin),
                       jnp.concatenate([apply_rope(l_k, cos, sin), c_k], axis=1),
                       jnp.concatenate([l_v, c_v], axis=1))

    def finish(hg_o, g, at_o, dtype):
        b, n = g.shape[:2]
        hg = from_heads(hg_o) * jax.nn.sigmoid(g.astype(F32))
        hg = rms_norm(hg.reshape(b, n, HG_HEADS, HG_DV), hg_norm_g).reshape(b, n, HG_VW)
        mixed = jnp.concatenate([hg, at_o.reshape(b, n, AT_QW).astype(F32)], axis=-1)
        return mixed.astype(dtype) @ w_out

    o_lat = finish(hg_l, l_g, at_l, u_lat.dtype)
    o_ctx = finish(hg_c, c_g, blocked_gqa(c_q, c_k, c_v), u_ctx.dtype) if need_ctx_out else None
    return o_ctx, o_lat


def odd_mixer(u_ctx, u_lat, w_in, w_out, gate_w, gate_b, gla_norm_g, conv_w, conv_b,
              dt_bias, a_log, d_skip, ssd_norm_g, need_ctx_out):
    def prep(u):
        b, n, _ = u.shape
        q, k, v, g, lr_f, lr_b, z, xs, bm, cm, dt_f, dt_b = split_cols(u @ w_in, ODD_SIZES)
        qh = to_heads(q.astype(F32), GLA_HEADS) * GLA_DK ** -0.5
        kh = to_heads(k.astype(F32), GLA_HEADS)
        vh = to_heads(v.astype(F32), GLA_HEADS)
        gla_dirs = []
        for d, lr in enumerate((lr_f, lr_b)):
            log_f = jax.nn.log_sigmoid((lr @ gate_w[d]).astype(F32) + gate_b[d]) / GLA_GATE_NORM
            gla_dirs.append((qh, kh, vh, to_heads(log_f, GLA_HEADS)))
        xbc = jax.nn.silu(depthwise_conv_centred(jnp.concatenate([xs, bm, cm], axis=-1), conv_w, conv_b))
        xs, bm, cm = split_cols(xbc, (SSD_INNER, SSD_BCW, SSD_BCW))
        xh = xs.reshape(b, n, SSD_HEADS, SSD_DH).astype(F32)
        bm = bm.reshape(b, n, SSD_GROUPS, SSD_STATE)
        cm = cm.reshape(b, n, SSD_GROUPS, SSD_STATE)
        ssd_dirs = []
        for d, dt_raw in enumerate((dt_f, dt_b)):
            dt = jax.nn.softplus(dt_raw.astype(F32) + dt_bias[d])
            ssd_dirs.append((xh * dt[..., None], bm, cm, -jnp.exp(a_log[d].astype(F32)) * dt))
        return gla_dirs, g, ssd_dirs, xh, z

    c_gla, c_g, c_ssd, c_xh, c_z = prep(u_ctx)
    l_gla, l_g, l_ssd, l_xh, l_z = prep(u_lat)
    gla_c, gla_l = bidir_prefix_scan(chunked_vector_scan, vector_final_state, 2, c_gla, l_gla, need_ctx_out)
    ssd_c, ssd_l = bidir_prefix_scan(chunked_ssd_scan, ssd_final_state, 1, c_ssd, l_ssd, need_ctx_out)

    def finish(gla_o, g, ssd_y, xh, z, dtype):
        b, n = g.shape[:2]
        gla = from_heads(rms_norm(gla_o, gla_norm_g)) * jax.nn.silu(g.astype(F32))
        y = (ssd_y + d_skip[:, None] * xh).reshape(b, n, SSD_INNER) * jax.nn.silu(z.astype(F32))
        y = rms_norm(y.reshape(b, n, SSD_GROUPS, SSD_INNER // SSD_GROUPS),
                     ssd_norm_g.reshape(SSD_GROUPS, -1)).reshape(b, n, SSD_INNER)
        return jnp.concatenate([gla, y], axis=-1).astype(dtype) @ w_out

    o_lat = finish(gla_l, l_g, ssd_l, l_xh, l_z, u_lat.dtype)
    o_ctx = finish(gla_c, c_g, ssd_c, c_xh, c_z, u_ctx.dtype) if need_ctx_out else None
    return o_ctx, o_lat


def peer_ffn(h, wq, subkeys, u_tab, v_tab):
    b, n, d = h.shape
    q = (h @ wq).reshape(b, n, PEER_HEADS, 2, PEER_DQ // 2)
    s = jnp.einsum('bnhsk,hsek->bnhse', q, subkeys, preferred_element_type=F32)
    ts, ti = lax.top_k(s, PEER_TOPK)
    cand_s = (ts[..., 0, :, None] + ts[..., 1, None, :]).reshape(b, n, PEER_HEADS, PEER_TOPK * PEER_TOPK)
    cand_i = (ti[..., 0, :, None] * PEER_NKEYS + ti[..., 1, None, :]).reshape(b, n, PEER_HEADS, PEER_TOPK * PEER_TOPK)
    best_s, pos = lax.top_k(cand_s, PEER_TOPK)
    idx = jnp.take_along_axis(cand_i, pos, axis=-1)
    gate = jax.nn.softmax(best_s, axis=-1)
    nb = (b * n) // PEER_TOKEN_BLOCK
    hk = PEER_HEADS * PEER_TOPK
    xb = h.reshape(nb, PEER_TOKEN_BLOCK, d)
    ib = idx.reshape(nb, PEER_TOKEN_BLOCK, hk)
    gb = gate.reshape(nb, PEER_TOKEN_BLOCK, hk)

    def expert_block(args):
        xt, it, gt = args
        act = jax.nn.gelu(jnp.einsum('td,tkd->tk', xt, jnp.take(u_tab, it, axis=0), preferred_element_type=F32))
        return jnp.einsum('tk,tkd->td', (act * gt).astype(xt.dtype), jnp.take(v_tab, it, axis=0))

    return lax.map(expert_block, (xb, ib, gb)).reshape(b, n, d)


def setup_inputs(seed: int = 0) -> dict:
    key = jax.random.key(seed)
    ks = iter(jax.random.split(key, 40))
    D = D_MODEL

    def nrm(shape, std):
        return std * jax.random.normal(next(ks), shape, F32)

    dt0 = jnp.exp(jax.random.uniform(next(ks), (N_ODD, 2, SSD_HEADS), F32, math.log(1e-3), math.log(1e-1)))
    return {
        "x": nrm((BATCH, SEQ, D), 1.0),
        "c": nrm((BATCH, D), 1.0),
        "ctx": nrm((BATCH, CTX_LEN, D), 1.0),
        "c_ctx": nrm((D,), 1.0),
        "mod_w": nrm((DEPTH, D, N_MOD * D), 0.5 * D ** -0.5),
        "mod_b": nrm((DEPTH, N_MOD * D), 0.02),
        "ln_g": 1.0 + nrm((DEPTH, 2, D), 0.02),
        "ln_b": nrm((DEPTH, 2, D), 0.02),
        "ev_w_in": nrm((N_EVEN, D, EVEN_IN), D ** -0.5),
        "ev_w_out": nrm((N_EVEN, EVEN_OUT, D), DN_BETA * EVEN_OUT ** -0.5),
        "hg_lb_logits": nrm((2, N_EVEN, HG_KW), 1.0),
        "hg_norm_g": 1.0 + nrm((N_EVEN, HG_DV), 0.02),
        "at_q_norm_g": 1.0 + nrm((N_EVEN, AT_DH), 0.02),
        "at_k_norm_g": 1.0 + nrm((N_EVEN, AT_DH), 0.02),
        "od_w_in": nrm((N_ODD, D, ODD_IN), D ** -0.5),
        "od_w_out": nrm((N_ODD, ODD_OUT, D), DN_BETA * ODD_OUT ** -0.5),
        "gla_gate_w": nrm((N_ODD, 2, GLA_GATE_RANK, GLA_KW), GLA_GATE_RANK ** -0.5),
        "gla_gate_b": nrm((N_ODD, 2, GLA_KW), 0.1),
        "gla_norm_g": 1.0 + nrm((N_ODD, GLA_DV), 0.02),
        "ssd_conv_w": nrm((N_ODD, SSD_CONV, SSD_CONV_DIM), SSD_CONV ** -0.5),
        "ssd_conv_b": nrm((N_ODD, SSD_CONV_DIM), 0.02),
        "ssd_dt_bias": dt0 + jnp.log(-jnp.expm1(-dt0)),
        "ssd_a_log": jnp.log(jax.random.uniform(next(ks), (N_ODD, 2, SSD_HEADS), F32, 1.0, 16.0)),
        "ssd_d": 1.0 + nrm((N_ODD, SSD_HEADS), 0.02),
        "ssd_norm_g": 1.0 + nrm((N_ODD, SSD_INNER), 0.02),
        "peer_wq": nrm((DEPTH, D, PEER_HEADS * PEER_DQ), D ** -0.5),
        "peer_subkeys": nrm((DEPTH, PEER_HEADS, 2, PEER_NKEYS, PEER_DQ // 2), (PEER_DQ // 2) ** -0.5),
        "peer_u": nrm((DEPTH, PEER_EXPERTS, D), D ** -0.5),
        "peer_v": nrm((DEPTH, PEER_EXPERTS, D), DN_BETA * (PEER_TOPK / PEER_HEADS) ** 0.5),
    }


def reference(x, c, ctx, c_ctx, mod_w, mod_b, ln_g, ln_b, ev_w_in, ev_w_out, hg_lb_logits,
              hg_norm_g, at_q_norm_g, at_k_norm_g, od_w_in, od_w_out, gla_gate_w, gla_gate_b,
              gla_norm_g, ssd_conv_w, ssd_conv_b, ssd_dt_bias, ssd_a_log, ssd_d, ssd_norm_g,
              peer_wq, peer_subkeys, peer_u, peer_v):
    rows = x.shape[1] // GRID_W
    cos, sin = axial_rope_tables(rows)
    sm = jax.nn.softmax(hg_lb_logits.astype(F32), axis=1)
    hg_lb = jnp.cumsum(sm, axis=1) - sm[:, :1]
    h, hc = x, ctx
    for l in range(DEPTH):
        last = l == DEPTH - 1
        sh1, sc1, g1, sh2, sc2, g2 = modulation(c, mod_w[l], mod_b[l])
        csh1, csc1, cg1, csh2, csc2, cg2 = modulation(c_ctx[None, :], mod_w[l], mod_b[l])
        u_lat = h * (1.0 + sc1) + sh1
        u_ctx = hc * (1.0 + csc1) + csh1
        j = l // 2
        if l % 2 == 0:
            o_ctx, o_lat = even_mixer(u_ctx, u_lat, cos, sin, ev_w_in[j], ev_w_out[j], hg_lb[:, j],
                                      hg_norm_g[j], at_q_norm_g[j], at_k_norm_g[j], not last)
        else:
            o_ctx, o_lat = odd_mixer(u_ctx, u_lat, od_w_in[j], od_w_out[j], gla_gate_w[j], gla_gate_b[j],
                                     gla_norm_g[j], ssd_conv_w[j], ssd_conv_b[j], ssd_dt_bias[j],
                                     ssd_a_log[j], ssd_d[j], ssd_norm_g[j], not last)
        h = layer_norm(DN_ALPHA * h + g1 * o_lat, ln_g[l, 0], ln_b[l, 0])
        h = layer_norm(DN_ALPHA * h + g2 * peer_ffn(h * (1.0 + sc2) + sh2, peer_wq[l], peer_subkeys[l],
                                                    peer_u[l], peer_v[l]), ln_g[l, 1], ln_b[l, 1])
        if not last:
            hc = layer_norm(DN_ALPHA * hc + cg1 * o_ctx, ln_g[l, 0], ln_b[l, 0])
            hc = layer_norm(DN_ALPHA * hc + cg2 * peer_ffn(hc * (1.0 + csc2) + csh2, peer_wq[l], peer_subkeys[l],
                                                           peer_u[l], peer_v[l]), ln_g[l, 1], ln_b[l, 1])
    return h
```

```python
import numpy as np
from contextlib import ExitStack
import concourse.bass as bass
import concourse.mybir as mybir
from concourse.bass_utils import run_bass_kernel_spmd

F32 = mybir.dt.float32
BF16 = mybir.dt.bfloat16
U32 = mybir.dt.uint32
I32 = mybir.dt.int32
AF = mybir.ActivationFunctionType
ALU = mybir.AluOpType
AX = mybir.AxisListType

NCORES = 8
ENGS = ("pe", "dve", "act", "pool", "sp")


class Buf:
    __slots__ = ("ap", "name", "w", "r", "sem", "is_dram")

    def __init__(self, ap, name, is_dram=False):
        self.ap = ap
        self.name = name
        self.w = None
        self.r = []
        self.sem = None
        self.is_dram = is_dram

    def __getitem__(self, idx):
        return self.ap[idx]


SEM_LIMIT = 30000


class P:
    def __init__(self, name="k"):
        self.nc = bass.Bass("TRN2", target_bir_lowering=False)
        self.es = ExitStack()
        self.scopes = []
        nc_ = self.nc
        self.engs = {"pe": nc_.tensor, "dve": nc_.vector, "act": nc_.scalar, "pool": nc_.gpsimd, "sp": nc_.sync}
        self.known = {e: {} for e in ENGS}
        self.nsem = 0
        self.esem = {}
        self.ecnt = {}
        self.retired = []
        for e in ENGS:
            self._new_esem(e)
        self.dpool = {'sw': [], 'hw': []}
        self.dlive = []
        self.nbuf = 0
        self.out_events = []

    def _alloc_sem(self, tag):
        self.nsem += 1
        return self.es.enter_context(self.nc.semaphore(f"{tag}{self.nsem}"))

    def _new_esem(self, e):
        if e in self.esem:
            self.retired.append((id(self.esem[e]), self.ecnt[e], self.esem[e], e == "pe"))
        self.esem[e] = self._alloc_sem(f"se_{e}_")
        self.ecnt[e] = 0

    def _stack(self):
        return self.scopes[-1][0] if self.scopes else self.es

    def dram(self, name, shape, dtype, kind="Internal"):
        t = self.nc.dram_tensor(name, list(shape), dtype, kind=kind)
        return Buf(t.ap(), name, is_dram=True)

    def sbuf(self, shape, dtype, name=None):
        self.nbuf += 1
        name = f"{name or 'sb'}_{self.nbuf}"
        t = self._stack().enter_context(self.nc.sbuf_tensor(name, list(shape), dtype))
        return Buf(t, name)

    def psum(self, shape, dtype=F32, name=None):
        self.nbuf += 1
        name = f"{name or 'ps'}_{self.nbuf}"
        t = self._stack().enter_context(self.nc.psum_tensor(name, list(shape), dtype))
        return Buf(t, name)

    def view(self, ap, name="v"):
        return Buf(ap, name)

    def open_scope(self):
        self.scopes.append((ExitStack(), []))

    def close_scope(self):
        self.barrier()
        st, bufs = self.scopes.pop()
        for (b, kind, cur) in bufs:
            self.dpool[kind].append(cur)
            if b.sem is not None and b.sem.get(kind) is cur:
                del b.sem[kind]
        st.close()

    def barrier(self):
        evs = []
        for e in ENGS:
            if self.ecnt[e] > 0:
                evs.append((id(self.esem[e]), self.ecnt[e], self.esem[e], False))
        for s in self.dlive:
            if s[1] > 0:
                evs.append((id(s[0]), s[1], s[0], False))
        for e in ENGS:
            waits = []
            for sid, val, semh, _ in evs:
                if self.known[e].get(sid, 0) >= val:
                    continue
                if sid == id(self.esem[e]) and e == "pe":
                    pass
                self.known[e][sid] = val
                waits.append((semh, val))
            if waits:
                for s, v in waits:
                    self.engs[e].wait_ge(s, v)

    def _deps(self, eng, reads, writes):
        deps = []
        for b in reads:
            if b.w is not None:
                deps.append(b.w)
        for b in writes:
            if b.r:
                deps.extend(b.r)
            elif b.w is not None:
                deps.append(b.w)
        need = {}
        for ev in deps:
            sid, val, semh, is_pe = ev
            if is_pe and eng == "pe":
                continue
            if self.known[eng].get(sid, 0) >= val:
                continue
            if sid not in need or need[sid][0] < val:
                need[sid] = (val, semh)
        waits = []
        for sid, (val, semh) in need.items():
            self.known[eng][sid] = val
            waits.append((semh, val))
        return waits

    def _commit(self, ev, reads, writes):
        for b in reads:
            b.r.append(ev)
            if len(b.r) > 48:
                b.r = b.r[-48:]
        for b in writes:
            b.w = ev
            b.r = []

    def op(self, eng, fn, reads=(), writes=()):
        if self.ecnt[eng] >= SEM_LIMIT:
            self._new_esem(eng)
        waits = self._deps(eng, reads, writes)
        self.ecnt[eng] += 1
        semh = self.esem[eng]
        ev = (id(semh), self.ecnt[eng], semh, eng == "pe")

        def emit(e, waits=waits, fn=fn, semh=semh):
            for s, v in waits:
                e.wait_ge(s, v)
            fn(e).then_inc(semh, 1)

        emit(self.engs[eng])
        self._commit(ev, reads, writes)
        return ev

    def dma(self, q, out_b, out_ap, in_b, in_ap, fn=None, extra_reads=(), **kw):
        owner = out_b if not out_b.is_dram else in_b
        kind = "sw" if q == "pool" else "hw"
        if owner.sem is None:
            owner.sem = {}
        cur = owner.sem.get(kind)
        if cur is None or cur[1] >= SEM_LIMIT:
            cand = [s for s in self.dpool[kind] if s[1] < SEM_LIMIT]
            if cand:
                cur = cand[0]
                self.dpool[kind].remove(cur)
            else:
                cur = [self._alloc_sem("sd_"), 0]
                self.dlive.append(cur)
            owner.sem[kind] = cur
            if self.scopes:
                self.scopes[-1][1].append((owner, kind, cur))
        waits = self._deps(q, [in_b] + list(extra_reads), [out_b])
        cur[1] += 16
        semh = cur[0]
        ev = (id(semh), cur[1], semh, False)

        def emit(e, waits=waits, semh=semh):
            for s, v in waits:
                e.wait_ge(s, v)
            if fn is None:
                e.dma_start(out=out_ap, in_=in_ap, **kw).then_inc(semh, 16)
            else:
                fn(e).then_inc(semh, 16)

        emit(self.engs[q])
        self._commit(ev, [in_b] + list(extra_reads), [out_b])
        return ev

    def finish(self):
        while self.scopes:
            self.close_scope()
        self.barrier()
        nc = self.nc
        self.es.close()
        return nc

    def load(self, dst, dst_ap, src, src_ap, q="sp", **kw):
        return self.dma(q, dst, dst_ap, src, src_ap, **kw)

    def store(self, dst, dst_ap, src, src_ap, q="pool", **kw):
        return self.dma(q, dst, dst_ap, src, src_ap, **kw)

    def mm(self, out_b, out_ap, lhsT_b, lhsT_ap, rhs_b, rhs_ap, start=True, stop=True, extra_reads=()):
        return self.op("pe", lambda e: e.matmul(out_ap, lhsT_ap, rhs_ap, start=start, stop=stop),
                       reads=[lhsT_b, rhs_b] + list(extra_reads), writes=[out_b])

    def transpose(self, out_b, out_ap, in_b, in_ap, ident_b, ident_ap):
        return self.op("pe", lambda e: e.transpose(out_ap, in_ap, ident_ap),
                       reads=[in_b, ident_b], writes=[out_b])

    def act(self, out_b, out_ap, in_b, in_ap, func, bias=None, scale=None, reads=(), accum=None, accum_b=None):
        kw = {}
        if bias is not None:
            kw["bias"] = bias
        if scale is not None:
            kw["scale"] = scale
        if accum is not None:
            kw["accum_out"] = accum
        w = [out_b] + ([accum_b] if accum_b is not None else [])
        return self.op("act", lambda e: e.activation(out_ap, in_ap, func, **kw),
                       reads=[in_b] + list(reads), writes=w)

    def tt(self, out_b, out_ap, a_b, a_ap, b_b, b_ap, op, eng="dve"):
        return self.op(eng, lambda e: e.tensor_tensor(out_ap, a_ap, b_ap, op),
                       reads=[a_b, b_b], writes=[out_b])

    def ts(self, out_b, out_ap, in_b, in_ap, s1, s2, op0, op1=None, reads=(), eng="dve", accum=None, accum_b=None):
        kw = {}
        if accum is not None:
            kw["accum_out"] = accum
        w = [out_b] + ([accum_b] if accum_b is not None else [])
        if op1 is None:
            return self.op(eng, lambda e: e.tensor_scalar(out_ap, in_ap, s1, None, op0, **kw),
                           reads=[in_b] + list(reads), writes=w)
        return self.op(eng, lambda e: e.tensor_scalar(out_ap, in_ap, s1, s2, op0, op1, **kw),
                       reads=[in_b] + list(reads), writes=w)

    def stt(self, out_b, out_ap, a_b, a_ap, scalar, b_b, b_ap, op0, op1, reads=(), accum=None, accum_b=None):
        kw = {}
        if accum is not None:
            kw["accum_out"] = accum
        w = [out_b] + ([accum_b] if accum_b is not None else [])
        return self.op("dve", lambda e: e.scalar_tensor_tensor(out_ap, a_ap, scalar, b_ap, op0, op1, **kw),
                       reads=[a_b, b_b] + list(reads), writes=w)

    def copy(self, out_b, out_ap, in_b, in_ap, eng="dve"):
        if eng == "act":
            return self.op("act", lambda e: e.activation(out_ap, in_ap, AF.Copy), reads=[in_b], writes=[out_b])
        return self.op(eng, lambda e: e.tensor_copy(out_ap, in_ap), reads=[in_b], writes=[out_b])

    def memset(self, out_b, out_ap, val, eng="dve"):
        return self.op(eng, lambda e: e.memset(out_ap, val), reads=[], writes=[out_b])


def run(prog, in_maps):
    nc = prog.finish()
    res = run_bass_kernel_spmd(nc, in_maps, core_ids=list(range(NCORES)))
    return res.results


D = 1024
KC = D // 128
N_MOD = 6
NORM_EPS = 1e-6
GRID_W = 64
LB_FLOOR = 1e-30
CH = 32
EV = dict(q=0, i=512, zf=1024, zb=1536, g=2048, aq=2560, ak=3072, av=3328, aqs=3584, aks=4096)
EV_NF = 4352
OD = dict(q=0, k=256, v=512, g=1024, z=1536, xs=2048, bm=2560, cm=2816, lrf=3072, lrb=3088, dtf=3104, dtb=3112)
OD_NF = 3200
PEER_HEADS, PEER_NKEYS, PEER_TOPK = 8, 128, 16


class Cfg:
    def __init__(self, nb, seq, ctx, depth):
        self.nb, self.seq, self.ctx, self.depth = nb, seq, ctx, depth
        self.tb = seq + ctx
        self.T = nb * self.tb
        self.R = nb + 1
        self.alpha = (2 * depth) ** 0.25
        assert seq % 128 == 0 and ctx % 128 == 0

    def segs(self):
        out = []
        for b in range(self.nb):
            o = b * self.tb
            out.append((o, o + self.ctx, self.nb))
            out.append((o + self.ctx, o + self.tb, b))
        return out

    def seg_of_tile(self, ti):
        t = ti * 128
        for (a, b, r) in self.segs():
            if a <= t < b:
                return r
        raise AssertionError


def blocks(n, step):
    return [(a, min(a + step, n)) for a in range(0, n, step)]


def stage_mod(p, cfg, io, G):
    R = cfg.R
    NM = N_MOD * D
    p.open_scope()
    cT = p.sbuf([128, KC * R], F32, "cT")
    p.load(cT, cT[:, :], io["cT"], io["cT"].ap[:, :])
    sc = p.sbuf([128, KC, R], F32, "silu_c")
    p.act(sc, sc.ap[:, :, :].rearrange("p k r -> p (k r)"), cT, cT[:, :], AF.Silu)
    mbT = p.sbuf([128, cfg.depth * 48], F32, "mbT")
    p.load(mbT, mbT[:, :], io["mod_bT"], io["mod_bT"].ap[:, :])
    CB = 768
    wst = [p.sbuf([128, KC, CB], F32, f"mw{i}") for i in range(2)]
    psF = [p.psum([128, 512], F32, f"psF{i}") for i in range(2)]
    psT = [p.psum([128, 512], F32, f"psT{i}") for i in range(2)]
    mbrow = p.sbuf([R, NM], F32, "mbrow")
    mtok = p.sbuf([R, NM], F32, "mtok")
    modT = G["modT"]
    it = 0
    for l in range(cfg.depth):
        p.load(mbrow, mbrow[:, :], io["mod_b"], io["mod_b"].ap[l:l + 1, :].partition_broadcast(R))
        for cb in range(NM // CB):
            w = wst[it % 2]
            src = io["mod_w"].ap[l, :, cb * CB:(cb + 1) * CB].rearrange("(kc p) n -> p kc n", p=128)
            p.load(w, w[:, :, :], io["mod_w"], src, q="sp" if it % 2 else "act")
            pf = psF[it % 2]
            for dcl in range(CB // 128):
                dc = cb * (CB // 128) + dcl
                for kc in range(KC):
                    p.mm(pf, pf[:, dcl * R:(dcl + 1) * R], w, w[:, kc, dcl * 128:(dcl + 1) * 128],
                         sc, sc[:, kc, :], start=(kc == 0), stop=(kc == KC - 1))
                p.ts(modT, modT[:, l, dc, :], pf, pf[:, dcl * R:(dcl + 1) * R],
                     mbT[:, l * 48 + dc:l * 48 + dc + 1], None, ALU.add, reads=[mbT])
            pt = psT[it % 2]
            for (n0, n1) in blocks(CB, 512):
                for kc in range(KC):
                    p.mm(pt, pt[0:R, 0:n1 - n0], sc, sc[:, kc, :], w, w[:, kc, n0:n1],
                         start=(kc == 0), stop=(kc == KC - 1))
                p.tt(mtok, mtok[:, cb * CB + n0:cb * CB + n1], pt, pt[0:R, 0:n1 - n0],
                     mbrow, mbrow[:, cb * CB + n0:cb * CB + n1], ALU.add)
            it += 1
        p.store(G["modtok"], G["modtok"].ap[l, :, :], mtok, mtok[:, :])
    p.close_scope()


def stage_inproj(p, cfg, G, l, W_ap, NF, YT):
    T = cfg.T
    p.open_scope()
    ident = G["ident"]
    modT = G["modT"]
    w_bf = p.sbuf([128, KC, NF], BF16, "w_bf")
    wst = [p.sbuf([128, NF], F32, f"wst{i}") for i in range(2)]
    for kc in range(KC):
        st = wst[kc % 2]
        p.load(st, st[:, :], G["ext"], W_ap[kc * 128:(kc + 1) * 128, :], q="sp" if kc % 2 else "act")
        p.copy(w_bf, w_bf[:, kc, :], st, st[:, :], eng="pool" if kc % 2 else "dve")
    scale1 = p.sbuf([128, KC, cfg.R], F32, "scale1")
    p.ts(scale1, scale1[:, :, :], modT, modT[:, l, 8:16, :], 1.0, None, ALU.add)
    hst = [p.sbuf([128, D], F32, f"hst{i}") for i in range(2)]
    uT = [p.sbuf([128, KC, 512], BF16, f"uT{i}") for i in range(2)]
    pT = [p.psum([128, 4, 128], F32, f"pT{i}") for i in range(4)]
    pY = [p.psum([128, 512], F32, f"pY{i}") for i in range(3)]
    ysb = [p.sbuf([128, 512], F32, f"ysb{i}") for i in range(4)]
    j = 0
    for bi, (t0, t1) in enumerate(blocks(T, 512)):
        u = uT[bi % 2]
        nt = (t1 - t0) // 128
        for tl in range(nt):
            ti = t0 // 128 + tl
            r = cfg.seg_of_tile(ti)
            hs = hst[ti % 2]
            p.load(hs, hs[:, :], G["H"], G["H"].ap[ti * 128:(ti + 1) * 128, :], q="sp")
            for half in range(2):
                pt = pT[(2 * ti + half) % 4]
                for kk in range(4):
                    kc = half * 4 + kk
                    p.transpose(pt, pt[:, kk, :], hs, hs[:, kc * 128:(kc + 1) * 128], ident, ident[:, :])
                for kk in range(4):
                    kc = half * 4 + kk
                    p.act(u, u[:, kc, tl * 128:(tl + 1) * 128], pt, pt[:, kk, :], AF.Identity,
                          bias=modT[:, l, kc, r:r + 1], scale=scale1[:, kc, r:r + 1], reads=[modT, scale1])
        w = t1 - t0
        for fc in range(NF // 128):
            py = pY[j % 3]
            for kc in range(KC):
                p.mm(py, py[:, 0:w], w_bf, w_bf[:, kc, fc * 128:(fc + 1) * 128], u, u[:, kc, 0:w],
                     start=(kc == 0), stop=(kc == KC - 1))
            ys = ysb[j % 4]
            p.copy(ys, ys[:, 0:w], py, py[:, 0:w], eng="dve" if j % 2 else "act")
            p.store(YT, YT.ap[fc * 128:(fc + 1) * 128, t0:t1], ys, ys[:, 0:w], q="pool" if j % 2 else "sp")
            j += 1
    p.close_scope()


def layernorm_tile(p, cfg, G, r_b, out_b, lng, lnb, scr):
    stats, mv, rstd, tmp = scr
    for c in range(2):
        p.op("dve", lambda e, c=c: e.bn_stats(stats[:, c, :], r_b[:, c * 512:(c + 1) * 512]),
             reads=[r_b], writes=[stats])
    p.op("dve", lambda e: e.bn_aggr(mv[:, :], stats.ap[:, :, :].rearrange("p a b -> p (a b)")),
         reads=[stats], writes=[mv])
    p.act(rstd, rstd[:, 0:1], mv, mv[:, 1:2], AF.Ln, bias=float(NORM_EPS))
    p.act(rstd, rstd[:, 1:2], rstd, rstd[:, 0:1], AF.Exp, scale=-0.5)
    p.ts(tmp, tmp[:, :], r_b, r_b[:, :], mv[:, 0:1], rstd[:, 1:2], ALU.subtract, ALU.mult, reads=[mv, rstd])
    p.tt(tmp, tmp[:, :], tmp, tmp[:, :], lng, lng[:, :], ALU.mult)
    p.tt(out_b, out_b[:, :], tmp, tmp[:, :], lnb, lnb[:, :], ALU.add, eng="pool")


def stage_resid_ln(p, cfg, G, io, l, which, W_ap, mix_dram):
    T = cfg.T
    p.open_scope()
    gate_off = (2 if which == 0 else 5) * D
    lng = p.sbuf([128, D], F32, "lng")
    lnb = p.sbuf([128, D], F32, "lnb")
    p.load(lng, lng[:, :], io["ln_g"], io["ln_g"].ap[l, which:which + 1, :].partition_broadcast(128))
    p.load(lnb, lnb[:, :], io["ln_b"], io["ln_b"].ap[l, which:which + 1, :].partition_broadcast(128))
    gb = []
    for r in range(cfg.R):
        g = p.sbuf([128, D], F32, f"gate{r}")
        p.load(g, g[:, :], G["modtok"], G["modtok"].ap[l, r:r + 1, gate_off:gate_off + D].partition_broadcast(128))
        gb.append(g)
    if which == 0:
        w_bf = p.sbuf([128, KC, D], BF16, "wo_bf")
        wst = [p.sbuf([128, D], F32, f"wost{i}") for i in range(2)]
        for kc in range(KC):
            st = wst[kc % 2]
            p.load(st, st[:, :], G["ext"], W_ap[kc * 128:(kc + 1) * 128, :], q="act")
            p.copy(w_bf, w_bf[:, kc, :], st, st[:, :], eng="pool")
        mx = [p.sbuf([128, KC, 128], BF16, f"mx{i}") for i in range(2)]
        po = [p.psum([128, 2, 512], F32, f"po{i}") for i in range(2)]
    else:
        br = [p.sbuf([128, D], F32, f"br{i}") for i in range(2)]
    hs = [p.sbuf([128, D], F32, f"h{i}") for i in range(2)]
    rb = [p.sbuf([128, D], F32, f"r{i}") for i in range(2)]
    ob = [p.sbuf([128, D], F32, f"o{i}") for i in range(2)]
    scr = (p.sbuf([128, 2, 6], F32, "stats"), p.sbuf([128, 2], F32, "mv"), p.sbuf([128, 2], F32, "rstd"),
           p.sbuf([128, D], F32, "lntmp"))
    for ti in range(T // 128):
        r = cfg.seg_of_tile(ti)
        h = hs[ti % 2]
        rr = rb[ti % 2]
        p.load(h, h[:, :], G["H"], G["H"].ap[ti * 128:(ti + 1) * 128, :], q="sp")
        if which == 0:
            m = mx[ti % 2]
            p.load(m, m[:, :, :], mix_dram,
                   mix_dram.ap[:, ti * 128:(ti + 1) * 128].rearrange("(kc p) t -> p kc t", p=128), q="act")
            ps = po[ti % 2]
            for nb in range(2):
                for kc in range(KC):
                    p.mm(ps, ps[:, nb, :], m, m[:, kc, :], w_bf, w_bf[:, kc, nb * 512:(nb + 1) * 512],
                         start=(kc == 0), stop=(kc == KC - 1))
            p.tt(rr, rr.ap[:, :], ps, ps.ap[:, :, :].rearrange("p a b -> p (a b)"), gb[r], gb[r][:, :], ALU.mult)
        else:
            b_ = br[ti % 2]
            p.load(b_, b_[:, :], mix_dram, mix_dram.ap[ti * 128:(ti + 1) * 128, :], q="act")
            p.tt(rr, rr[:, :], b_, b_[:, :], gb[r], gb[r][:, :], ALU.mult, eng="pool")
        p.stt(rr, rr[:, :], h, h[:, :], float(cfg.alpha), rr, rr[:, :], ALU.mult, ALU.add)
        o = ob[ti % 2]
        layernorm_tile(p, cfg, G, rr, o, lng, lnb, scr)
        p.store(G["H"], G["H"].ap[ti * 128:(ti + 1) * 128, :], o, o[:, :], q="pool")
    p.close_scope()


def build_program(cfg, stop_after=None, debug=()):
    p = P("mk")
    io = {}
    G = {}

    def ext(name, shape, dtype=F32):
        io[name] = p.dram(name, shape, dtype, "ExternalInput")
        return io[name]

    T, R, L = cfg.T, cfg.R, cfg.depth
    ne, no = (L + 1) // 2, L // 2
    ext("h0", [T, D])
    ext("cT", [128, KC * R])
    ext("mod_w", [L, D, N_MOD * D])
    ext("mod_b", [L, N_MOD * D])
    ext("mod_bT", [128, L * 48])
    ext("ln_g", [L, 2, D])
    ext("ln_b", [L, 2, D])
    ext("ev_w_in", [ne, D, EV_NF])
    ext("ev_w_out", [ne, D, D])
    if no:
        ext("od_w_in", [no, D, OD_NF])
        ext("od_w_out", [no, D, D])
    ext("ident", [128, 128])
    ext("peer_wq", [L, D, 2048])
    ext("peer_subkT", [L, 128, 2048])
    for l_ in range(L):
        ext(f"peer_u{l_}", [PEER_NKEYS * PEER_NKEYS, D])
        ext(f"peer_v{l_}", [PEER_NKEYS * PEER_NKEYS, D])
    ext("cmask", [128, 128])
    ext("iota16", [128, 16])
    ext("rm3", [128, 1])
    ext("rmask", [128, cfg.tb])
    ext("cosT", [128, cfg.tb])
    ext("sinT", [128, cfg.tb])
    ext("hg_lbT", [128, 2 * ne * 4])
    ext("hg_norm_gT", [ne, 128, 1])
    ext("at_normT", [ne, 128, 4])
    if no:
        ext("ssd_conv_wT", [no, 128, 40])
        ext("ssd_conv_bT", [no, 128, 8])
        ext("ssd_dt_biasT", [no, 8, 2])
        ext("ssd_a_logT", [no, 8, 2])
        ext("ssd_dT", [no, 128, 4])
        ext("ssd_norm_gT", [no, 128, 4])
        ext("selT", [8, 8 * 128])
        ext("mbias", [128, 128])
        ext("rmask128", [8, cfg.tb])
        ext("gla_gate_w", [no, 2, 16, 256])
        ext("gla_gate_bT", [no, 64, 8])
        ext("gla_norm_gT", [no, 128, 1])
    G["ext"] = Buf(None, "ext", is_dram=True)
    out = p.dram("out", [T, D], F32, "ExternalOutput")
    G["H"] = out
    kind = "ExternalOutput" if debug else "Internal"
    G["modtok"] = p.dram("modtok", [L, R, N_MOD * D], F32, kind)
    G["YT"] = p.dram("YT", [EV_NF, T], F32, kind)
    G["MIXT"] = p.dram("MIXT", [D, T], BF16, kind)
    G["PEERO"] = p.dram("PEERO", [T, D], F32, kind)
    G["UVB"] = p.dram("UVB", [PEER_NKEYS * PEER_NKEYS, 2 * D], BF16, "Internal")
    G["XSF"] = p.dram("XSF", [512, T], F32, "Internal")
    G["YPRE"] = p.dram("YPRE", [512, T], F32, "Internal")
    G["ident"] = p.sbuf([128, 128], F32, "ident")
    p.load(G["ident"], G["ident"][:, :], io["ident"], io["ident"].ap[:, :])
    G["ident_bf"] = p.sbuf([128, 128], BF16, "ident_bf")
    p.copy(G["ident_bf"], G["ident_bf"][:, :], G["ident"], G["ident"][:, :])
    G["modT"] = p.sbuf([128, L, 48, R], F32, "modT")
    G["ones_mean"] = p.sbuf([128, 128], F32, "ones_mean")
    p.memset(G["ones_mean"], G["ones_mean"][:, :], 1.0 / 128.0)
    for nm in ("cmask", "rm3"):
        G[nm] = p.sbuf(list(io[nm].ap.shape), F32, nm)
        p.load(G[nm], G[nm][:, :], io[nm], io[nm].ap[:, :])

    p.open_scope()
    cp = [p.sbuf([128, D], F32, f"cp{i}") for i in range(3)]
    for ti in range(T // 128):
        c = cp[ti % 3]
        p.load(c, c[:, :], io["h0"], io["h0"].ap[ti * 128:(ti + 1) * 128, :], q="sp")
        p.store(G["H"], G["H"].ap[ti * 128:(ti + 1) * 128, :], c, c[:, :], q="act")
    p.close_scope()
    stage_mod(p, cfg, io, G)
    if stop_after == "mod":
        return p
    for l in range(L):
        j = l // 2
        if l % 2 == 0:
            stage_inproj(p, cfg, G, l, io["ev_w_in"].ap[j], EV_NF, G["YT"])
        else:
            stage_inproj(p, cfg, G, l, io["od_w_in"].ap[j], OD_NF, G["YT"])
        if stop_after == f"inproj{l}":
            return p
        if l % 2 == 0:
            stage_vscan(p, cfg, G, io, l, "hgrn")
            if stop_after == f"scan{l}":
                return p
            stage_attn(p, cfg, G, io, l)
            if stop_after == f"attn{l}":
                return p
            stage_resid_ln(p, cfg, G, io, l, 0, io["ev_w_out"].ap[j], G["MIXT"])
        else:
            stage_vscan(p, cfg, G, io, l, "gla")
            if stop_after == f"scan{l}":
                return p
            stage_ssd(p, cfg, G, io, l)
            if stop_after == f"ssd{l}":
                return p
            stage_resid_ln(p, cfg, G, io, l, 0, io["od_w_out"].ap[j], G["MIXT"])
        if stop_after == f"mix{l}":
            return p
        stage_peer(p, cfg, G, io, l)
        if stop_after == f"peer{l}":
            return p
        stage_resid_ln(p, cfg, G, io, l, 1, None, G["PEERO"])
        if stop_after == f"layer{l}":
            return p
    return p


def _pair_swap(n):
    idx = np.arange(n)
    return idx ^ 1


def prep_inputs(inp, cfg, ncores):
    L = cfg.depth
    f32 = np.float32
    x, c, ctx, c_ctx = (np.asarray(inp[k], f32) for k in ("x", "c", "ctx", "c_ctx"))
    ev_w_in = np.asarray(inp["ev_w_in"], f32)
    aq = ev_w_in[:, :, EV["aq"]:EV["aq"] + 512][:, :, _pair_swap(512)]
    ak = ev_w_in[:, :, EV["ak"]:EV["ak"] + 256][:, :, _pair_swap(256)]
    ev_ext = np.ascontiguousarray(np.concatenate([ev_w_in, aq, ak], axis=2))
    od_w_in = np.asarray(inp["od_w_in"], f32)
    no = od_w_in.shape[0]
    if no:
        q, k, v, g, lrf, lrb, z, xs, bm, cm, dtf, dtb = np.split(
            od_w_in, np.cumsum([256, 256, 512, 512, 16, 16, 512, 512, 256, 256, 8, 8])[:-1].tolist(), axis=2)
        pad = np.zeros((no, D, OD_NF - 3120), f32)
        od_ext = np.ascontiguousarray(np.concatenate([q, k, v, g, z, xs, bm, cm, lrf, lrb, dtf, dtb, pad], axis=2))
    mod_b = np.asarray(inp["mod_b"], f32)
    mod_bT = np.ascontiguousarray(mod_b.reshape(L, 48, 128).transpose(2, 0, 1).reshape(128, L * 48))
    shared = {
        "mod_w": np.asarray(inp["mod_w"], f32), "mod_b": mod_b, "mod_bT": mod_bT,
        "ln_g": np.asarray(inp["ln_g"], f32), "ln_b": np.asarray(inp["ln_b"], f32),
        "ev_w_in": ev_ext, "ev_w_out": np.asarray(inp["ev_w_out"], f32),
        "ident": np.eye(128, dtype=f32),
    }
    shared["peer_wq"] = np.asarray(inp["peer_wq"], f32)
    sk = np.asarray(inp["peer_subkeys"], f32)
    shared["peer_subkT"] = np.ascontiguousarray(sk.transpose(0, 4, 1, 2, 3).reshape(L, 128, 2048))
    for l_ in range(L):
        shared[f"peer_u{l_}"] = np.asarray(inp["peer_u"][l_], f32)
        shared[f"peer_v{l_}"] = np.asarray(inp["peer_v"][l_], f32)
    jj = np.arange(128)
    shared["cmask"] = ((jj[:, None] // CH == jj[None, :] // CH) & (jj[:, None] <= jj[None, :])).astype(f32)
    shared["iota16"] = np.tile(np.arange(16, dtype=f32), (128, 1))
    shared["rm3"] = (jj >= 96).astype(f32)[:, None].copy()
    rm = np.ones((128, cfg.tb), f32)
    rm[:, ::CH] = 0.0
    shared["rmask"] = rm
    rows_ = cfg.seq // GRID_W
    trow = np.repeat(np.arange(rows_), GRID_W).astype(np.float64)
    tcol = np.tile(np.arange(GRID_W), rows_).astype(np.float64)
    inv = 10000.0 ** (-np.arange(32, dtype=np.float64) / 32)
    ang = np.concatenate([trow[:, None] * inv, tcol[:, None] * inv], axis=-1)
    angd = np.repeat(ang, 2, axis=1).T
    cosT = np.ones((128, cfg.tb)); sinT = np.zeros((128, cfg.tb))
    cosT[:, cfg.ctx:] = np.cos(angd)
    sgn = np.where(np.arange(128) % 2 == 0, -1.0, 1.0)[:, None]
    sinT[:, cfg.ctx:] = np.sin(angd) * sgn
    shared["cosT"] = cosT.astype(f32); shared["sinT"] = sinT.astype(f32)
    ne = ev_w_in.shape[0]
    lbl = np.asarray(inp["hg_lb_logits"], f32)
    shared["hg_lbT"] = np.ascontiguousarray(lbl.reshape(2, ne, 4, 128).transpose(3, 0, 1, 2).reshape(128, 2 * ne * 4))
    shared["hg_norm_gT"] = np.ascontiguousarray(np.asarray(inp["hg_norm_g"], f32)[:, :, None])
    gqn = np.asarray(inp["at_q_norm_g"], f32); gkn = np.asarray(inp["at_k_norm_g"], f32)
    sw = _pair_swap(128)
    shared["at_normT"] = np.ascontiguousarray(np.stack([gqn, gqn[:, sw], gkn, gkn[:, sw]], axis=2))
    if no:
        cwt = np.asarray(inp["ssd_conv_w"], f32)
        shared["ssd_conv_wT"] = np.ascontiguousarray(cwt.reshape(no, 5, 8, 128).transpose(0, 3, 2, 1).reshape(no, 128, 40))
        shared["ssd_conv_bT"] = np.ascontiguousarray(np.asarray(inp["ssd_conv_b"], f32).reshape(no, 8, 128).transpose(0, 2, 1))
        shared["ssd_dt_biasT"] = np.ascontiguousarray(np.asarray(inp["ssd_dt_bias"], f32).transpose(0, 2, 1))
        shared["ssd_a_logT"] = np.ascontiguousarray(np.asarray(inp["ssd_a_log"], f32).transpose(0, 2, 1))
        dd = np.asarray(inp["ssd_d"], f32)
        shared["ssd_dT"] = np.ascontiguousarray(np.repeat(dd, 64, axis=1).reshape(no, 4, 128).transpose(0, 2, 1))
        shared["ssd_norm_gT"] = np.ascontiguousarray(np.asarray(inp["ssd_norm_g"], f32).reshape(no, 4, 128).transpose(0, 2, 1))
        selT = np.zeros((8, 8, 128), f32)
        for h_ in range(8):
            selT[h_, h_, :] = 1.0
        shared["selT"] = selT.reshape(8, 8 * 128)
        shared["mbias"] = np.where(jj[:, None] <= jj[None, :], 0.0, -30000.0).astype(f32)
        rm128 = np.ones((8, cfg.tb), f32)
        rm128[:, ::128] = 0.0
        shared["rmask128"] = rm128
        shared["gla_gate_w"] = np.asarray(inp["gla_gate_w"], f32)
        gb_ = np.asarray(inp["gla_gate_b"], f32)
        shared["gla_gate_bT"] = np.ascontiguousarray(gb_.reshape(no, 2, 4, 64).transpose(0, 3, 1, 2).reshape(no, 64, 8))
        shared["gla_norm_gT"] = np.ascontiguousarray(np.asarray(inp["gla_norm_g"], f32)[:, :, None])
    if no:
        shared["od_w_in"] = od_ext
        shared["od_w_out"] = np.asarray(inp["od_w_out"], f32)
    maps = []
    for ci in range(ncores):
        bs = range(ci * cfg.nb, (ci + 1) * cfg.nb)
        h0 = np.concatenate([np.concatenate([ctx[b], x[b]], axis=0) for b in bs], axis=0)
        cv = np.stack([c[b] for b in bs] + [c_ctx], axis=0)
        cT = np.ascontiguousarray(cv.T.reshape(KC, 128, cfg.R).transpose(1, 0, 2).reshape(128, KC * cfg.R))
        m = dict(shared)
        m["h0"] = np.ascontiguousarray(h0)
        m["cT"] = cT
        maps.append(m)
    return maps


def nat_slice(cfg, s0, s1, d):
    if d == 0:
        return slice(s0, s1), False
    if s1 <= cfg.ctx:
        return slice(cfg.ctx - s1, cfg.ctx - s0), True
    assert s0 >= cfg.ctx
    return slice(cfg.tb - (s1 - cfg.ctx), cfg.tb - (s0 - cfg.ctx)), True


def nat_ap(ap, cfg, s0, s1, d, rows=None):
    sl, rev = nat_slice(cfg, s0, s1, d)
    a = ap[:, sl] if rows is None else ap[rows, sl]
    return a[:, ::-1] if rev else a


def stage_vscan(p, cfg, G, io, l, kind):
    Tb, CTX = cfg.tb, cfg.ctx
    j = l // 2
    hgrn = kind == "hgrn"
    dk = 128 if hgrn else 64
    dv = 128
    NH = 4
    YT = G["YT"]
    p.open_scope()
    segs = [(0, CTX), (CTX, Tb)]
    identb = G["ident_bf"]
    cmask = G["cmask"]
    onesm = G["ones_mean"]
    pTrK = p.psum([128, 1024], BF16, "pTrK")
    pTrV = p.psum([128, 1024], BF16, "pTrV")
    pAtt = [p.psum([128, 512], F32, f"pAtt{i}") for i in range(1)]
    pO = [p.psum([128, 512], F32, f"pO{i}") for i in range(2)]
    pKV = [p.psum([128, 512], F32, f"pKV{i}") for i in range(2)]
    pX = p.psum([128, 512], F32, "pX")
    if hgrn:
        ne = (cfg.depth + 1) // 2
        lg = p.sbuf([128, 2, ne, NH], F32, "lb_logits")
        p.load(lg, lg.ap[:, :, :, :].rearrange("p a b c -> p (a b c)"), io["hg_lbT"], io["hg_lbT"].ap[:, :])
        p.act(lg, lg[:, :, :, :], lg, lg[:, :, :, :], AF.Exp)
        ssum = p.sbuf([128, 2, NH], F32, "lb_sum")
        p.copy(ssum, ssum[:, :, :], lg, lg[:, :, 0, :])
        for i in range(1, ne):
            p.tt(ssum, ssum[:, :, :], ssum, ssum[:, :, :], lg, lg[:, :, i, :], ALU.add)
        ssf = ssum.ap[:, :, :].rearrange("p a b -> p (a b)")
        p.op("dve", lambda e: e.reciprocal(ssf, ssf), reads=[ssum], writes=[ssum])
        lb = p.sbuf([128, 2, NH], F32, "lb")
        p.memset(lb, lb[:, :, :], 0.0)
        for i in range(1, j + 1):
            p.tt(lb, lb[:, :, :], lb, lb[:, :, :], lg, lg[:, :, i, :], ALU.add)
        p.tt(lb, lb[:, :, :], lb, lb[:, :, :], ssum, ssum[:, :, :], ALU.mult)
        oml = p.sbuf([128, 2, NH], F32, "oml")
        p.ts(oml, oml[:, :, :], lb, lb[:, :, :], -1.0, 1.0, ALU.mult, ALU.add)
        noml = p.sbuf([128, 2, NH], F32, "noml")
        p.ts(noml, noml[:, :, :], oml, oml[:, :, :], -1.0, None, ALU.mult)
        lbf = p.sbuf([128, 2, NH], F32, "lbf")
        p.ts(lbf, lbf[:, :, :], lb, lb[:, :, :], float(LB_FLOOR), None, ALU.max)
        gn = p.sbuf([128, 1], F32, "gn")
        p.load(gn, gn[:, :], io["hg_norm_gT"], io["hg_norm_gT"].ap[j])
        rows = dict(q=EV["q"], k=None, v=EV["i"], g=EV["g"], z=(EV["zf"], EV["zb"]))
        qscale = 128 ** -0.5
    else:
        gw = p.sbuf([16, 2, 256], F32, "gw")
        p.load(gw, gw[:, :, :], io["gla_gate_w"], io["gla_gate_w"].ap[j].rearrange("d r c -> r d c"))
        gw_bf = p.sbuf([16, 2, 256], BF16, "gw_bf")
        p.copy(gw_bf, gw_bf[:, :, :], gw, gw[:, :, :])
        gbT = p.sbuf([64, 2 * NH], F32, "gbT")
        p.load(gbT, gbT[:, :], io["gla_gate_bT"], io["gla_gate_bT"].ap[j])
        gn = p.sbuf([128, 1], F32, "gn")
        p.load(gn, gn[:, :], io["gla_norm_gT"], io["gla_norm_gT"].ap[j])
        rows = dict(q=OD["q"], k=OD["k"], v=OD["v"], g=OD["g"], z=(OD["lrf"], OD["lrb"]))
        qscale = 64 ** -0.5
    q_raw = p.sbuf([dk, Tb], F32, "q_raw")
    k_raw = None if hgrn else p.sbuf([dk, Tb], F32, "k_raw")
    v_raw = p.sbuf([dv, Tb], F32, "v_raw")
    g_raw = p.sbuf([dv, Tb], F32, "g_raw")
    z_raw = [p.sbuf([dk if hgrn else 16, Tb], F32, f"z_raw{i}") for i in range(2)]
    A1 = p.sbuf([dk, Tb], F32, "A1")
    A2 = p.sbuf([dk, Tb], F32, "A2")
    A3 = p.sbuf([dk, Tb], F32, "A3")
    A4 = p.sbuf([dk, Tb], F32, "A4")
    oacc = p.sbuf([dv, Tb], F32, "oacc")
    Qh = p.sbuf([dk, Tb], BF16, "Qh")
    Kh = p.sbuf([dk, Tb], BF16, "Kh")
    vb = p.sbuf([dv, Tb], BF16, "vb")
    lr_bf = None if hgrn else p.sbuf([16, Tb], BF16, "lr_bf")
    outb = p.sbuf([dv, Tb], BF16, "outb")
    Ktok = [p.sbuf([128, dk], BF16, f"Ktok{i}") for i in range(2)]
    Vtok = [p.sbuf([128, dv], BF16, f"Vtok{i}") for i in range(2)]
    Ktok3 = [p.sbuf([128, dk], BF16, f"Ktok3{i}") for i in range(2)]
    att_sb = [p.sbuf([128, 128], BF16, f"att{i}") for i in range(2)]
    S = p.sbuf([dk, dv], F32, "S")
    Stmp = p.sbuf([dk, dv], F32, "Stmp")
    Sbf = [p.sbuf([dk, dv], BF16, f"Sbf{i}") for i in range(2)]
    rmask = p.sbuf([128, Tb], F32, "rmask")
    p.load(rmask, rmask[:, :], io["rmask"], io["rmask"].ap[:, :])
    tabgen = peer_tables_gen(p, cfg, G, io, l)
    n_tile_iters = cfg.nb * NH * 2 * (Tb // 128)
    tab_per_iter = -(-(PEER_NKEYS * PEER_NKEYS // 128) // n_tile_iters)
    import os
    VCUT = int(os.environ.get("VCUT", "0"))
    if VCUT == 1:
        p.close_scope(); return
    nt = Tb // 128
    NCK = 128 // CH
    for b in range(cfg.nb):
        c0 = b * Tb
        for h in range(NH):
            p.load(q_raw, q_raw[:, :], YT, YT.ap[rows["q"] + h * dk:rows["q"] + (h + 1) * dk, c0:c0 + Tb], q="sp")
            if not hgrn:
                p.load(k_raw, k_raw[:, :], YT, YT.ap[rows["k"] + h * dk:rows["k"] + (h + 1) * dk, c0:c0 + Tb], q="act")
            p.load(v_raw, v_raw[:, :], YT, YT.ap[rows["v"] + h * dv:rows["v"] + (h + 1) * dv, c0:c0 + Tb], q="act")
            p.load(g_raw, g_raw[:, :], YT, YT.ap[rows["g"] + h * dv:rows["g"] + (h + 1) * dv, c0:c0 + Tb], q="sp")
            for d in range(2):
                zr = z_raw[d]
                if hgrn:
                    p.load(zr, zr[:, :], YT, YT.ap[rows["z"][d] + h * dk:rows["z"][d] + (h + 1) * dk, c0:c0 + Tb], q="sp")
                else:
                    p.load(zr, zr[:, :], YT, YT.ap[rows["z"][d]:rows["z"][d] + 16, c0:c0 + Tb], q="sp")
                if VCUT == 2:
                    p.close_scope(); return
                if hgrn:
                    for (s0, s1) in segs:
                        p.act(A1, A1[:, s0:s1], zr, nat_ap(zr.ap, cfg, s0, s1, d), AF.Sigmoid)
                    p.ts(A2, A2[:, :], A1, A1[:, :], oml[:, d, h:h + 1], lbf[:, d, h:h + 1], ALU.mult, ALU.add,
                         reads=[oml, lbf])
                    p.ts(A4, A4[:, :], A1, A1[:, :], noml[:, d, h:h + 1], oml[:, d, h:h + 1], ALU.mult, ALU.add,
                         reads=[oml, noml], eng="pool")
                    p.act(A2, A2[:, :], A2, A2[:, :], AF.Ln)
                    esc = 1.0
                else:
                    for (s0, s1) in segs:
                        p.copy(lr_bf, lr_bf[:, s0:s1], zr, nat_ap(zr.ap, cfg, s0, s1, d), eng="pool")
                    for (t0, t1) in blocks(Tb, 512):
                        p.mm(pX, pX[0:dk, 0:t1 - t0], gw_bf, gw_bf[:, d, h * dk:(h + 1) * dk], lr_bf, lr_bf[:, t0:t1])
                        p.act(A2, A2[:, t0:t1], pX, pX[0:dk, 0:t1 - t0], AF.Sigmoid,
                              bias=gbT[:, d * NH + h:d * NH + h + 1], reads=[gbT])
                    p.act(A2, A2[:, :], A2, A2[:, :], AF.Ln)
                    esc = 1.0 / 16.0
                p.op("dve", lambda e: e.tensor_tensor_scan(A3[:, :], rmask[0:dk, :], A2[:, :], 0.0, ALU.mult, ALU.add),
                     reads=[rmask, A2], writes=[A3])
                p.act(A1, A1[:, :], A3, A3[:, :], AF.Exp, scale=-esc)
                p.act(A3, A3[:, :], A3, A3[:, :], AF.Exp, scale=esc)
                if hgrn:
                    for (s0, s1) in segs:
                        p.act(A2, A2[:, s0:s1], q_raw, nat_ap(q_raw.ap, cfg, s0, s1, d), AF.Silu)
                    p.stt(Qh, Qh[:, :], A2, A2[:, :], float(qscale), A3, A3[:, :], ALU.mult, ALU.mult)
                    p.tt(Kh, Kh[:, :], A4, A4[:, :], A1, A1[:, :], ALU.mult, eng="pool")
                else:
                    for (s0, s1) in segs:
                        p.stt(Qh, Qh[:, s0:s1], q_raw, nat_ap(q_raw.ap, cfg, s0, s1, d), float(qscale),
                              A3, A3[:, s0:s1], ALU.mult, ALU.mult)
                        p.tt(Kh, Kh[:, s0:s1], k_raw, nat_ap(k_raw.ap, cfg, s0, s1, d), A1, A1[:, s0:s1], ALU.mult)
                for (s0, s1) in segs:
                    p.copy(vb, vb[:, s0:s1], v_raw, nat_ap(v_raw.ap, cfg, s0, s1, d), eng="pool")
                if VCUT == 3:
                    p.close_scope(); return
                p.memset(S, S[:, :], 0.0)
                p.memset(Sbf[0], Sbf[0][:, :], 0.0)
                cur = 0
                for ti in range(nt):
                    cs = slice(ti * 128, (ti + 1) * 128)
                    kt, vt = Ktok[ti % 2], Vtok[ti % 2]
                    kt3 = Ktok3[ti % 2]
                    p.transpose(pTrK, pTrK[:, 0:dk], Kh, Kh[:, cs], identb, identb[0:dk, 0:dk])
                    p.transpose(pTrV, pTrV[:, 0:dv], vb, vb[:, cs], identb, identb[:, :])
                    p.copy(kt, kt[:, :], pTrK, pTrK[:, 0:dk], eng="act")
                    p.copy(vt, vt[:, :], pTrV, pTrV[:, 0:dv], eng="dve")
                    p.ts(kt3, kt3[:, :], kt, kt[:, :], G["rm3"][:, 0:1], None, ALU.mult, reads=[G["rm3"]], eng="pool")
                    if VCUT == 4:
                        p.close_scope(); return
                    pa = pAtt[0]
                    p.mm(pa, pa[:, 0:128], Kh, Kh[:, cs], Qh, Qh[:, cs])
                    at = att_sb[ti % 2]
                    p.tt(at, at[:, :], pa, pa[:, 0:128], cmask, cmask[:, :], ALU.mult)
                    if VCUT == 5:
                        p.close_scope(); return
                    po = pO[ti % 2]
                    p.mm(po, po[0:dv, 0:128], vt, vt[:, :], at, at[:, :], start=True, stop=False)
                    for c in range(NCK):
                        cc = slice(ti * 128 + c * CH, ti * 128 + (c + 1) * CH)
                        p.mm(po, po[0:dv, c * CH:(c + 1) * CH], Sbf[cur], Sbf[cur][:, :], Qh, Qh[:, cc],
                             start=False, stop=(c == NCK - 1))
                        pk = pKV[c % 2]
                        if c * CH < 96:
                            p.mm(pk, pk[0:dk, 0:dv], kt, kt[c * CH:(c + 1) * CH, :], vt, vt[c * CH:(c + 1) * CH, :])
                        else:
                            p.mm(pk, pk[0:dk, 0:dv], kt3, kt3[64:128, :], vt, vt[64:128, :])
                        p.tt(Stmp, Stmp[:, :], pk, pk[0:dk, 0:dv], S, S[:, :], ALU.add)
                        dcol = A3[:, ti * 128 + (c + 1) * CH - 1:ti * 128 + (c + 1) * CH]
                        p.ts(S, S[:, :], Stmp, Stmp[:, :], dcol, None, ALU.mult, reads=[A3])
                        nxt = 1 - cur
                        p.act(Sbf[nxt], Sbf[nxt][:, :], Stmp, Stmp[:, :], AF.Identity, scale=dcol, reads=[A3])
                        cur = nxt
                    if VCUT == 6:
                        p.close_scope(); return
                    tabgen = drain_gen(tabgen, tab_per_iter)
                    sl, rev = nat_slice(cfg, ti * 128, (ti + 1) * 128, d)
                    if d == 0:
                        p.copy(oacc, oacc[:, sl], po, po[0:dv, 0:128], eng="act")
                    else:
                        oa = oacc.ap[:, sl][:, ::-1]
                        p.tt(oacc, oa, oacc, oa, po, po[0:dv, 0:128], ALU.add)
            if VCUT == 7:
                p.close_scope(); return
            if hgrn:
                p.act(g_raw, g_raw[:, :], g_raw, g_raw[:, :], AF.Sigmoid)
                p.tt(oacc, oacc[:, :], oacc, oacc[:, :], g_raw, g_raw[:, :], ALU.mult)
            else:
                p.act(g_raw, g_raw[:, :], g_raw, g_raw[:, :], AF.Silu)
            p.act(v_raw, v_raw[:, :], oacc, oacc[:, :], AF.Square)
            for (t0, t1) in blocks(Tb, 512):
                p.mm(pX, pX[:, 0:t1 - t0], onesm, onesm[:, :], v_raw, v_raw[:, t0:t1])
                p.act(v_raw, v_raw[:, t0:t1], pX, pX[:, 0:t1 - t0], AF.Ln, bias=float(NORM_EPS))
            p.act(v_raw, v_raw[:, :], v_raw, v_raw[:, :], AF.Exp, scale=-0.5)
            if hgrn:
                p.stt(outb, outb[:, :], oacc, oacc[:, :], gn[:, 0:1], v_raw, v_raw[:, :], ALU.mult, ALU.mult, reads=[gn])
            else:
                p.stt(oacc, oacc[:, :], oacc, oacc[:, :], gn[:, 0:1], v_raw, v_raw[:, :], ALU.mult, ALU.mult, reads=[gn])
                p.tt(outb, outb[:, :], oacc, oacc[:, :], g_raw, g_raw[:, :], ALU.mult)
            p.store(G["MIXT"], G["MIXT"].ap[h * dv:(h + 1) * dv, c0:c0 + Tb], outb, outb[:, :], q="pool")
    drain_gen(tabgen)
    p.close_scope()


def stage_attn(p, cfg, G, io, l):
    Tb, CTX, SEQ = cfg.tb, cfg.ctx, cfg.seq
    j = l // 2
    YT = G["YT"]
    p.open_scope()
    identb = G["ident_bf"]
    onesm = G["ones_mean"]
    cosT = p.sbuf([128, Tb], F32, "cosT")
    sinT = p.sbuf([128, Tb], F32, "sinT")
    p.load(cosT, cosT[:, :], io["cosT"], io["cosT"].ap[:, :])
    p.load(sinT, sinT[:, :], io["sinT"], io["sinT"].ap[:, :])
    onesb = p.sbuf([128, 128], BF16, "onesb")
    p.memset(onesb, onesb[:, :], 1.0)
    gq = p.sbuf([128, 4], F32, "gq")
    p.load(gq, gq[:, :], io["at_normT"], io["at_normT"].ap[j])
    gab = p.sbuf([128, 2], F32, "gab")
    p.act(gab, gab[:, 0:1], gq, gq[:, 0:1], AF.Abs)
    p.act(gab, gab[:, 1:2], gq, gq[:, 2:3], AF.Abs)
    pS = [p.psum([128, 512], F32, f"pS{i}") for i in range(3)]
    pOa = p.psum([128, 512], F32, "pOa")
    pSum = p.psum([128, 512], F32, "pSum")
    pM = p.psum([128, 512], F32, "pM")
    trs = [p.psum([128, 4, 256], BF16, f"pTrA{s}") for s in range(2)]
    ident = G["ident"]
    p.transpose(pM, pM[0:2, 0:128], gab, gab[:, 0:2], ident, ident[:, :])
    gmx = p.sbuf([2, 2], F32, "gmx")
    p.op("dve", lambda e: e.reduce_max(gmx[0:2, 0:1], pM[0:2, 0:128], axis=AX.X), reads=[pM], writes=[gmx])
    gmx2 = p.sbuf([2, 2], F32, "gmx2")
    p.memset(gmx2, gmx2[:, :], 0.0)
    p.tt(gmx2, gmx2[0:2, 0:2], ident, ident[0:2, 0:2], gmx, gmx.ap[0:2, 0:1].broadcast_to([2, 2]), ALU.mult)
    ones2 = p.sbuf([2, 128], F32, "ones2")
    p.memset(ones2, ones2[:, :], 1.0)
    p.mm(pM, pM[:, 0:2], ones2, ones2[:, :], gmx2, gmx2[:, :])
    nshift = p.sbuf([128, 1], F32, "nshift")
    p.copy(gab, gab[:, 0:2], pM, pM[:, 0:2])
    p.stt(nshift, nshift[:, 0:1], gab, gab[:, 0:1], float(-(128 ** 0.5)), gab, gab[:, 1:2], ALU.mult, ALU.mult)
    x_raw = p.sbuf([128, Tb], F32, "x_raw")
    xs_raw = p.sbuf([128, Tb], F32, "xs_raw")
    t1 = p.sbuf([128, Tb], F32, "t1")
    t2 = p.sbuf([128, Tb], F32, "t2")
    qr = p.sbuf([128, 4, Tb], BF16, "qr")
    kr = p.sbuf([128, 2, Tb], BF16, "kr")
    vbf = p.sbuf([128, Tb], BF16, "vbf")
    nkt = Tb // 128
    Vtok = p.sbuf([128, 2, nkt, 128], BF16, "Vtok")
    Pt = [p.sbuf([128, 512], BF16, f"Pt{i}") for i in range(3)]
    osb = p.sbuf([128, 512], F32, "osb")
    rs = p.sbuf([128, 512], F32, "rs")
    obf = [p.sbuf([128, 512], BF16, f"obf{i}") for i in range(2)]
    scale = 128 ** -0.5
    for b in range(cfg.nb):
        c0 = b * Tb
        heads = [("q", h, EV["aq"] + h * 128, EV["aqs"] + h * 128, 0) for h in range(4)] + \
                [("k", g, EV["ak"] + g * 128, EV["aks"] + g * 128, 2) for g in range(2)]
        for (kind, hi, r0, rs0, gc) in heads:
            p.load(x_raw, x_raw[:, :], YT, YT.ap[r0:r0 + 128, c0:c0 + Tb], q="sp")
            p.load(xs_raw, xs_raw[:, :], YT, YT.ap[rs0:rs0 + 128, c0:c0 + Tb], q="act")
            p.act(t1, t1[:, :], x_raw, x_raw[:, :], AF.Square)
            for (a0, a1) in blocks(Tb, 512):
                p.mm(pM, pM[:, 0:a1 - a0], onesm, onesm[:, :], t1, t1[:, a0:a1])
                p.act(t2, t2[:, a0:a1], pM, pM[:, 0:a1 - a0], AF.Ln, bias=float(NORM_EPS))
            p.act(t2, t2[:, :], t2, t2[:, :], AF.Exp, scale=-0.5)
            p.stt(t1, t1[:, :], x_raw, x_raw[:, :], gq[:, gc:gc + 1], cosT, cosT[:, :], ALU.mult, ALU.mult, reads=[gq])
            p.stt(x_raw, x_raw[:, :], xs_raw, xs_raw[:, :], gq[:, gc + 1:gc + 2], sinT, sinT[:, :], ALU.mult, ALU.mult,
                  reads=[gq])
            p.tt(t1, t1[:, :], t1, t1[:, :], x_raw, x_raw[:, :], ALU.add, eng="pool")
            dst = qr if kind == "q" else kr
            p.tt(dst, dst[:, hi, :], t1, t1[:, :], t2, t2[:, :], ALU.mult)
        for g in range(2):
            p.load(x_raw, x_raw[:, :], YT, YT.ap[EV["av"] + g * 128:EV["av"] + (g + 1) * 128, c0:c0 + Tb], q="sp")
            p.copy(vbf, vbf[:, :], x_raw, x_raw[:, :], eng="pool")
            for kt in range(nkt):
                tr = trs[kt % 2]
                p.transpose(tr, tr[:, 0, 0:128], vbf, vbf[:, kt * 128:(kt + 1) * 128], identb, identb[:, :])
                p.copy(Vtok, Vtok[:, g, kt, :], tr, tr[:, 0, 0:128], eng="act" if kt % 2 else "dve")
        it = 0
        for h in range(4):
            g = h // 2
            qblocks = [(a0, a1, 0, CTX // 128) for (a0, a1) in blocks(CTX, 512)] + \
                      [(CTX + a0, CTX + a1, 0, nkt) for (a0, a1) in blocks(SEQ, 512)]
            for (q0, q1, k0, k1) in qblocks:
                w = q1 - q0
                for kt in range(k0, k1):
                    ps = pS[it % 3]
                    pt = Pt[it % 3]
                    it += 1
                    p.mm(ps, ps[:, 0:w], kr, kr[:, g, kt * 128:(kt + 1) * 128], qr, qr[:, h, q0:q1])
                    p.act(pt, pt[:, 0:w], ps, ps[:, 0:w], AF.Exp, bias=nshift[:, 0:1], scale=float(scale), reads=[nshift])
                    p.mm(pOa, pOa[:, 0:w], Vtok, Vtok[:, g, kt, :], pt, pt[:, 0:w], start=(kt == k0), stop=(kt == k1 - 1))
                    p.mm(pSum, pSum[:, 0:w], onesb, onesb[:, :], pt, pt[:, 0:w], start=(kt == k0), stop=(kt == k1 - 1))
                p.op("dve", lambda e, w=w: e.reciprocal(rs[:, 0:w], pSum[:, 0:w]), reads=[pSum], writes=[rs])
                ob = obf[(it // 1) % 2]
                p.tt(ob, ob[:, 0:w], pOa, pOa[:, 0:w], rs, rs[:, 0:w], ALU.mult)
                p.store(G["MIXT"], G["MIXT"].ap[512 + h * 128:512 + (h + 1) * 128, c0 + q0:c0 + q1], ob, ob[:, 0:w], q="pool")
    p.close_scope()


NEG = -1.0e30

def peer_tables_gen(p, cfg, G, io, l):
    NE = PEER_NKEYS * PEER_NKEYS
    su = [p.sbuf([128, D], F32, f"tsu{i}") for i in range(2)]
    sv = [p.sbuf([128, D], F32, f"tsv{i}") for i in range(2)]
    ob = [p.sbuf([128, 2 * D], BF16, f"tob{i}") for i in range(2)]
    u_tab = io[f"peer_u{l}"].ap
    v_tab = io[f"peer_v{l}"].ap
    for rb in range(NE // 128):
        a, b_, o = su[rb % 2], sv[rb % 2], ob[rb % 2]
        p.load(a, a[:, :], G["ext"], u_tab[rb * 128:(rb + 1) * 128, :], q="sp")
        p.load(b_, b_[:, :], G["ext"], v_tab[rb * 128:(rb + 1) * 128, :], q="act")
        p.copy(o, o[:, 0:D], a, a[:, :], eng="act")
        p.copy(o, o[:, D:2 * D], b_, b_[:, :], eng="dve" if rb % 2 else "pool")
        p.store(G["UVB"], G["UVB"].ap[rb * 128:(rb + 1) * 128, :], o, o[:, :], q="sp")
        yield


def drain_gen(g, n=None):
    if g is None:
        return None
    try:
        if n is None:
            while True:
                next(g)
        for _ in range(n):
            next(g)
    except StopIteration:
        return None
    return g


def stage_peer(p, cfg, G, io, l):
    import os
    T = cfg.T
    H8, NK, TK = PEER_HEADS, PEER_NKEYS, PEER_TOPK
    NQ = H8 * 2 * NK
    p.open_scope()
    ident = G["ident"]
    wq_bf = p.sbuf([128, KC, NQ], BF16, "wq_bf")
    p.open_scope()
    wst = [p.sbuf([128, NQ], F32, f"wqst{i}") for i in range(2)]
    for kc in range(KC):
        st = wst[kc % 2]
        p.load(st, st[:, :], G["ext"], io["peer_wq"].ap[l, kc * 128:(kc + 1) * 128, :], q="sp" if kc % 2 else "act")
        p.copy(wq_bf, wq_bf[:, kc, :], st, st[:, :], eng="pool" if kc % 2 else "dve")
    sk = p.sbuf([128, 16 * NK], F32, "sk")
    p.load(sk, sk[:, :], G["ext"], io["peer_subkT"].ap[l])
    p.close_scope()
    sk_bf = p.sbuf([128, 16, NK], BF16, "sk_bf")
    p.open_scope()
    sk2 = p.sbuf([128, 16 * NK], F32, "sk2")
    p.load(sk2, sk2[:, :], G["ext"], io["peer_subkT"].ap[l])
    p.copy(sk_bf, sk_bf.ap[:, :, :].rearrange("p a b -> p (a b)"), sk2, sk2[:, :])
    p.close_scope()
    scb, shb = [], []
    for r in range(cfg.R):
        a = p.sbuf([128, D], F32, f"sc2b{r}")
        b_ = p.sbuf([128, D], F32, f"sh2b{r}")
        p.load(a, a[:, :], G["modtok"], G["modtok"].ap[l, r:r + 1, 4 * D:5 * D].partition_broadcast(128))
        p.load(b_, b_[:, :], G["modtok"], G["modtok"].ap[l, r:r + 1, 3 * D:4 * D].partition_broadcast(128))
        p.ts(a, a[:, :], a, a[:, :], 1.0, None, ALU.add)
        scb.append(a)
        shb.append(b_)
    TB = 2
    hst = [p.sbuf([128, D], F32, f"phst{i}") for i in range(2)]
    hm = p.sbuf([128, 4, D], F32, "hm_tok")
    hmT = p.sbuf([128, KC, TB * 128], BF16, "hmT")
    qT = p.sbuf([128, 16, TB * 128], BF16, "qT")
    pT = [p.psum([128, 4, 128], F32, "ppT")] * 2
    pQ = [p.psum([128, 512], F32, "ppQ")] * 2
    pS = [p.psum([128, 4, 128], F32, f"ppS{i}") for i in range(2)] * 2
    pHM = p.psum([128, 1024], F32, "pHM")
    pACC = [p.psum([128, 512], F32, f"pACC{i}") for i in range(2)]
    s_sb = p.sbuf([128, 16, NK], F32, "s_sb")
    s_wk = p.sbuf([128, NK], F32, "s_wk")
    ts16 = p.sbuf([128, 16, TK], F32, "ts16")
    ti16 = p.sbuf([128, 16, TK], U32, "ti16")
    ti16f = p.sbuf([128, 16, TK], F32, "ti16f")
    cand = p.sbuf([128, H8, TK * TK], F32, "cand")
    cwk = p.sbuf([128, TK * TK], F32, "cwk")
    eid = p.sbuf([128, H8, TK * TK], F32, "eid")
    best = p.sbuf([128, H8, TK], F32, "best")
    gate = p.sbuf([128, H8, TK], F32, "gate")
    gsum = p.sbuf([128, H8], F32, "gsum")
    idxf = p.sbuf([128, H8 * TK], F32, "idxf")
    idxu = p.sbuf([128, H8 * TK], U32, "idxu")
    aval = p.sbuf([128, H8 * TK], F32, "aval")
    coef = p.sbuf([128, H8 * TK], F32, "coef")
    tmpa = p.sbuf([128, H8 * TK], F32, "tmpa")
    junk = p.sbuf([128, D], BF16, "junk")
    NUB = int(os.environ.get("NUB", "10"))
    GRP = 4
    ub = [p.sbuf([128, 2 * D], BF16, f"ub{i}") for i in range(NUB)]
    dgs = [p.sbuf([128, 128], BF16, f"dg{i}") for i in range(8)]
    osb = p.sbuf([128, D], F32, "posb")
    identb = G["ident_bf"]
    UVB = G["UVB"]
    nslot = H8 * TK
    ntiles = T // 128
    idxus = [idxu, p.sbuf([128, H8 * TK], U32, "idxu_b")]
    gates = [gate, p.sbuf([128, H8, TK], F32, "gate_b")]
    posu = p.sbuf([128, H8, TK], U32, "posu")
    abu = p.sbuf([128, 2, H8 * TK], U32, "abu")
    abf = p.sbuf([128, 2, H8 * TK], F32, "abf")
    ijf = p.sbuf([128, 2, H8 * TK], F32, "ijf")
    iota16 = p.sbuf([128, TK], F32, "iota16")
    p.load(iota16, iota16[:, :], G["ext"], io["iota16"].ap[:, :])

    def producer(ti):
        t0 = (ti // TB) * TB * 128
        t1 = min(t0 + TB * 128, T)
        ntl = (t1 - t0) // 128
        w = t1 - t0
        tl = ti % TB
        if tl == 0:
            for tl2 in range(ntl):
                tj = ti + tl2
                r = cfg.seg_of_tile(tj)
                hs = hst[tj % 2]
                slot = tj % 4
                p.load(hs, hs[:, :], G["H"], G["H"].ap[tj * 128:(tj + 1) * 128, :], q="sp")
                p.tt(hm, hm[:, slot, :], hs, hs[:, :], scb[r], scb[r][:, :], ALU.mult)
                p.tt(hm, hm[:, slot, :], hm, hm[:, slot, :], shb[r], shb[r][:, :], ALU.add)
                yield
                for half in range(2):
                    pt = pT[half]
                    for kk in range(4):
                        kc = half * 4 + kk
                        p.transpose(pt, pt[:, kk, :], hm, hm[:, slot, kc * 128:(kc + 1) * 128], ident, ident[:, :])
                    p.copy(hmT, hmT[:, half * 4:(half + 1) * 4, tl2 * 128:(tl2 + 1) * 128], pt, pt[:, :, :],
                           eng="act" if half else "dve")
                    yield
            for hsx in range(16):
                pq = pQ[hsx % 2]
                for kc in range(KC):
                    p.mm(pq, pq[:, 0:w], wq_bf, wq_bf[:, kc, hsx * NK:(hsx + 1) * NK], hmT, hmT[:, kc, 0:w],
                         start=(kc == 0), stop=(kc == KC - 1))
                p.copy(qT, qT[:, hsx, 0:w], pq, pq[:, 0:w], eng="act" if hsx % 2 else "dve")
                yield
        idxu_, gate_ = idxus[ti % 2], gates[ti % 2]
        for q4 in range(4):
            ps = pS[q4]
            for k4 in range(4):
                hsx = q4 * 4 + k4
                p.mm(ps, ps[:, k4, :], qT, qT[:, hsx, tl * 128:(tl + 1) * 128], sk_bf, sk_bf[:, hsx, :])
            p.copy(s_sb, s_sb[:, q4 * 4:(q4 + 1) * 4, :], ps, ps[:, :, :], eng="act" if q4 % 2 else "dve")
            yield
        for hsx in range(16):
            src_ = s_sb[:, hsx, :]
            p.op("dve", lambda e, hsx=hsx, src_=src_: e.max(out=ts16[:, hsx, 0:8], in_=src_), reads=[s_sb], writes=[ts16])
            p.op("dve", lambda e, hsx=hsx, src_=src_: e.max_index(ti16[:, hsx, 0:8], ts16[:, hsx, 0:8], src_),
                 reads=[s_sb, ts16], writes=[ti16])
            p.op("dve", lambda e, hsx=hsx, src_=src_: e.match_replace(out=s_wk[:, :], in_to_replace=ts16[:, hsx, 0:8],
                                                                      in_values=src_, imm_value=NEG),
                 reads=[s_sb, ts16], writes=[s_wk])
            p.op("dve", lambda e, hsx=hsx: e.max(out=ts16[:, hsx, 8:16], in_=s_wk[:, :]), reads=[s_wk], writes=[ts16])
            p.op("dve", lambda e, hsx=hsx: e.max_index(ti16[:, hsx, 8:16], ts16[:, hsx, 8:16], s_wk[:, :]),
                 reads=[s_wk, ts16], writes=[ti16])
            yield
        p.copy(ti16f, ti16f.ap[:, :, :].rearrange("p a b -> p (a b)"), ti16, ti16.ap[:, :, :].rearrange("p a b -> p (a b)"))
        ts4 = ts16.ap[:, :, :].rearrange("p (h s) k -> p h s k", s=2)
        ti4 = ti16f.ap[:, :, :].rearrange("p (h s) k -> p h s k", s=2)
        c4 = cand.ap[:, :, :].rearrange("p h (a b) -> p h a b", a=TK)
        e4 = eid.ap[:, :, :].rearrange("p h (a b) -> p h a b", a=TK)
        for h in range(H8):
            a_b = ts4[:, h, 0, :].unsqueeze(2).broadcast_to([128, TK, TK])
            b_b = ts4[:, h, 1, :].unsqueeze(1).broadcast_to([128, TK, TK])
            p.tt(cand, c4[:, h, :, :], ts16, a_b, ts16, b_b, ALU.add)
            yield
        for h in range(H8):
            src_ = cand[:, h, :]
            p.op("dve", lambda e, h=h, src_=src_: e.max(out=best[:, h, 0:8], in_=src_), reads=[cand], writes=[best])
            p.op("dve", lambda e, h=h, src_=src_: e.match_replace(out=cwk[:, :], in_to_replace=best[:, h, 0:8],
                                                                  in_values=src_, imm_value=NEG),
                 reads=[cand, best], writes=[cwk])
            p.op("dve", lambda e, h=h, src_=src_: e.max_index(posu[:, h, 0:8], best[:, h, 0:8], src_),
                 reads=[cand, best], writes=[posu])
            p.op("dve", lambda e, h=h: e.max(out=best[:, h, 8:16], in_=cwk[:, :]), reads=[cwk], writes=[best])
            p.op("dve", lambda e, h=h: e.max_index(posu[:, h, 8:16], best[:, h, 8:16], cwk[:, :]),
                 reads=[cwk, best], writes=[posu])
            yield
        mx_b = best.ap[:, :, 0:1].broadcast_to([128, H8, TK])
        p.tt(gate_, gate_[:, :, :], best, best[:, :, :], best, mx_b, ALU.subtract)
        p.act(gate_, gate_[:, :, :], gate_, gate_[:, :, :], AF.Exp)
        p.op("dve", lambda e: e.tensor_reduce(out=gsum[:, :], in_=gate_[:, :, :], axis=AX.X, op=ALU.add),
             reads=[gate_], writes=[gsum])
        p.op("dve", lambda e: e.reciprocal(gsum[:, :], gsum[:, :]), reads=[gsum], writes=[gsum])
        p.tt(gate_, gate_[:, :, :], gate_, gate_[:, :, :], gsum, gsum.ap[:, :].unsqueeze(2).broadcast_to([128, H8, TK]), ALU.mult)
        yield
        pflat = posu.ap[:, :, :].rearrange("p a b -> p (a b)")
        p.op("dve", lambda e: e.tensor_single_scalar(abu[:, 0, :], pflat, 4, op=ALU.logical_shift_right),
             reads=[posu], writes=[abu])
        p.op("dve", lambda e: e.tensor_single_scalar(abu[:, 1, :], pflat, 15, op=ALU.bitwise_and),
             reads=[posu], writes=[abu])
        p.copy(abf, abf.ap[:, :, :].rearrange("p a b -> p (a b)"), abu, abu.ap[:, :, :].rearrange("p a b -> p (a b)"))
        yield
        oh4 = eid.ap[:, :, :].rearrange("p h (k a) -> p h k a", k=TK)
        oh3 = eid.ap[:, :, :].rearrange("p h (k a) -> p (h k) a", k=TK)
        io16 = iota16.ap[:, :].unsqueeze(1).unsqueeze(1).broadcast_to([128, H8, TK, TK])
        for half in range(2):
            sel_ = abf.ap[:, half, :].rearrange("p (h k) -> p h k", h=H8).unsqueeze(3).broadcast_to([128, H8, TK, TK])
            tab_ = ti4[:, :, half, :].unsqueeze(2).broadcast_to([128, H8, TK, TK])
            p.tt(eid, oh4, abf, sel_, iota16, io16, ALU.is_equal)
            p.tt(eid, oh4, eid, oh4, ti16f, tab_, ALU.mult)
            yield
            p.op("dve", lambda e, half=half: e.tensor_reduce(out=ijf[:, half, :], in_=oh3, axis=AX.X, op=ALU.add),
                 reads=[eid], writes=[ijf])
            yield
        p.stt(idxf, idxf[:, :], ijf, ijf[:, 0, :], float(NK), ijf, ijf[:, 1, :], ALU.mult, ALU.add)
        p.ts(idxf, idxf[:, :], idxf, idxf[:, :], 0.0, float(NK * NK - 1), ALU.max, ALU.min)
        p.copy(idxu_, idxu_[:, :], idxf, idxf[:, :])
        yield

    def drain(g, n=None):
        if g is None:
            return None
        try:
            if n is None:
                while True:
                    next(g)
            for _ in range(n):
                next(g)
        except StopIteration:
            return None
        return g

    gi = 0
    drain(producer(0))
    for ti in range(ntiles):
        nxt = producer(ti + 1) if ti + 1 < ntiles else None
        idxu_, gate_ = idxus[ti % 2], gates[ti % 2]
        slot = ti % 4
        for hf in range(2):
            p.mm(pHM, pHM[:, hf * 512:(hf + 1) * 512], ident, ident[:, :], hm, hm[:, slot, hf * 512:(hf + 1) * 512])
        gflat = gate_.ap[:, :, :].rearrange("p a b -> p (a b)")
        for s0 in range(0, nslot, GRP):
            bufs = []
            for s in range(s0, s0 + GRP):
                u = ub[gi % NUB]
                gi += 1
                bufs.append(u)
                p.dma("pool", u, u[:, :], UVB, None, extra_reads=[idxu_],
                      fn=lambda e, u=u, s=s, idxu_=idxu_: e.indirect_dma_start(
                          out=u[:, :], out_offset=None, in_=UVB.ap,
                          in_offset=bass.IndirectOffsetOnAxis(ap=idxu_[:, s:s + 1], axis=0)))
                p.stt(junk, junk[:, :], u, u[:, 0:D], 1.0, pHM, pHM[:, :], ALU.mult, ALU.mult,
                      accum=aval[:, s:s + 1], accum_b=aval)
            sl = slice(s0, s0 + GRP)
            p.tt(tmpa, tmpa[:, sl], aval, aval[:, sl], aval, aval[:, sl], ALU.mult)
            p.ts(tmpa, tmpa[:, sl], tmpa, tmpa[:, sl], 0.044715, 1.0, ALU.mult, ALU.add)
            p.tt(tmpa, tmpa[:, sl], tmpa, tmpa[:, sl], aval, aval[:, sl], ALU.mult)
            p.act(tmpa, tmpa[:, sl], tmpa, tmpa[:, sl], AF.Sigmoid, scale=float(2.0 * (2.0 / np.pi) ** 0.5))
            p.tt(tmpa, tmpa[:, sl], tmpa, tmpa[:, sl], aval, aval[:, sl], ALU.mult)
            p.tt(coef, coef[:, sl], tmpa, tmpa[:, sl], gate_, gflat[:, sl], ALU.mult)
            for k_, s in enumerate(range(s0, s0 + GRP)):
                u = bufs[k_]
                dg = dgs[s % 8]
                p.act(dg, dg[:, :], identb, identb[:, :], AF.Identity, scale=coef[:, s:s + 1], reads=[coef])
                for hf in range(2):
                    p.mm(pACC[hf], pACC[hf][:, :], dg, dg[:, :], u, u[:, D + hf * 512:D + (hf + 1) * 512],
                         start=(s == 0), stop=(s == nslot - 1))
            nxt = drain(nxt, 4)
        drain(nxt)
        p.copy(osb, osb[:, 0:512], pACC[0], pACC[0][:, :], eng="act")
        p.copy(osb, osb[:, 512:1024], pACC[1], pACC[1][:, :], eng="dve")
        p.store(G["PEERO"], G["PEERO"].ap[ti * 128:(ti + 1) * 128, :], osb, osb[:, :], q="sp")
    p.close_scope()


def stage_ssd(p, cfg, G, io, l):
    Tb, CTX = cfg.tb, cfg.ctx
    j = l // 2
    YT = G["YT"]
    XSF = G["XSF"]
    YPRE = G["YPRE"]
    nt = Tb // 128
    segs = [(0, CTX), (CTX, Tb)]
    p.open_scope()
    identb, ident = G["ident_bf"], G["ident"]
    cw = p.sbuf([128, 8, 5], F32, "convw")
    p.load(cw, cw.ap[:, :, :].rearrange("p a b -> p (a b)"), G["ext"], io["ssd_conv_wT"].ap[j])
    cb = p.sbuf([128, 8], F32, "convb")
    p.load(cb, cb[:, :], G["ext"], io["ssd_conv_bT"].ap[j])
    dtb = p.sbuf([8, 2], F32, "dtb")
    p.load(dtb, dtb[:, :], G["ext"], io["ssd_dt_biasT"].ap[j])
    nA = p.sbuf([8, 2], F32, "nA")
    p.load(nA, nA[:, :], G["ext"], io["ssd_a_logT"].ap[j])
    p.act(nA, nA[:, :], nA, nA[:, :], AF.Exp)
    p.ts(nA, nA[:, :], nA, nA[:, :], -1.0, None, ALU.mult)
    dsk = p.sbuf([128, 4], F32, "dskip")
    p.load(dsk, dsk[:, :], G["ext"], io["ssd_dT"].ap[j])
    gnm = p.sbuf([128, 4], F32, "ssd_g")
    p.load(gnm, gnm[:, :], G["ext"], io["ssd_norm_gT"].ap[j])
    sel = p.sbuf([8, 8, 128], F32, "sel")
    p.load(sel, sel.ap[:, :, :].rearrange("p a b -> p (a b)"), G["ext"], io["selT"].ap[:, :])
    mbias = p.sbuf([128, 128], F32, "mbias")
    p.load(mbias, mbias[:, :], G["ext"], io["mbias"].ap[:, :])
    rm128 = p.sbuf([8, Tb], F32, "rm128")
    p.load(rm128, rm128[:, :], G["ext"], io["rmask128"].ap[:, :])
    ones256 = p.sbuf([128, 128], F32, "ones256")
    p.memset(ones256, ones256[:, :], 1.0 / 256.0)
    pTrX = p.psum([128, 1024], BF16, "pTrX")
    pTrB = p.psum([128, 1024], BF16, "pTrB")
    pTrD = p.psum([128, 512], F32, "pTrD")
    pCR = p.psum([128, 2, 256], F32, "pCR")
    pSc = p.psum([128, 512], F32, "pSc")
    pY = p.psum([128, 512], F32, "pY")
    pKV = p.psum([128, 512], F32, "pKV")
    pN = p.psum([128, 512], F32, "pN")
    XCb = p.sbuf([128, 8, Tb], BF16, "XCb")
    raw = [p.sbuf([128, Tb], F32, f"sraw{i}") for i in range(2)]
    cacc = p.sbuf([128, Tb], F32, "cacc")
    dtT = [p.sbuf([8, Tb], F32, f"dtT{d}") for d in range(2)]
    cumT = [p.sbuf([8, Tb], F32, f"cumT{d}") for d in range(2)]
    t8a = p.sbuf([8, Tb], F32, "t8a")
    t8b = p.sbuf([8, Tb], F32, "t8b")
    Xd = p.sbuf([128, 3, Tb], BF16, "Xd")
    yacc = p.sbuf([128, Tb], F32, "yacc")
    xs_tok = [p.sbuf([128, 128], BF16, f"xs_tok{i}") for i in range(2)]
    B_tok = [p.sbuf([128, 128], BF16, f"B_tok{i}") for i in range(2)]
    dc_tok = [p.sbuf([128, 16], F32, f"dc_tok{i}") for i in range(2)]
    crow = [p.sbuf([128, 2, 128], F32, f"crow{i}") for i in range(2)]
    sc_sb = [p.sbuf([128, 128], F32, f"sc_sb{i}") for i in range(2)]
    D1 = [p.sbuf([128, 128], F32, f"D1{i}") for i in range(2)]
    attT = [p.sbuf([128, 128], BF16, f"attT{i}") for i in range(2)]
    ec = [p.sbuf([128, 128], F32, f"ec{i}") for i in range(2)]
    Chat = [p.sbuf([128, 128], BF16, f"Chat{i}") for i in range(2)]
    w2 = p.sbuf([128, 2], F32, "w2")
    el2 = p.sbuf([128, 2], F32, "el2")
    xhat = [p.sbuf([128, 2, 64], BF16, f"xhat{i}") for i in range(2)]
    S2 = p.sbuf([128, 2, 64], F32, "S2")
    Sbf = [p.sbuf([128, 2, 64], BF16, f"S2bf{i}") for i in range(2)]
    obf = p.sbuf([128, Tb], BF16, "ssd_obf")
    for b in range(cfg.nb):
        c0 = b * Tb
        for ch in range(8):
            r0 = OD["xs"] + ch * 128
            rw = raw[ch % 2]
            p.load(rw, rw[:, :], YT, YT.ap[r0:r0 + 128, c0:c0 + Tb], q="sp" if ch % 2 else "act")
            for (s0, s1) in segs:
                p.ts(cacc, cacc[:, s0:s1], rw, rw[:, s0:s1], cw[:, ch, 2:3], None, ALU.mult, reads=[cw])
                for k in (0, 1, 3, 4):
                    sh = k - 2
                    o0, o1 = max(s0, s0 - sh), min(s1, s1 - sh)
                    p.stt(cacc, cacc[:, o0:o1], rw, rw[:, o0 + sh:o1 + sh], cw[:, ch, k:k + 1], cacc, cacc[:, o0:o1],
                          ALU.mult, ALU.add, reads=[cw])
            if ch < 4:
                p.act(cacc, cacc[:, :], cacc, cacc[:, :], AF.Silu, bias=cb[:, ch:ch + 1], reads=[cb])
                p.copy(XCb, XCb[:, ch, :], cacc, cacc[:, :], eng="pool")
                p.store(XSF, XSF.ap[ch * 128:(ch + 1) * 128, c0:c0 + Tb], cacc, cacc[:, :], q="sp")
            else:
                p.act(XCb, XCb[:, ch, :], cacc, cacc[:, :], AF.Silu, bias=cb[:, ch:ch + 1], reads=[cb])
        for d in range(2):
            r0 = OD["dtf"] if d == 0 else OD["dtb"]
            p.load(t8a, t8a[:, :], YT, YT.ap[r0:r0 + 8, c0:c0 + Tb], q="sp")
            for (s0, s1) in segs:
                p.ts(t8b, t8b[:, s0:s1], t8a, nat_ap(t8a.ap, cfg, s0, s1, d), dtb[:, d:d + 1], None, ALU.add, reads=[dtb])
            p.act(t8a, t8a[:, :], t8b, t8b[:, :], AF.Abs)
            p.act(t8a, t8a[:, :], t8a, t8a[:, :], AF.Exp, scale=-1.0)
            p.act(t8a, t8a[:, :], t8a, t8a[:, :], AF.Ln, bias=1.0)
            p.ts(t8b, t8b[:, :], t8b, t8b[:, :], 0.0, None, ALU.max)
            p.tt(dtT[d], dtT[d][:, :], t8a, t8a[:, :], t8b, t8b[:, :], ALU.add)
            p.ts(t8a, t8a[:, :], dtT[d], dtT[d][:, :], nA[:, d:d + 1], None, ALU.mult, reads=[nA])
            p.op("dve", lambda e, d=d: e.tensor_tensor_scan(cumT[d][:, :], rm128[:, :], t8a[:, :], 0.0, ALU.mult, ALU.add),
                 reads=[rm128, t8a], writes=[cumT[d]])
        for cc in range(4):
            g = cc // 2
            for d in range(2):
                for (s0, s1) in segs:
                    for k_, src_ch in enumerate((cc, 4 + g, 6 + g)):
                        p.copy(Xd, Xd[:, k_, s0:s1], XCb, nat_ap(XCb.ap[:, src_ch, :], cfg, s0, s1, d),
                               eng=("pool", "act", "dve")[k_])
                p.memset(S2, S2.ap[:, :, :].rearrange("p a b -> p (a b)"), 0.0)
                p.memset(Sbf[0], Sbf[0].ap[:, :, :].rearrange("p a b -> p (a b)"), 0.0)
                cur = 0
                for ti in range(nt):
                    cs = slice(ti * 128, (ti + 1) * 128)
                    i2 = ti % 2
                    p.transpose(pTrX, pTrX[:, 0:128], Xd, Xd[:, 0, cs], identb, identb[:, :])
                    p.transpose(pTrB, pTrB[:, 0:128], Xd, Xd[:, 1, cs], identb, identb[:, :])
                    p.copy(xs_tok[i2], xs_tok[i2][:, :], pTrX, pTrX[:, 0:128], eng="act")
                    p.copy(B_tok[i2], B_tok[i2][:, :], pTrB, pTrB[:, 0:128], eng="dve")
                    p.transpose(pTrD, pTrD[:, 0:8], dtT[d], dtT[d][:, cs], ident, ident[0:8, 0:8])
                    p.transpose(pTrD, pTrD[:, 8:16], cumT[d], cumT[d][:, cs], ident, ident[0:8, 0:8])
                    dc = dc_tok[i2]
                    p.copy(dc, dc[:, :], pTrD, pTrD[:, 0:16], eng="dve")
                    for hh in range(2):
                        p.mm(pCR, pCR[:, hh, 0:128], sel, sel[:, 2 * cc + hh, :], cumT[d], cumT[d][:, cs])
                    cr = crow[i2]
                    p.copy(cr, cr[:, :, :], pCR, pCR[:, :, 0:128], eng="act")
                    p.mm(pSc, pSc[:, 0:128], Xd, Xd[:, 1, cs], Xd, Xd[:, 2, cs])
                    sc = sc_sb[i2]
                    p.copy(sc, sc[:, :], pSc, pSc[:, 0:128], eng="dve")
                    for hh in range(2):
                        h = 2 * cc + hh
                        p.stt(D1[hh], D1[hh][:, :], cr, cr[:, hh, :], dc[:, 8 + h:9 + h], mbias, mbias[:, :],
                              ALU.subtract, ALU.add, reads=[dc])
                        p.act(D1[hh], D1[hh][:, :], D1[hh], D1[hh][:, :], AF.Exp)
                        p.stt(attT[hh], attT[hh][:, :], D1[hh], D1[hh][:, :], dc[:, h:h + 1], sc, sc[:, :],
                              ALU.mult, ALU.mult, reads=[dc])
                        p.act(ec[hh], ec[hh][:, :], cr, cr[:, hh, :], AF.Exp)
                        p.tt(Chat[hh], Chat[hh][:, :], Xd, Xd[:, 2, cs], ec[hh], ec[hh][:, :], ALU.mult, eng="pool")
                        p.mm(pY, pY[hh * 64:(hh + 1) * 64, 0:128], xs_tok[i2], xs_tok[i2][:, hh * 64:(hh + 1) * 64],
                             attT[hh], attT[hh][:, :], start=True, stop=False)
                        p.mm(pY, pY[hh * 64:(hh + 1) * 64, 0:128], Sbf[cur], Sbf[cur][:, hh, :],
                             Chat[hh], Chat[hh][:, :], start=False, stop=True)
                    lastb = cr[:, :, 127]
                    p.tt(w2, w2[:, :], cr, lastb, dc, dc[:, 8 + 2 * cc:10 + 2 * cc], ALU.subtract)
                    p.act(w2, w2[:, :], w2, w2[:, :], AF.Exp)
                    p.tt(w2, w2[:, :], w2, w2[:, :], dc, dc[:, 2 * cc:2 * cc + 2], ALU.mult)
                    p.act(el2, el2[:, :], cr, lastb, AF.Exp)
                    xh = xhat[i2]
                    p.tt(xh, xh[:, :, :], xs_tok[i2], xs_tok[i2].ap[:, :].rearrange("p (a b) -> p a b", a=2),
                         w2, w2.ap[:, :].unsqueeze(2).broadcast_to([128, 2, 64]), ALU.mult)
                    p.mm(pKV, pKV[:, 0:128], B_tok[i2], B_tok[i2][:, :], xh, xh.ap[:, :, :].rearrange("p a b -> p (a b)"))
                    p.tt(S2, S2[:, :, :], S2, S2[:, :, :], el2, el2.ap[:, :].unsqueeze(2).broadcast_to([128, 2, 64]), ALU.mult)
                    S2f = S2.ap[:, :, :].rearrange("p a b -> p (a b)")
                    p.tt(S2, S2f, S2, S2f, pKV, pKV[:, 0:128], ALU.add)
                    nxt = 1 - cur
                    p.copy(Sbf[nxt], Sbf[nxt].ap[:, :, :].rearrange("p a b -> p (a b)"), S2, S2f, eng="act")
                    cur = nxt
                    sl, rev = nat_slice(cfg, ti * 128, (ti + 1) * 128, d)
                    if d == 0:
                        p.copy(yacc, yacc[:, sl], pY, pY[:, 0:128], eng="act")
                    else:
                        ya = yacc.ap[:, sl][:, ::-1]
                        p.tt(yacc, ya, yacc, ya, pY, pY[:, 0:128], ALU.add)
            p.load(raw[0], raw[0][:, :], XSF, XSF.ap[cc * 128:(cc + 1) * 128, c0:c0 + Tb], q="sp")
            p.load(raw[1], raw[1][:, :], YT, YT.ap[OD["z"] + cc * 128:OD["z"] + (cc + 1) * 128, c0:c0 + Tb], q="act")
            p.stt(yacc, yacc[:, :], raw[0], raw[0][:, :], dsk[:, cc:cc + 1], yacc, yacc[:, :], ALU.mult, ALU.add, reads=[dsk])
            p.act(raw[1], raw[1][:, :], raw[1], raw[1][:, :], AF.Silu)
            p.tt(yacc, yacc[:, :], yacc, yacc[:, :], raw[1], raw[1][:, :], ALU.mult)
            p.store(YPRE, YPRE.ap[cc * 128:(cc + 1) * 128, c0:c0 + Tb], yacc, yacc[:, :], q="sp")
        for g in range(2):
            for k_ in range(2):
                p.load(raw[k_], raw[k_][:, :], YPRE, YPRE.ap[(2 * g + k_) * 128:(2 * g + k_ + 1) * 128, c0:c0 + Tb],
                       q="sp" if k_ else "act")
            p.act(cacc, cacc[:, :], raw[0], raw[0][:, :], AF.Square)
            p.act(yacc, yacc[:, :], raw[1], raw[1][:, :], AF.Square)
            for (a0, a1) in blocks(Tb, 512):
                p.mm(pN, pN[:, 0:a1 - a0], ones256, ones256[:, :], cacc, cacc[:, a0:a1], start=True, stop=False)
                p.mm(pN, pN[:, 0:a1 - a0], ones256, ones256[:, :], yacc, yacc[:, a0:a1], start=False, stop=True)
                p.act(cacc, cacc[:, a0:a1], pN, pN[:, 0:a1 - a0], AF.Ln, bias=float(NORM_EPS))
            p.act(cacc, cacc[:, :], cacc, cacc[:, :], AF.Exp, scale=-0.5)
            for k_ in range(2):
                p.stt(obf, obf[:, :], raw[k_], raw[k_][:, :], gnm[:, 2 * g + k_:2 * g + k_ + 1], cacc, cacc[:, :],
                      ALU.mult, ALU.mult, reads=[gnm])
                p.store(G["MIXT"], G["MIXT"].ap[512 + (2 * g + k_) * 128:512 + (2 * g + k_ + 1) * 128, c0:c0 + Tb],
                        obf, obf[:, :], q="pool")
    p.close_scope()


def kernel(**inputs):
    x = np.asarray(inputs["x"])
    B, SEQ, _ = x.shape
    CTX = np.asarray(inputs["ctx"]).shape[1]
    depth = np.asarray(inputs["mod_w"]).shape[0]
    ncores = NCORES if B % NCORES == 0 else 1
    cfg = Cfg(B // ncores, SEQ, CTX, depth)
    p = build_program(cfg)
    nc = p.finish()
    maps = prep_inputs(inputs, cfg, ncores)
    res = run_bass_kernel_spmd(nc, maps, core_ids=list(range(ncores))).results
    out = np.empty((B, SEQ, D), np.float32)
    for ci in range(ncores):
        o = res[ci]["out"].reshape(cfg.nb, cfg.tb, D)
        for bl in range(cfg.nb):
            out[ci * cfg.nb + bl] = o[bl, CTX:, :]
    return out
```

```python
import numpy as np
from contextlib import ExitStack
import concourse.bass as bass
import concourse.mybir as mybir
from concourse.bass_utils import run_bass_kernel_spmd

F32 = mybir.dt.float32
BF16 = mybir.dt.bfloat16
U32 = mybir.dt.uint32
I32 = mybir.dt.int32
AF = mybir.ActivationFunctionType
ALU = mybir.AluOpType
AX = mybir.AxisListType

NCORES = 8
ENGS = ("pe", "dve", "act", "pool", "sp")


class Buf:
    __slots__ = ("ap", "name", "w", "r", "sem", "is_dram")

    def __init__(self, ap, name, is_dram=False):
        self.ap = ap
        self.name = name
        self.w = None
        self.r = []
        self.sem = None
        self.is_dram = is_dram

    def __getitem__(self, idx):
        return self.ap[idx]


SEM_LIMIT = 30000


class P:
    def __init__(self, name="k"):
        self.nc = bass.Bass("TRN2", target_bir_lowering=False)
        self.es = ExitStack()
        self.scopes = []
        nc_ = self.nc
        self.engs = {"pe": nc_.tensor, "dve": nc_.vector, "act": nc_.scalar, "pool": nc_.gpsimd, "sp": nc_.sync}
        self.known = {e: {} for e in ENGS}
        self.nsem = 0
        self.esem = {}
        self.ecnt = {}
        self.retired = []
        for e in ENGS:
            self._new_esem(e)
        self.dpool = {'sw': [], 'hw': []}
        self.dlive = []
        self.nbuf = 0
        self.out_events = []

    def _alloc_sem(self, tag):
        self.nsem += 1
        return self.es.enter_context(self.nc.semaphore(f"{tag}{self.nsem}"))

    def _new_esem(self, e):
        if e in self.esem:
            self.retired.append((id(self.esem[e]), self.ecnt[e], self.esem[e], e == "pe"))
        self.esem[e] = self._alloc_sem(f"se_{e}_")
        self.ecnt[e] = 0

    def _stack(self):
        return self.scopes[-1][0] if self.scopes else self.es

    def dram(self, name, shape, dtype, kind="Internal"):
        t = self.nc.dram_tensor(name, list(shape), dtype, kind=kind)
        return Buf(t.ap(), name, is_dram=True)

    def sbuf(self, shape, dtype, name=None):
        self.nbuf += 1
        name = f"{name or 'sb'}_{self.nbuf}"
        t = self._stack().enter_context(self.nc.sbuf_tensor(name, list(shape), dtype))
        return Buf(t, name)

    def psum(self, shape, dtype=F32, name=None):
        self.nbuf += 1
        name = f"{name or 'ps'}_{self.nbuf}"
        t = self._stack().enter_context(self.nc.psum_tensor(name, list(shape), dtype))
        return Buf(t, name)

    def view(self, ap, name="v"):
        return Buf(ap, name)

    def open_scope(self):
        self.scopes.append((ExitStack(), []))

    def close_scope(self):
        self.barrier()
        st, bufs = self.scopes.pop()
        for (b, kind, cur) in bufs:
            self.dpool[kind].append(cur)
            if b.sem is not None and b.sem.get(kind) is cur:
                del b.sem[kind]
        st.close()

    def barrier(self):
        evs = []
        for e in ENGS:
            if self.ecnt[e] > 0:
                evs.append((id(self.esem[e]), self.ecnt[e], self.esem[e], False))
        for s in self.dlive:
            if s[1] > 0:
                evs.append((id(s[0]), s[1], s[0], False))
        for e in ENGS:
            waits = []
            for sid, val, semh, _ in evs:
                if self.known[e].get(sid, 0) >= val:
                    continue
                if sid == id(self.esem[e]) and e == "pe":
                    pass
                self.known[e][sid] = val
                waits.append((semh, val))
            if waits:
                for s, v in waits:
                    self.engs[e].wait_ge(s, v)

    def _deps(self, eng, reads, writes):
        deps = []
        for b in reads:
            if b.w is not None:
                deps.append(b.w)
        for b in writes:
            if b.r:
                deps.extend(b.r)
            elif b.w is not None:
                deps.append(b.w)
        need = {}
        for ev in deps:
            sid, val, semh, is_pe = ev
            if is_pe and eng == "pe":
                continue
            if self.known[eng].get(sid, 0) >= val:
                continue
            if sid not in need or need[sid][0] < val:
                need[sid] = (val, semh)
        waits = []
        for sid, (val, semh) in need.items():
            self.known[eng][sid] = val
            waits.append((semh, val))
        return waits

    def _commit(self, ev, reads, writes):
        for b in reads:
            b.r.append(ev)
            if len(b.r) > 48:
                b.r = b.r[-48:]
        for b in writes:
            b.w = ev
            b.r = []

    def op(self, eng, fn, reads=(), writes=()):
        if self.ecnt[eng] >= SEM_LIMIT:
            self._new_esem(eng)
        waits = self._deps(eng, reads, writes)
        self.ecnt[eng] += 1
        semh = self.esem[eng]
        ev = (id(semh), self.ecnt[eng], semh, eng == "pe")

        def emit(e, waits=waits, fn=fn, semh=semh):
            for s, v in waits:
                e.wait_ge(s, v)
            fn(e).then_inc(semh, 1)

        emit(self.engs[eng])
        self._commit(ev, reads, writes)
        return ev

    def dma(self, q, out_b, out_ap, in_b, in_ap, fn=None, extra_reads=(), **kw):
        owner = out_b if not out_b.is_dram else in_b
        kind = "sw" if q == "pool" else "hw"
        if owner.sem is None:
            owner.sem = {}
        cur = owner.sem.get(kind)
        if cur is None or cur[1] >= SEM_LIMIT:
            cand = [s for s in self.dpool[kind] if s[1] < SEM_LIMIT]
            if cand:
                cur = cand[0]
                self.dpool[kind].remove(cur)
            else:
                cur = [self._alloc_sem("sd_"), 0]
                self.dlive.append(cur)
            owner.sem[kind] = cur
            if self.scopes:
                self.scopes[-1][1].append((owner, kind, cur))
        waits = self._deps(q, [in_b] + list(extra_reads), [out_b])
        cur[1] += 16
        semh = cur[0]
        ev = (id(semh), cur[1], semh, False)

        def emit(e, waits=waits, semh=semh):
            for s, v in waits:
                e.wait_ge(s, v)
            if fn is None:
                e.dma_start(out=out_ap, in_=in_ap, **kw).then_inc(semh, 16)
            else:
                fn(e).then_inc(semh, 16)

        emit(self.engs[q])
        self._commit(ev, [in_b] + list(extra_reads), [out_b])
        return ev

    def finish(self):
        while self.scopes:
            self.close_scope()
        self.barrier()
        nc = self.nc
        self.es.close()
        return nc

    def load(self, dst, dst_ap, src, src_ap, q="sp", **kw):
        return self.dma(q, dst, dst_ap, src, src_ap, **kw)

    def store(self, dst, dst_ap, src, src_ap, q="pool", **kw):
        return self.dma(q, dst, dst_ap, src, src_ap, **kw)

    def mm(self, out_b, out_ap, lhsT_b, lhsT_ap, rhs_b, rhs_ap, start=True, stop=True, extra_reads=()):
        return self.op("pe", lambda e: e.matmul(out_ap, lhsT_ap, rhs_ap, start=start, stop=stop),
                       reads=[lhsT_b, rhs_b] + list(extra_reads), writes=[out_b])

    def transpose(self, out_b, out_ap, in_b, in_ap, ident_b, ident_ap):
        return self.op("pe", lambda e: e.transpose(out_ap, in_ap, ident_ap),
                       reads=[in_b, ident_b], writes=[out_b])

    def act(self, out_b, out_ap, in_b, in_ap, func, bias=None, scale=None, reads=(), accum=None, accum_b=None):
        kw = {}
        if bias is not None:
            kw["bias"] = bias
        if scale is not None:
            kw["scale"] = scale
        if accum is not None:
            kw["accum_out"] = accum
        w = [out_b] + ([accum_b] if accum_b is not None else [])
        return self.op("act", lambda e: e.activation(out_ap, in_ap, func, **kw),
                       reads=[in_b] + list(reads), writes=w)

    def tt(self, out_b, out_ap, a_b, a_ap, b_b, b_ap, op, eng="dve"):
        return self.op(eng, lambda e: e.tensor_tensor(out_ap, a_ap, b_ap, op),
                       reads=[a_b, b_b], writes=[out_b])

    def ts(self, out_b, out_ap, in_b, in_ap, s1, s2, op0, op1=None, reads=(), eng="dve", accum=None, accum_b=None):
        kw = {}
        if accum is not None:
            kw["accum_out"] = accum
        w = [out_b] + ([accum_b] if accum_b is not None else [])
        if op1 is None:
            return self.op(eng, lambda e: e.tensor_scalar(out_ap, in_ap, s1, None, op0, **kw),
                           reads=[in_b] + list(reads), writes=w)
        return self.op(eng, lambda e: e.tensor_scalar(out_ap, in_ap, s1, s2, op0, op1, **kw),
                       reads=[in_b] + list(reads), writes=w)

    def stt(self, out_b, out_ap, a_b, a_ap, scalar, b_b, b_ap, op0, op1, reads=(), accum=None, accum_b=None):
        kw = {}
        if accum is not None:
            kw["accum_out"] = accum
        w = [out_b] + ([accum_b] if accum_b is not None else [])
        return self.op("dve", lambda e: e.scalar_tensor_tensor(out_ap, a_ap, scalar, b_ap, op0, op1, **kw),
                       reads=[a_b, b_b] + list(reads), writes=w)

    def copy(self, out_b, out_ap, in_b, in_ap, eng="dve"):
        if eng == "act":
            return self.op("act", lambda e: e.activation(out_ap, in_ap, AF.Copy), reads=[in_b], writes=[out_b])
        return self.op(eng, lambda e: e.tensor_copy(out_ap, in_ap), reads=[in_b], writes=[out_b])

    def memset(self, out_b, out_ap, val, eng="dve"):
        return self.op(eng, lambda e: e.memset(out_ap, val), reads=[], writes=[out_b])


def run(prog, in_maps):
    nc = prog.finish()
    res = run_bass_kernel_spmd(nc, in_maps, core_ids=list(range(NCORES)))
    return res.results


D = 1024
KC = D // 128
N_MOD = 6
NORM_EPS = 1e-6
GRID_W = 64
LB_FLOOR = 1e-30
CH = 32
EV = dict(q=0, i=512, zf=1024, zb=1536, g=2048, aq=2560, ak=3072, av=3328, aqs=3584, aks=4096)
EV_NF = 4352
OD = dict(q=0, k=256, v=512, g=1024, z=1536, xs=2048, bm=2560, cm=2816, lrf=3072, lrb=3088, dtf=3104, dtb=3112)
OD_NF = 3200
PEER_HEADS, PEER_NKEYS, PEER_TOPK = 8, 128, 16


class Cfg:
    def __init__(self, nb, seq, ctx, depth):
        self.nb, self.seq, self.ctx, self.depth = nb, seq, ctx, depth
        self.tb = seq + ctx
        self.T = nb * self.tb
        self.R = nb + 1
        self.alpha = (2 * depth) ** 0.25
        assert seq % 128 == 0 and ctx % 128 == 0

    def segs(self):
        out = []
        for b in range(self.nb):
            o = b * self.tb
            out.append((o, o + self.ctx, self.nb))
            out.append((o + self.ctx, o + self.tb, b))
        return out

    def seg_of_tile(self, ti):
        t = ti * 128
        for (a, b, r) in self.segs():
            if a <= t < b:
                return r
        raise AssertionError


def blocks(n, step):
    return [(a, min(a + step, n)) for a in range(0, n, step)]


def stage_mod(p, cfg, io, G):
    R = cfg.R
    NM = N_MOD * D
    p.open_scope()
    cT = p.sbuf([128, KC * R], F32, "cT")
    p.load(cT, cT[:, :], io["cT"], io["cT"].ap[:, :])
    sc = p.sbuf([128, KC, R], F32, "silu_c")
    p.act(sc, sc.ap[:, :, :].rearrange("p k r -> p (k r)"), cT, cT[:, :], AF.Silu)
    mbT = p.sbuf([128, cfg.depth * 48], F32, "mbT")
    p.load(mbT, mbT[:, :], io["mod_bT"], io["mod_bT"].ap[:, :])
    CB = 768
    wst = [p.sbuf([128, KC, CB], F32, f"mw{i}") for i in range(2)]
    psF = [p.psum([128, 512], F32, f"psF{i}") for i in range(2)]
    psT = [p.psum([128, 512], F32, f"psT{i}") for i in range(2)]
    mbrow = p.sbuf([R, NM], F32, "mbrow")
    mtok = p.sbuf([R, NM], F32, "mtok")
    modT = G["modT"]
    it = 0
    for l in range(cfg.depth):
        p.load(mbrow, mbrow[:, :], io["mod_b"], io["mod_b"].ap[l:l + 1, :].partition_broadcast(R))
        for cb in range(NM // CB):
            w = wst[it % 2]
            src = io["mod_w"].ap[l, :, cb * CB:(cb + 1) * CB].rearrange("(kc p) n -> p kc n", p=128)
            p.load(w, w[:, :, :], io["mod_w"], src, q="sp" if it % 2 else "act")
            pf = psF[it % 2]
            for dcl in range(CB // 128):
                dc = cb * (CB // 128) + dcl
                for kc in range(KC):
                    p.mm(pf, pf[:, dcl * R:(dcl + 1) * R], w, w[:, kc, dcl * 128:(dcl + 1) * 128],
                         sc, sc[:, kc, :], start=(kc == 0), stop=(kc == KC - 1))
                p.ts(modT, modT[:, l, dc, :], pf, pf[:, dcl * R:(dcl + 1) * R],
                     mbT[:, l * 48 + dc:l * 48 + dc + 1], None, ALU.add, reads=[mbT])
            pt = psT[it % 2]
            for (n0, n1) in blocks(CB, 512):
                for kc in range(KC):
                    p.mm(pt, pt[0:R, 0:n1 - n0], sc, sc[:, kc, :], w, w[:, kc, n0:n1],
                         start=(kc == 0), stop=(kc == KC - 1))
                p.tt(mtok, mtok[:, cb * CB + n0:cb * CB + n1], pt, pt[0:R, 0:n1 - n0],
                     mbrow, mbrow[:, cb * CB + n0:cb * CB + n1], ALU.add)
            it += 1
        p.store(G["modtok"], G["modtok"].ap[l, :, :], mtok, mtok[:, :])
    p.close_scope()


def stage_inproj(p, cfg, G, l, W_ap, NF, YT):
    T = cfg.T
    p.open_scope()
    ident = G["ident"]
    modT = G["modT"]
    w_bf = p.sbuf([128, KC, NF], BF16, "w_bf")
    wst = [p.sbuf([128, NF], F32, f"wst{i}") for i in range(2)]
    for kc in range(KC):
        st = wst[kc % 2]
        p.load(st, st[:, :], G["ext"], W_ap[kc * 128:(kc + 1) * 128, :], q="sp" if kc % 2 else "act")
        p.copy(w_bf, w_bf[:, kc, :], st, st[:, :], eng="pool" if kc % 2 else "dve")
    scale1 = p.sbuf([128, KC, cfg.R], F32, "scale1")
    p.ts(scale1, scale1[:, :, :], modT, modT[:, l, 8:16, :], 1.0, None, ALU.add)
    hst = [p.sbuf([128, D], F32, f"hst{i}") for i in range(2)]
    uT = [p.sbuf([128, KC, 512], BF16, f"uT{i}") for i in range(2)]
    pT = [p.psum([128, 4, 128], F32, f"pT{i}") for i in range(4)]
    pY = [p.psum([128, 512], F32, f"pY{i}") for i in range(3)]
    ysb = [p.sbuf([128, 512], F32, f"ysb{i}") for i in range(4)]
    j = 0
    for bi, (t0, t1) in enumerate(blocks(T, 512)):
        u = uT[bi % 2]
        nt = (t1 - t0) // 128
        for tl in range(nt):
            ti = t0 // 128 + tl
            r = cfg.seg_of_tile(ti)
            hs = hst[ti % 2]
            p.load(hs, hs[:, :], G["H"], G["H"].ap[ti * 128:(ti + 1) * 128, :], q="sp")
            for half in range(2):
                pt = pT[(2 * ti + half) % 4]
                for kk in range(4):
                    kc = half * 4 + kk
                    p.transpose(pt, pt[:, kk, :], hs, hs[:, kc * 128:(kc + 1) * 128], ident, ident[:, :])
                for kk in range(4):
                    kc = half * 4 + kk
                    p.act(u, u[:, kc, tl * 128:(tl + 1) * 128], pt, pt[:, kk, :], AF.Identity,
                          bias=modT[:, l, kc, r:r + 1], scale=scale1[:, kc, r:r + 1], reads=[modT, scale1])
        w = t1 - t0
        for fc in range(NF // 128):
            py = pY[j % 3]
            for kc in range(KC):
                p.mm(py, py[:, 0:w], w_bf, w_bf[:, kc, fc * 128:(fc + 1) * 128], u, u[:, kc, 0:w],
                     start=(kc == 0), stop=(kc == KC - 1))
            ys = ysb[j % 4]
            p.copy(ys, ys[:, 0:w], py, py[:, 0:w], eng="dve" if j % 2 else "act")
            p.store(YT, YT.ap[fc * 128:(fc + 1) * 128, t0:t1], ys, ys[:, 0:w], q="pool" if j % 2 else "sp")
            j += 1
    p.close_scope()


def layernorm_tile(p, cfg, G, r_b, out_b, lng, lnb, scr):
    stats, mv, rstd, tmp = scr
    for c in range(2):
        p.op("dve", lambda e, c=c: e.bn_stats(stats[:, c, :], r_b[:, c * 512:(c + 1) * 512]),
             reads=[r_b], writes=[stats])
    p.op("dve", lambda e: e.bn_aggr(mv[:, :], stats.ap[:, :, :].rearrange("p a b -> p (a b)")),
         reads=[stats], writes=[mv])
    p.act(rstd, rstd[:, 0:1], mv, mv[:, 1:2], AF.Ln, bias=float(NORM_EPS))
    p.act(rstd, rstd[:, 1:2], rstd, rstd[:, 0:1], AF.Exp, scale=-0.5)
    p.ts(tmp, tmp[:, :], r_b, r_b[:, :], mv[:, 0:1], rstd[:, 1:2], ALU.subtract, ALU.mult, reads=[mv, rstd])
    p.tt(tmp, tmp[:, :], tmp, tmp[:, :], lng, lng[:, :], ALU.mult)
    p.tt(out_b, out_b[:, :], tmp, tmp[:, :], lnb, lnb[:, :], ALU.add, eng="pool")


def stage_resid_ln(p, cfg, G, io, l, which, W_ap, mix_dram):
    T = cfg.T
    p.open_scope()
    gate_off = (2 if which == 0 else 5) * D
    lng = p.sbuf([128, D], F32, "lng")
    lnb = p.sbuf([128, D], F32, "lnb")
    p.load(lng, lng[:, :], io["ln_g"], io["ln_g"].ap[l, which:which + 1, :].partition_broadcast(128))
    p.load(lnb, lnb[:, :], io["ln_b"], io["ln_b"].ap[l, which:which + 1, :].partition_broadcast(128))
    gb = []
    for r in range(cfg.R):
        g = p.sbuf([128, D], F32, f"gate{r}")
        p.load(g, g[:, :], G["modtok"], G["modtok"].ap[l, r:r + 1, gate_off:gate_off + D].partition_broadcast(128))
        gb.append(g)
    if which == 0:
        w_bf = p.sbuf([128, KC, D], BF16, "wo_bf")
        wst = [p.sbuf([128, D], F32, f"wost{i}") for i in range(2)]
        for kc in range(KC):
            st = wst[kc % 2]
            p.load(st, st[:, :], G["ext"], W_ap[kc * 128:(kc + 1) * 128, :], q="act")
            p.copy(w_bf, w_bf[:, kc, :], st, st[:, :], eng="pool")
        mx = [p.sbuf([128, KC, 128], BF16, f"mx{i}") for i in range(2)]
        po = [p.psum([128, 2, 512], F32, f"po{i}") for i in range(2)]
    else:
        br = [p.sbuf([128, D], F32, f"br{i}") for i in range(2)]
    hs = [p.sbuf([128, D], F32, f"h{i}") for i in range(2)]
    rb = [p.sbuf([128, D], F32, f"r{i}") for i in range(2)]
    ob = [p.sbuf([128, D], F32, f"o{i}") for i in range(2)]
    scr = (p.sbuf([128, 2, 6], F32, "stats"), p.sbuf([128, 2], F32, "mv"), p.sbuf([128, 2], F32, "rstd"),
           p.sbuf([128, D], F32, "lntmp"))
    for ti in range(T // 128):
        r = cfg.seg_of_tile(ti)
        h = hs[ti % 2]
        rr = rb[ti % 2]
        p.load(h, h[:, :], G["H"], G["H"].ap[ti * 128:(ti + 1) * 128, :], q="sp")
        if which == 0:
            m = mx[ti % 2]
            p.load(m, m[:, :, :], mix_dram,
                   mix_dram.ap[:, ti * 128:(ti + 1) * 128].rearrange("(kc p) t -> p kc t", p=128), q="act")
            ps = po[ti % 2]
            for nb in range(2):
                for kc in range(KC):
                    p.mm(ps, ps[:, nb, :], m, m[:, kc, :], w_bf, w_bf[:, kc, nb * 512:(nb + 1) * 512],
                         start=(kc == 0), stop=(kc == KC - 1))
            p.tt(rr, rr.ap[:, :], ps, ps.ap[:, :, :].rearrange("p a b -> p (a b)"), gb[r], gb[r][:, :], ALU.mult)
        else:
            b_ = br[ti % 2]
            p.load(b_, b_[:, :], mix_dram, mix_dram.ap[ti * 128:(ti + 1) * 128, :], q="act")
            p.tt(rr, rr[:, :], b_, b_[:, :], gb[r], gb[r][:, :], ALU.mult, eng="pool")
        p.stt(rr, rr[:, :], h, h[:, :], float(cfg.alpha), rr, rr[:, :], ALU.mult, ALU.add)
        o = ob[ti % 2]
        layernorm_tile(p, cfg, G, rr, o, lng, lnb, scr)
        p.store(G["H"], G["H"].ap[ti * 128:(ti + 1) * 128, :], o, o[:, :], q="pool")
    p.close_scope()


def build_program(cfg, stop_after=None, debug=()):
    p = P("mk")
    io = {}
    G = {}

    def ext(name, shape, dtype=F32):
        io[name] = p.dram(name, shape, dtype, "ExternalInput")
        return io[name]

    T, R, L = cfg.T, cfg.R, cfg.depth
    ne, no = (L + 1) // 2, L // 2
    ext("h0", [T, D])
    ext("cT", [128, KC * R])
    ext("mod_w", [L, D, N_MOD * D])
    ext("mod_b", [L, N_MOD * D])
    ext("mod_bT", [128, L * 48])
    ext("ln_g", [L, 2, D])
    ext("ln_b", [L, 2, D])
    ext("ev_w_in", [ne, D, EV_NF])
    ext("ev_w_out", [ne, D, D])
    if no:
        ext("od_w_in", [no, D, OD_NF])
        ext("od_w_out", [no, D, D])
    ext("ident", [128, 128])
    ext("peer_wq", [L, D, 2048])
    ext("peer_subkT", [L, 128, 2048])
    for l_ in range(L):
        ext(f"peer_u{l_}", [PEER_NKEYS * PEER_NKEYS, D])
        ext(f"peer_v{l_}", [PEER_NKEYS * PEER_NKEYS, D])
    ext("cmask", [128, 128])
    ext("iota16", [128, 16])
    ext("rm3", [128, 1])
    ext("rmask", [128, cfg.tb])
    ext("cosT", [128, cfg.tb])
    ext("sinT", [128, cfg.tb])
    ext("hg_lbT", [128, 2 * ne * 4])
    ext("hg_norm_gT", [ne, 128, 1])
    ext("at_normT", [ne, 128, 4])
    if no:
        ext("ssd_conv_wT", [no, 128, 40])
        ext("ssd_conv_bT", [no, 128, 8])
        ext("ssd_dt_biasT", [no, 8, 2])
        ext("ssd_a_logT", [no, 8, 2])
        ext("ssd_dT", [no, 128, 4])
        ext("ssd_norm_gT", [no, 128, 4])
        ext("selT", [8, 8 * 128])
        ext("mbias", [128, 128])
        ext("rmask128", [8, cfg.tb])
        ext("gla_gate_w", [no, 2, 16, 256])
        ext("gla_gate_bT", [no, 64, 8])
        ext("gla_norm_gT", [no, 128, 1])
    G["ext"] = Buf(None, "ext", is_dram=True)
    out = p.dram("out", [T, D], F32, "ExternalOutput")
    G["H"] = out
    kind = "ExternalOutput" if debug else "Internal"
    G["modtok"] = p.dram("modtok", [L, R, N_MOD * D], F32, kind)
    G["YT"] = p.dram("YT", [EV_NF, T], F32, kind)
    G["MIXT"] = p.dram("MIXT", [D, T], BF16, kind)
    G["PEERO"] = p.dram("PEERO", [T, D], F32, kind)
    G["UVB"] = p.dram("UVB", [PEER_NKEYS * PEER_NKEYS, 2 * D], BF16, "Internal")
    G["XSF"] = p.dram("XSF", [512, T], F32, "Internal")
    G["YPRE"] = p.dram("YPRE", [512, T], F32, "Internal")
    G["ident"] = p.sbuf([128, 128], F32, "ident")
    p.load(G["ident"], G["ident"][:, :], io["ident"], io["ident"].ap[:, :])
    G["ident_bf"] = p.sbuf([128, 128], BF16, "ident_bf")
    p.copy(G["ident_bf"], G["ident_bf"][:, :], G["ident"], G["ident"][:, :])
    G["modT"] = p.sbuf([128, L, 48, R], F32, "modT")
    G["ones_mean"] = p.sbuf([128, 128], F32, "ones_mean")
    p.memset(G["ones_mean"], G["ones_mean"][:, :], 1.0 / 128.0)
    for nm in ("cmask", "rm3"):
        G[nm] = p.sbuf(list(io[nm].ap.shape), F32, nm)
        p.load(G[nm], G[nm][:, :], io[nm], io[nm].ap[:, :])

    p.open_scope()
    cp = [p.sbuf([128, D], F32, f"cp{i}") for i in range(3)]
    for ti in range(T // 128):
        c = cp[ti % 3]
        p.load(c, c[:, :], io["h0"], io["h0"].ap[ti * 128:(ti + 1) * 128, :], q="sp")
        p.store(G["H"], G["H"].ap[ti * 128:(ti + 1) * 128, :], c, c[:, :], q="act")
    p.close_scope()
    stage_mod(p, cfg, io, G)
    if stop_after == "mod":
        return p
    for l in range(L):
        j = l // 2
        if l % 2 == 0:
            stage_inproj(p, cfg, G, l, io["ev_w_in"].ap[j], EV_NF, G["YT"])
        else:
            stage_inproj(p, cfg, G, l, io["od_w_in"].ap[j], OD_NF, G["YT"])
        if stop_after == f"inproj{l}":
            return p
        if l % 2 == 0:
            stage_vscan(p, cfg, G, io, l, "hgrn")
            if stop_after == f"scan{l}":
                return p
            stage_attn(p, cfg, G, io, l)
            if stop_after == f"attn{l}":
                return p
            stage_resid_ln(p, cfg, G, io, l, 0, io["ev_w_out"].ap[j], G["MIXT"])
        else:
            stage_vscan(p, cfg, G, io, l, "gla")
            if stop_after == f"scan{l}":
                return p
            stage_ssd(p, cfg, G, io, l)
            if stop_after == f"ssd{l}":
                return p
            stage_resid_ln(p, cfg, G, io, l, 0, io["od_w_out"].ap[j], G["MIXT"])
        if stop_after == f"mix{l}":
            return p
        stage_peer(p, cfg, G, io, l)
        if stop_after == f"peer{l}":
            return p
        stage_resid_ln(p, cfg, G, io, l, 1, None, G["PEERO"])
        if stop_after == f"layer{l}":
            return p
    return p


def _pair_swap(n):
    idx = np.arange(n)
    return idx ^ 1


def prep_inputs(inp, cfg, ncores):
    L = cfg.depth
    f32 = np.float32
    x, c, ctx, c_ctx = (np.asarray(inp[k], f32) for k in ("x", "c", "ctx", "c_ctx"))
    ev_w_in = np.asarray(inp["ev_w_in"], f32)
    aq = ev_w_in[:, :, EV["aq"]:EV["aq"] + 512][:, :, _pair_swap(512)]
    ak = ev_w_in[:, :, EV["ak"]:EV["ak"] + 256][:, :, _pair_swap(256)]
    ev_ext = np.ascontiguousarray(np.concatenate([ev_w_in, aq, ak], axis=2))
    od_w_in = np.asarray(inp["od_w_in"], f32)
    no = od_w_in.shape[0]
    if no:
        q, k, v, g, lrf, lrb, z, xs, bm, cm, dtf, dtb = np.split(
            od_w_in, np.cumsum([256, 256, 512, 512, 16, 16, 512, 512, 256, 256, 8, 8])[:-1].tolist(), axis=2)
        pad = np.zeros((no, D, OD_NF - 3120), f32)
        od_ext = np.ascontiguousarray(np.concatenate([q, k, v, g, z, xs, bm, cm, lrf, lrb, dtf, dtb, pad], axis=2))
    mod_b = np.asarray(inp["mod_b"], f32)
    mod_bT = np.ascontiguousarray(mod_b.reshape(L, 48, 128).transpose(2, 0, 1).reshape(128, L * 48))
    shared = {
        "mod_w": np.asarray(inp["mod_w"], f32), "mod_b": mod_b, "mod_bT": mod_bT,
        "ln_g": np.asarray(inp["ln_g"], f32), "ln_b": np.asarray(inp["ln_b"], f32),
        "ev_w_in": ev_ext, "ev_w_out": np.asarray(inp["ev_w_out"], f32),
        "ident": np.eye(128, dtype=f32),
    }
    shared["peer_wq"] = np.asarray(inp["peer_wq"], f32)
    sk = np.asarray(inp["peer_subkeys"], f32)
    shared["peer_subkT"] = np.ascontiguousarray(sk.transpose(0, 4, 1, 2, 3).reshape(L, 128, 2048))
    for l_ in range(L):
        shared[f"peer_u{l_}"] = np.asarray(inp["peer_u"][l_], f32)
        shared[f"peer_v{l_}"] = np.asarray(inp["peer_v"][l_], f32)
    jj = np.arange(128)
    shared["cmask"] = ((jj[:, None] // CH == jj[None, :] // CH) & (jj[:, None] <= jj[None, :])).astype(f32)
    shared["iota16"] = np.tile(np.arange(16, dtype=f32), (128, 1))
    shared["rm3"] = (jj >= 96).astype(f32)[:, None].copy()
    rm = np.ones((128, cfg.tb), f32)
    rm[:, ::CH] = 0.0
    shared["rmask"] = rm
    rows_ = cfg.seq // GRID_W
    trow = np.repeat(np.arange(rows_), GRID_W).astype(np.float64)
    tcol = np.tile(np.arange(GRID_W), rows_).astype(np.float64)
    inv = 10000.0 ** (-np.arange(32, dtype=np.float64) / 32)
    ang = np.concatenate([trow[:, None] * inv, tcol[:, None] * inv], axis=-1)
    angd = np.repeat(ang, 2, axis=1).T
    cosT = np.ones((128, cfg.tb)); sinT = np.zeros((128, cfg.tb))
    cosT[:, cfg.ctx:] = np.cos(angd)
    sgn = np.where(np.arange(128) % 2 == 0, -1.0, 1.0)[:, None]
    sinT[:, cfg.ctx:] = np.sin(angd) * sgn
    shared["cosT"] = cosT.astype(f32); shared["sinT"] = sinT.astype(f32)
    ne = ev_w_in.shape[0]
    lbl = np.asarray(inp["hg_lb_logits"], f32)
    shared["hg_lbT"] = np.ascontiguousarray(lbl.reshape(2, ne, 4, 128).transpose(3, 0, 1, 2).reshape(128, 2 * ne * 4))
    shared["hg_norm_gT"] = np.ascontiguousarray(np.asarray(inp["hg_norm_g"], f32)[:, :, None])
    gqn = np.asarray(inp["at_q_norm_g"], f32); gkn = np.asarray(inp["at_k_norm_g"], f32)
    sw = _pair_swap(128)
    shared["at_normT"] = np.ascontiguousarray(np.stack([gqn, gqn[:, sw], gkn, gkn[:, sw]], axis=2))
    if no:
        cwt = np.asarray(inp["ssd_conv_w"], f32)
        shared["ssd_conv_wT"] = np.ascontiguousarray(cwt.reshape(no, 5, 8, 128).transpose(0, 3, 2, 1).reshape(no, 128, 40))
        shared["ssd_conv_bT"] = np.ascontiguousarray(np.asarray(inp["ssd_conv_b"], f32).reshape(no, 8, 128).transpose(0, 2, 1))
        shared["ssd_dt_biasT"] = np.ascontiguousarray(np.asarray(inp["ssd_dt_bias"], f32).transpose(0, 2, 1))
        shared["ssd_a_logT"] = np.ascontiguousarray(np.asarray(inp["ssd_a_log"], f32).transpose(0, 2, 1))
        dd = np.asarray(inp["ssd_d"], f32)
        shared["ssd_dT"] = np.ascontiguousarray(np.repeat(dd, 64, axis=1).reshape(no, 4, 128).transpose(0, 2, 1))
        shared["ssd_norm_gT"] = np.ascontiguousarray(np.asarray(inp["ssd_norm_g"], f32).reshape(no, 4, 128).transpose(0, 2, 1))
        selT = np.zeros((8, 8, 128), f32)
        for h_ in range(8):
            selT[h_, h_, :] = 1.0
        shared["selT"] = selT.reshape(8, 8 * 128)
        shared["mbias"] = np.where(jj[:, None] <= jj[None, :], 0.0, -30000.0).astype(f32)
        rm128 = np.ones((8, cfg.tb), f32)
        rm128[:, ::128] = 0.0
        shared["rmask128"] = rm128
        shared["gla_gate_w"] = np.asarray(inp["gla_gate_w"], f32)
        gb_ = np.asarray(inp["gla_gate_b"], f32)
        shared["gla_gate_bT"] = np.ascontiguousarray(gb_.reshape(no, 2, 4, 64).transpose(0, 3, 1, 2).reshape(no, 64, 8))
        shared["gla_norm_gT"] = np.ascontiguousarray(np.asarray(inp["gla_norm_g"], f32)[:, :, None])
    if no:
        shared["od_w_in"] = od_ext
        shared["od_w_out"] = np.asarray(inp["od_w_out"], f32)
    maps = []
    for ci in range(ncores):
        bs = range(ci * cfg.nb, (ci + 1) * cfg.nb)
        h0 = np.concatenate([np.concatenate([ctx[b], x[b]], axis=0) for b in bs], axis=0)
        cv = np.stack([c[b] for b in bs] + [c_ctx], axis=0)
        cT = np.ascontiguousarray(cv.T.reshape(KC, 128, cfg.R).transpose(1, 0, 2).reshape(128, KC * cfg.R))
        m = dict(shared)
        m["h0"] = np.ascontiguousarray(h0)
        m["cT"] = cT
        maps.append(m)
    return maps


def nat_slice(cfg, s0, s1, d):
    if d == 0:
        return slice(s0, s1), False
    if s1 <= cfg.ctx:
        return slice(cfg.ctx - s1, cfg.ctx - s0), True
    assert s0 >= cfg.ctx
    return slice(cfg.tb - (s1 - cfg.ctx), cfg.tb - (s0 - cfg.ctx)), True


def nat_ap(ap, cfg, s0, s1, d, rows=None):
    sl, rev = nat_slice(cfg, s0, s1, d)
    a = ap[:, sl] if rows is None else ap[rows, sl]
    return a[:, ::-1] if rev else a


def stage_vscan(p, cfg, G, io, l, kind):
    Tb, CTX = cfg.tb, cfg.ctx
    j = l // 2
    hgrn = kind == "hgrn"
    dk = 128 if hgrn else 64
    dv = 128
    NH = 4
    YT = G["YT"]
    p.open_scope()
    segs = [(0, CTX), (CTX, Tb)]
    identb = G["ident_bf"]
    cmask = G["cmask"]
    onesm = G["ones_mean"]
    pTrK = p.psum([128, 1024], BF16, "pTrK")
    pTrV = p.psum([128, 1024], BF16, "pTrV")
    pAtt = [p.psum([128, 512], F32, f"pAtt{i}") for i in range(1)]
    pO = [p.psum([128, 512], F32, f"pO{i}") for i in range(2)]
    pKV = [p.psum([128, 512], F32, f"pKV{i}") for i in range(2)]
    pX = p.psum([128, 512], F32, "pX")
    if hgrn:
        ne = (cfg.depth + 1) // 2
        lg = p.sbuf([128, 2, ne, NH], F32, "lb_logits")
        p.load(lg, lg.ap[:, :, :, :].rearrange("p a b c -> p (a b c)"), io["hg_lbT"], io["hg_lbT"].ap[:, :])
        p.act(lg, lg[:, :, :, :], lg, lg[:, :, :, :], AF.Exp)
        ssum = p.sbuf([128, 2, NH], F32, "lb_sum")
        p.copy(ssum, ssum[:, :, :], lg, lg[:, :, 0, :])
        for i in range(1, ne):
            p.tt(ssum, ssum[:, :, :], ssum, ssum[:, :, :], lg, lg[:, :, i, :], ALU.add)
        ssf = ssum.ap[:, :, :].rearrange("p a b -> p (a b)")
        p.op("dve", lambda e: e.reciprocal(ssf, ssf), reads=[ssum], writes=[ssum])
        lb = p.sbuf([128, 2, NH], F32, "lb")
        p.memset(lb, lb[:, :, :], 0.0)
        for i in range(1, j + 1):
            p.tt(lb, lb[:, :, :], lb, lb[:, :, :], lg, lg[:, :, i, :], ALU.add)
        p.tt(lb, lb[:, :, :], lb, lb[:, :, :], ssum, ssum[:, :, :], ALU.mult)
        oml = p.sbuf([128, 2, NH], F32, "oml")
        p.ts(oml, oml[:, :, :], lb, lb[:, :, :], -1.0, 1.0, ALU.mult, ALU.add)
        noml = p.sbuf([128, 2, NH], F32, "noml")
        p.ts(noml, noml[:, :, :], oml, oml[:, :, :], -1.0, None, ALU.mult)
        lbf = p.sbuf([128, 2, NH], F32, "lbf")
        p.ts(lbf, lbf[:, :, :], lb, lb[:, :, :], float(LB_FLOOR), None, ALU.max)
        gn = p.sbuf([128, 1], F32, "gn")
        p.load(gn, gn[:, :], io["hg_norm_gT"], io["hg_norm_gT"].ap[j])
        rows = dict(q=EV["q"], k=None, v=EV["i"], g=EV["g"], z=(EV["zf"], EV["zb"]))
        qscale = 128 ** -0.5
    else:
        gw = p.sbuf([16, 2, 256], F32, "gw")
        p.load(gw, gw[:, :, :], io["gla_gate_w"], io["gla_gate_w"].ap[j].rearrange("d r c -> r d c"))
        gw_bf = p.sbuf([16, 2, 256], BF16, "gw_bf")
        p.copy(gw_bf, gw_bf[:, :, :], gw, gw[:, :, :])
        gbT = p.sbuf([64, 2 * NH], F32, "gbT")
        p.load(gbT, gbT[:, :], io["gla_gate_bT"], io["gla_gate_bT"].ap[j])
        gn = p.sbuf([128, 1], F32, "gn")
        p.load(gn, gn[:, :], io["gla_norm_gT"], io["gla_norm_gT"].ap[j])
        rows = dict(q=OD["q"], k=OD["k"], v=OD["v"], g=OD["g"], z=(OD["lrf"], OD["lrb"]))
        qscale = 64 ** -0.5
    q_raw = p.sbuf([dk, Tb], F32, "q_raw")
    k_raw = None if hgrn else p.sbuf([dk, Tb], F32, "k_raw")
    v_raw = p.sbuf([dv, Tb], F32, "v_raw")
    g_raw = p.sbuf([dv, Tb], F32, "g_raw")
    z_raw = [p.sbuf([dk if hgrn else 16, Tb], F32, f"z_raw{i}") for i in range(2)]
    A1 = p.sbuf([dk, Tb], F32, "A1")
    A2 = p.sbuf([dk, Tb], F32, "A2")
    A3 = p.sbuf([dk, Tb], F32, "A3")
    A4 = p.sbuf([dk, Tb], F32, "A4")
    oacc = p.sbuf([dv, Tb], F32, "oacc")
    Qh = p.sbuf([dk, Tb], BF16, "Qh")
    Kh = p.sbuf([dk, Tb], BF16, "Kh")
    vb = p.sbuf([dv, Tb], BF16, "vb")
    lr_bf = None if hgrn else p.sbuf([16, Tb], BF16, "lr_bf")
    outb = p.sbuf([dv, Tb], BF16, "outb")
    Ktok = [p.sbuf([128, dk], BF16, f"Ktok{i}") for i in range(2)]
    Vtok = [p.sbuf([128, dv], BF16, f"Vtok{i}") for i in range(2)]
    Ktok3 = [p.sbuf([128, dk], BF16, f"Ktok3{i}") for i in range(2)]
    att_sb = [p.sbuf([128, 128], BF16, f"att{i}") for i in range(2)]
    S = p.sbuf([dk, dv], F32, "S")
    Stmp = p.sbuf([dk, dv], F32, "Stmp")
    Sbf = [p.sbuf([dk, dv], BF16, f"Sbf{i}") for i in range(2)]
    rmask = p.sbuf([128, Tb], F32, "rmask")
    p.load(rmask, rmask[:, :], io["rmask"], io["rmask"].ap[:, :])
    tabgen = peer_tables_gen(p, cfg, G, io, l)
    n_tile_iters = cfg.nb * NH * 2 * (Tb // 128)
    tab_per_iter = -(-(PEER_NKEYS * PEER_NKEYS // 128) // n_tile_iters)
    import os
    VCUT = int(os.environ.get("VCUT", "0"))
    if VCUT == 1:
        p.close_scope(); return
    nt = Tb // 128
    NCK = 128 // CH
    for b in range(cfg.nb):
        c0 = b * Tb
        for h in range(NH):
            p.load(q_raw, q_raw[:, :], YT, YT.ap[rows["q"] + h * dk:rows["q"] + (h + 1) * dk, c0:c0 + Tb], q="sp")
            if not hgrn:
                p.load(k_raw, k_raw[:, :], YT, YT.ap[rows["k"] + h * dk:rows["k"] + (h + 1) * dk, c0:c0 + Tb], q="act")
            p.load(v_raw, v_raw[:, :], YT, YT.ap[rows["v"] + h * dv:rows["v"] + (h + 1) * dv, c0:c0 + Tb], q="act")
            p.load(g_raw, g_raw[:, :], YT, YT.ap[rows["g"] + h * dv:rows["g"] + (h + 1) * dv, c0:c0 + Tb], q="sp")
            for d in range(2):
                zr = z_raw[d]
                if hgrn:
                    p.load(zr, zr[:, :], YT, YT.ap[rows["z"][d] + h * dk:rows["z"][d] + (h + 1) * dk, c0:c0 + Tb], q="sp")
                else:
                    p.load(zr, zr[:, :], YT, YT.ap[rows["z"][d]:rows["z"][d] + 16, c0:c0 + Tb], q="sp")
                if VCUT == 2:
                    p.close_scope(); return
                if hgrn:
                    for (s0, s1) in segs:
                        p.act(A1, A1[:, s0:s1], zr, nat_ap(zr.ap, cfg, s0, s1, d), AF.Sigmoid)
                    p.ts(A2, A2[:, :], A1, A1[:, :], oml[:, d, h:h + 1], lbf[:, d, h:h + 1], ALU.mult, ALU.add,
                         reads=[oml, lbf])
                    p.ts(A4, A4[:, :], A1, A1[:, :], noml[:, d, h:h + 1], oml[:, d, h:h + 1], ALU.mult, ALU.add,
                         reads=[oml, noml], eng="pool")
                    p.act(A2, A2[:, :], A2, A2[:, :], AF.Ln)
                    esc = 1.0
                else:
                    for (s0, s1) in segs:
                        p.copy(lr_bf, lr_bf[:, s0:s1], zr, nat_ap(zr.ap, cfg, s0, s1, d), eng="pool")
                    for (t0, t1) in blocks(Tb, 512):
                        p.mm(pX, pX[0:dk, 0:t1 - t0], gw_bf, gw_bf[:, d, h * dk:(h + 1) * dk], lr_bf, lr_bf[:, t0:t1])
                        p.act(A2, A2[:, t0:t1], pX, pX[0:dk, 0:t1 - t0], AF.Sigmoid,
                              bias=gbT[:, d * NH + h:d * NH + h + 1], reads=[gbT])
                    p.act(A2, A2[:, :], A2, A2[:, :], AF.Ln)
                    esc = 1.0 / 16.0
                p.op("dve", lambda e: e.tensor_tensor_scan(A3[:, :], rmask[0:dk, :], A2[:, :], 0.0, ALU.mult, ALU.add),
                     reads=[rmask, A2], writes=[A3])
                p.act(A1, A1[:, :], A3, A3[:, :], AF.Exp, scale=-esc)
                p.act(A3, A3[:, :], A3, A3[:, :], AF.Exp, scale=esc)
                if hgrn:
                    for (s0, s1) in segs:
                        p.act(A2, A2[:, s0:s1], q_raw, nat_ap(q_raw.ap, cfg, s0, s1, d), AF.Silu)
                    p.stt(Qh, Qh[:, :], A2, A2[:, :], float(qscale), A3, A3[:, :], ALU.mult, ALU.mult)
                    p.tt(Kh, Kh[:, :], A4, A4[:, :], A1, A1[:, :], ALU.mult, eng="pool")
                else:
                    for (s0, s1) in segs:
                        p.stt(Qh, Qh[:, s0:s1], q_raw, nat_ap(q_raw.ap, cfg, s0, s1, d), float(qscale),
                              A3, A3[:, s0:s1], ALU.mult, ALU.mult)
                        p.tt(Kh, Kh[:, s0:s1], k_raw, nat_ap(k_raw.ap, cfg, s0, s1, d), A1, A1[:, s0:s1], ALU.mult)
                for (s0, s1) in segs:
                    p.copy(vb, vb[:, s0:s1], v_raw, nat_ap(v_raw.ap, cfg, s0, s1, d), eng="pool")
                if VCUT == 3:
                    p.close_scope(); return
                p.memset(S, S[:, :], 0.0)
                p.memset(Sbf[0], Sbf[0][:, :], 0.0)
                cur = 0
                dprev = A3[:, 0:1]
                for ti in range(nt):
                    cs = slice(ti * 128, (ti + 1) * 128)
                    kt, vt = Ktok[ti % 2], Vtok[ti % 2]
                    kt3 = Ktok3[ti % 2]
                    p.transpose(pTrK, pTrK[:, 0:dk], Kh, Kh[:, cs], identb, identb[0:dk, 0:dk])
                    p.transpose(pTrV, pTrV[:, 0:dv], vb, vb[:, cs], identb, identb[:, :])
                    p.copy(kt, kt[:, :], pTrK, pTrK[:, 0:dk], eng="act")
                    p.copy(vt, vt[:, :], pTrV, pTrV[:, 0:dv], eng="dve")
                    p.ts(kt3, kt3[:, :], kt, kt[:, :], G["rm3"][:, 0:1], None, ALU.mult, reads=[G["rm3"]], eng="pool")
                    if VCUT == 4:
                        p.close_scope(); return
                    pa = pAtt[0]
                    p.mm(pa, pa[:, 0:128], Kh, Kh[:, cs], Qh, Qh[:, cs])
                    at = att_sb[ti % 2]
                    p.tt(at, at[:, :], pa, pa[:, 0:128], cmask, cmask[:, :], ALU.mult)
                    if VCUT == 5:
                        p.close_scope(); return
                    po = pO[ti % 2]
                    p.mm(po, po[0:dv, 0:128], vt, vt[:, :], at, at[:, :], start=True, stop=False)
                    for c in range(NCK):
                        cc = slice(ti * 128 + c * CH, ti * 128 + (c + 1) * CH)
                        p.mm(po, po[0:dv, c * CH:(c + 1) * CH], Sbf[cur], Sbf[cur][:, :], Qh, Qh[:, cc],
                             start=False, stop=(c == NCK - 1))
                        pk = pKV[c % 2]
                        if c * CH < 96:
                            p.mm(pk, pk[0:dk, 0:dv], kt, kt[c * CH:(c + 1) * CH, :], vt, vt[c * CH:(c + 1) * CH, :])
                        else:
                            p.mm(pk, pk[0:dk, 0:dv], kt3, kt3[64:128, :], vt, vt[64:128, :])
                        dcol = A3[:, ti * 128 + (c + 1) * CH - 1:ti * 128 + (c + 1) * CH]
                        p.stt(S, S[:, :], S, S[:, :], dprev, pk, pk[0:dk, 0:dv], ALU.mult, ALU.add, reads=[A3])
                        nxt = 1 - cur
                        p.act(Sbf[nxt], Sbf[nxt][:, :], S, S[:, :], AF.Identity, scale=dcol, reads=[A3])
                        cur = nxt
                        dprev = dcol
                    if VCUT == 6:
                        p.close_scope(); return
                    tabgen = drain_gen(tabgen, tab_per_iter)
                    sl, rev = nat_slice(cfg, ti * 128, (ti + 1) * 128, d)
                    if d == 0:
                        p.copy(oacc, oacc[:, sl], po, po[0:dv, 0:128], eng="act")
                    else:
                        oa = oacc.ap[:, sl][:, ::-1]
                        p.tt(oacc, oa, oacc, oa, po, po[0:dv, 0:128], ALU.add)
            if VCUT == 7:
                p.close_scope(); return
            if hgrn:
                p.act(g_raw, g_raw[:, :], g_raw, g_raw[:, :], AF.Sigmoid)
                p.tt(oacc, oacc[:, :], oacc, oacc[:, :], g_raw, g_raw[:, :], ALU.mult)
            else:
                p.act(g_raw, g_raw[:, :], g_raw, g_raw[:, :], AF.Silu)
            p.act(v_raw, v_raw[:, :], oacc, oacc[:, :], AF.Square)
            for (t0, t1) in blocks(Tb, 512):
                p.mm(pX, pX[:, 0:t1 - t0], onesm, onesm[:, :], v_raw, v_raw[:, t0:t1])
                p.act(v_raw, v_raw[:, t0:t1], pX, pX[:, 0:t1 - t0], AF.Ln, bias=float(NORM_EPS))
            p.act(v_raw, v_raw[:, :], v_raw, v_raw[:, :], AF.Exp, scale=-0.5)
            if hgrn:
                p.stt(outb, outb[:, :], oacc, oacc[:, :], gn[:, 0:1], v_raw, v_raw[:, :], ALU.mult, ALU.mult, reads=[gn])
            else:
                p.stt(oacc, oacc[:, :], oacc, oacc[:, :], gn[:, 0:1], v_raw, v_raw[:, :], ALU.mult, ALU.mult, reads=[gn])
                p.tt(outb, outb[:, :], oacc, oacc[:, :], g_raw, g_raw[:, :], ALU.mult)
            p.store(G["MIXT"], G["MIXT"].ap[h * dv:(h + 1) * dv, c0:c0 + Tb], outb, outb[:, :], q="pool")
    drain_gen(tabgen)
    p.close_scope()


def stage_attn(p, cfg, G, io, l):
    Tb, CTX, SEQ = cfg.tb, cfg.ctx, cfg.seq
    j = l // 2
    YT = G["YT"]
    p.open_scope()
    identb = G["ident_bf"]
    onesm = G["ones_mean"]
    cosT = p.sbuf([128, Tb], F32, "cosT")
    sinT = p.sbuf([128, Tb], F32, "sinT")
    p.load(cosT, cosT[:, :], io["cosT"], io["cosT"].ap[:, :])
    p.load(sinT, sinT[:, :], io["sinT"], io["sinT"].ap[:, :])
    onesb = p.sbuf([128, 128], BF16, "onesb")
    p.memset(onesb, onesb[:, :], 1.0)
    gq = p.sbuf([128, 4], F32, "gq")
    p.load(gq, gq[:, :], io["at_normT"], io["at_normT"].ap[j])
    gab = p.sbuf([128, 2], F32, "gab")
    p.act(gab, gab[:, 0:1], gq, gq[:, 0:1], AF.Abs)
    p.act(gab, gab[:, 1:2], gq, gq[:, 2:3], AF.Abs)
    pS = [p.psum([128, 512], F32, f"pS{i}") for i in range(3)]
    pOa = p.psum([128, 512], F32, "pOa")
    pSum = p.psum([128, 512], F32, "pSum")
    pM = p.psum([128, 512], F32, "pM")
    trs = [p.psum([128, 4, 256], BF16, f"pTrA{s}") for s in range(2)]
    ident = G["ident"]
    p.transpose(pM, pM[0:2, 0:128], gab, gab[:, 0:2], ident, ident[:, :])
    gmx = p.sbuf([2, 2], F32, "gmx")
    p.op("dve", lambda e: e.reduce_max(gmx[0:2, 0:1], pM[0:2, 0:128], axis=AX.X), reads=[pM], writes=[gmx])
    gmx2 = p.sbuf([2, 2], F32, "gmx2")
    p.memset(gmx2, gmx2[:, :], 0.0)
    p.tt(gmx2, gmx2[0:2, 0:2], ident, ident[0:2, 0:2], gmx, gmx.ap[0:2, 0:1].broadcast_to([2, 2]), ALU.mult)
    ones2 = p.sbuf([2, 128], F32, "ones2")
    p.memset(ones2, ones2[:, :], 1.0)
    p.mm(pM, pM[:, 0:2], ones2, ones2[:, :], gmx2, gmx2[:, :])
    nshift = p.sbuf([128, 1], F32, "nshift")
    p.copy(gab, gab[:, 0:2], pM, pM[:, 0:2])
    p.stt(nshift, nshift[:, 0:1], gab, gab[:, 0:1], float(-(128 ** 0.5)), gab, gab[:, 1:2], ALU.mult, ALU.mult)
    x_raw = p.sbuf([128, Tb], F32, "x_raw")
    xs_raw = p.sbuf([128, Tb], F32, "xs_raw")
    t1 = p.sbuf([128, Tb], F32, "t1")
    t2 = p.sbuf([128, Tb], F32, "t2")
    qr = p.sbuf([128, 4, Tb], BF16, "qr")
    kr = p.sbuf([128, 2, Tb], BF16, "kr")
    vbf = p.sbuf([128, Tb], BF16, "vbf")
    nkt = Tb // 128
    Vtok = p.sbuf([128, 2, nkt, 128], BF16, "Vtok")
    Pt = [p.sbuf([128, 512], BF16, f"Pt{i}") for i in range(3)]
    osb = p.sbuf([128, 512], F32, "osb")
    rs = p.sbuf([128, 512], F32, "rs")
    obf = [p.sbuf([128, 512], BF16, f"obf{i}") for i in range(2)]
    scale = 128 ** -0.5
    for b in range(cfg.nb):
        c0 = b * Tb
        heads = [("q", h, EV["aq"] + h * 128, EV["aqs"] + h * 128, 0) for h in range(4)] + \
                [("k", g, EV["ak"] + g * 128, EV["aks"] + g * 128, 2) for g in range(2)]
        for (kind, hi, r0, rs0, gc) in heads:
            p.load(x_raw, x_raw[:, :], YT, YT.ap[r0:r0 + 128, c0:c0 + Tb], q="sp")
            p.load(xs_raw, xs_raw[:, :], YT, YT.ap[rs0:rs0 + 128, c0:c0 + Tb], q="act")
            p.act(t1, t1[:, :], x_raw, x_raw[:, :], AF.Square)
            for (a0, a1) in blocks(Tb, 512):
                p.mm(pM, pM[:, 0:a1 - a0], onesm, onesm[:, :], t1, t1[:, a0:a1])
                p.act(t2, t2[:, a0:a1], pM, pM[:, 0:a1 - a0], AF.Ln, bias=float(NORM_EPS))
            p.act(t2, t2[:, :], t2, t2[:, :], AF.Exp, scale=-0.5)
            p.stt(t1, t1[:, :], x_raw, x_raw[:, :], gq[:, gc:gc + 1], cosT, cosT[:, :], ALU.mult, ALU.mult, reads=[gq])
            p.stt(x_raw, x_raw[:, :], xs_raw, xs_raw[:, :], gq[:, gc + 1:gc + 2], sinT, sinT[:, :], ALU.mult, ALU.mult,
                  reads=[gq])
            p.tt(t1, t1[:, :], t1, t1[:, :], x_raw, x_raw[:, :], ALU.add, eng="pool")
            dst = qr if kind == "q" else kr
            p.tt(dst, dst[:, hi, :], t1, t1[:, :], t2, t2[:, :], ALU.mult)
        for g in range(2):
            p.load(x_raw, x_raw[:, :], YT, YT.ap[EV["av"] + g * 128:EV["av"] + (g + 1) * 128, c0:c0 + Tb], q="sp")
            p.copy(vbf, vbf[:, :], x_raw, x_raw[:, :], eng="pool")
            for kt in range(nkt):
                tr = trs[kt % 2]
                p.transpose(tr, tr[:, 0, 0:128], vbf, vbf[:, kt * 128:(kt + 1) * 128], identb, identb[:, :])
                p.copy(Vtok, Vtok[:, g, kt, :], tr, tr[:, 0, 0:128], eng="act" if kt % 2 else "dve")
        it = 0
        for h in range(4):
            g = h // 2
            qblocks = [(a0, a1, 0, CTX // 128) for (a0, a1) in blocks(CTX, 512)] + \
                      [(CTX + a0, CTX + a1, 0, nkt) for (a0, a1) in blocks(SEQ, 512)]
            for (q0, q1, k0, k1) in qblocks:
                w = q1 - q0
                for kt in range(k0, k1):
                    ps = pS[it % 3]
                    pt = Pt[it % 3]
                    it += 1
                    p.mm(ps, ps[:, 0:w], kr, kr[:, g, kt * 128:(kt + 1) * 128], qr, qr[:, h, q0:q1])
                    p.act(pt, pt[:, 0:w], ps, ps[:, 0:w], AF.Exp, bias=nshift[:, 0:1], scale=float(scale), reads=[nshift])
                    p.mm(pOa, pOa[:, 0:w], Vtok, Vtok[:, g, kt, :], pt, pt[:, 0:w], start=(kt == k0), stop=(kt == k1 - 1))
                    p.mm(pSum, pSum[:, 0:w], onesb, onesb[:, :], pt, pt[:, 0:w], start=(kt == k0), stop=(kt == k1 - 1))
                p.op("dve", lambda e, w=w: e.reciprocal(rs[:, 0:w], pSum[:, 0:w]), reads=[pSum], writes=[rs])
                ob = obf[(it // 1) % 2]
                p.tt(ob, ob[:, 0:w], pOa, pOa[:, 0:w], rs, rs[:, 0:w], ALU.mult)
                p.store(G["MIXT"], G["MIXT"].ap[512 + h * 128:512 + (h + 1) * 128, c0 + q0:c0 + q1], ob, ob[:, 0:w], q="pool")
    p.close_scope()


NEG = -1.0e30

def peer_tables_gen(p, cfg, G, io, l):
    NE = PEER_NKEYS * PEER_NKEYS
    su = [p.sbuf([128, D], F32, f"tsu{i}") for i in range(2)]
    sv = [p.sbuf([128, D], F32, f"tsv{i}") for i in range(2)]
    ob = [p.sbuf([128, 2 * D], BF16, f"tob{i}") for i in range(2)]
    u_tab = io[f"peer_u{l}"].ap
    v_tab = io[f"peer_v{l}"].ap
    for rb in range(NE // 128):
        a, b_, o = su[rb % 2], sv[rb % 2], ob[rb % 2]
        p.load(a, a[:, :], G["ext"], u_tab[rb * 128:(rb + 1) * 128, :], q="sp")
        p.load(b_, b_[:, :], G["ext"], v_tab[rb * 128:(rb + 1) * 128, :], q="act")
        p.copy(o, o[:, 0:D], a, a[:, :], eng="act")
        p.copy(o, o[:, D:2 * D], b_, b_[:, :], eng="dve" if rb % 2 else "pool")
        p.store(G["UVB"], G["UVB"].ap[rb * 128:(rb + 1) * 128, :], o, o[:, :], q="sp")
        yield


def drain_gen(g, n=None):
    if g is None:
        return None
    try:
        if n is None:
            while True:
                next(g)
        for _ in range(n):
            next(g)
    except StopIteration:
        return None
    return g


def stage_peer(p, cfg, G, io, l):
    import os
    T = cfg.T
    H8, NK, TK = PEER_HEADS, PEER_NKEYS, PEER_TOPK
    NQ = H8 * 2 * NK
    p.open_scope()
    ident = G["ident"]
    wq_bf = p.sbuf([128, KC, NQ], BF16, "wq_bf")
    p.open_scope()
    wst = [p.sbuf([128, NQ], F32, f"wqst{i}") for i in range(2)]
    for kc in range(KC):
        st = wst[kc % 2]
        p.load(st, st[:, :], G["ext"], io["peer_wq"].ap[l, kc * 128:(kc + 1) * 128, :], q="sp" if kc % 2 else "act")
        p.copy(wq_bf, wq_bf[:, kc, :], st, st[:, :], eng="pool" if kc % 2 else "dve")
    sk = p.sbuf([128, 16 * NK], F32, "sk")
    p.load(sk, sk[:, :], G["ext"], io["peer_subkT"].ap[l])
    p.close_scope()
    sk_bf = p.sbuf([128, 16, NK], BF16, "sk_bf")
    p.open_scope()
    sk2 = p.sbuf([128, 16 * NK], F32, "sk2")
    p.load(sk2, sk2[:, :], G["ext"], io["peer_subkT"].ap[l])
    p.copy(sk_bf, sk_bf.ap[:, :, :].rearrange("p a b -> p (a b)"), sk2, sk2[:, :])
    p.close_scope()
    scb, shb = [], []
    for r in range(cfg.R):
        a = p.sbuf([128, D], F32, f"sc2b{r}")
        b_ = p.sbuf([128, D], F32, f"sh2b{r}")
        p.load(a, a[:, :], G["modtok"], G["modtok"].ap[l, r:r + 1, 4 * D:5 * D].partition_broadcast(128))
        p.load(b_, b_[:, :], G["modtok"], G["modtok"].ap[l, r:r + 1, 3 * D:4 * D].partition_broadcast(128))
        p.ts(a, a[:, :], a, a[:, :], 1.0, None, ALU.add)
        scb.append(a)
        shb.append(b_)
    TB = 2
    hst = [p.sbuf([128, D], F32, f"phst{i}") for i in range(2)]
    hm = p.sbuf([128, 4, D], F32, "hm_tok")
    hmT = p.sbuf([128, KC, TB * 128], BF16, "hmT")
    qT = p.sbuf([128, 16, TB * 128], BF16, "qT")
    pT = [p.psum([128, 4, 128], F32, "ppT")] * 2
    pQ = [p.psum([128, 512], F32, "ppQ")] * 2
    pS = [p.psum([128, 4, 128], F32, f"ppS{i}") for i in range(2)] * 2
    pHM = p.psum([128, 1024], F32, "pHM")
    pACC = [p.psum([128, 512], F32, f"pACC{i}") for i in range(2)]
    s_sb = p.sbuf([128, 16, NK], F32, "s_sb")
    s_wk = p.sbuf([128, NK], F32, "s_wk")
    ts16 = p.sbuf([128, 16, TK], F32, "ts16")
    ti16 = p.sbuf([128, 16, TK], U32, "ti16")
    ti16f = p.sbuf([128, 16, TK], F32, "ti16f")
    cand = p.sbuf([128, H8, TK * TK], F32, "cand")
    cwk = p.sbuf([128, TK * TK], F32, "cwk")
    eid = p.sbuf([128, H8, TK * TK], F32, "eid")
    best = p.sbuf([128, H8, TK], F32, "best")
    gate = p.sbuf([128, H8, TK], F32, "gate")
    gsum = p.sbuf([128, H8], F32, "gsum")
    idxf = p.sbuf([128, H8 * TK], F32, "idxf")
    idxu = p.sbuf([128, H8 * TK], U32, "idxu")
    aval = p.sbuf([128, H8 * TK], F32, "aval")
    coef = p.sbuf([128, H8 * TK], F32, "coef")
    tmpa = p.sbuf([128, H8 * TK], F32, "tmpa")
    junk = p.sbuf([128, D], BF16, "junk")
    NUB = int(os.environ.get("NUB", "10"))
    GRP = 4
    ub = [p.sbuf([128, 2 * D], BF16, f"ub{i}") for i in range(NUB)]
    dgs = [p.sbuf([128, 128], BF16, f"dg{i}") for i in range(8)]
    osb = p.sbuf([128, D], F32, "posb")
    identb = G["ident_bf"]
    UVB = G["UVB"]
    nslot = H8 * TK
    ntiles = T // 128
    idxus = [idxu, p.sbuf([128, H8 * TK], U32, "idxu_b")]
    gates = [gate, p.sbuf([128, H8, TK], F32, "gate_b")]
    posu = p.sbuf([128, H8, TK], U32, "posu")
    xg = p.sbuf([128, H8 * TK], F32, "xg")
    abu = p.sbuf([128, 2, H8 * TK], U32, "abu")
    abf = p.sbuf([128, 2, H8 * TK], F32, "abf")
    ijf = p.sbuf([128, 2, H8 * TK], F32, "ijf")
    iota16 = p.sbuf([128, TK], F32, "iota16")
    p.load(iota16, iota16[:, :], G["ext"], io["iota16"].ap[:, :])

    def producer(ti):
        t0 = (ti // TB) * TB * 128
        t1 = min(t0 + TB * 128, T)
        ntl = (t1 - t0) // 128
        w = t1 - t0
        tl = ti % TB
        if tl == 0:
            for tl2 in range(ntl):
                tj = ti + tl2
                r = cfg.seg_of_tile(tj)
                hs = hst[tj % 2]
                slot = tj % 4
                p.load(hs, hs[:, :], G["H"], G["H"].ap[tj * 128:(tj + 1) * 128, :], q="sp")
                p.tt(hm, hm[:, slot, :], hs, hs[:, :], scb[r], scb[r][:, :], ALU.mult)
                p.tt(hm, hm[:, slot, :], hm, hm[:, slot, :], shb[r], shb[r][:, :], ALU.add)
                yield
                for half in range(2):
                    pt = pT[half]
                    for kk in range(4):
                        kc = half * 4 + kk
                        p.transpose(pt, pt[:, kk, :], hm, hm[:, slot, kc * 128:(kc + 1) * 128], ident, ident[:, :])
                    p.copy(hmT, hmT[:, half * 4:(half + 1) * 4, tl2 * 128:(tl2 + 1) * 128], pt, pt[:, :, :],
                           eng="act" if half else "dve")
                    yield
            for hsx in range(16):
                pq = pQ[hsx % 2]
                for kc in range(KC):
                    p.mm(pq, pq[:, 0:w], wq_bf, wq_bf[:, kc, hsx * NK:(hsx + 1) * NK], hmT, hmT[:, kc, 0:w],
                         start=(kc == 0), stop=(kc == KC - 1))
                p.copy(qT, qT[:, hsx, 0:w], pq, pq[:, 0:w], eng="act" if hsx % 2 else "dve")
                yield
        idxu_, gate_ = idxus[ti % 2], gates[ti % 2]
        for q4 in range(4):
            ps = pS[q4]
            for k4 in range(4):
                hsx = q4 * 4 + k4
                p.mm(ps, ps[:, k4, :], qT, qT[:, hsx, tl * 128:(tl + 1) * 128], sk_bf, sk_bf[:, hsx, :])
            p.copy(s_sb, s_sb[:, q4 * 4:(q4 + 1) * 4, :], ps, ps[:, :, :], eng="act" if q4 % 2 else "dve")
            yield
        for hsx in range(16):
            src_ = s_sb[:, hsx, :]
            p.op("dve", lambda e, hsx=hsx, src_=src_: e.max(out=ts16[:, hsx, 0:8], in_=src_), reads=[s_sb], writes=[ts16])
            p.op("dve", lambda e, hsx=hsx, src_=src_: e.max_index(ti16[:, hsx, 0:8], ts16[:, hsx, 0:8], src_),
                 reads=[s_sb, ts16], writes=[ti16])
            p.op("dve", lambda e, hsx=hsx, src_=src_: e.match_replace(out=s_wk[:, :], in_to_replace=ts16[:, hsx, 0:8],
                                                                      in_values=src_, imm_value=NEG),
                 reads=[s_sb, ts16], writes=[s_wk])
            p.op("dve", lambda e, hsx=hsx: e.max(out=ts16[:, hsx, 8:16], in_=s_wk[:, :]), reads=[s_wk], writes=[ts16])
            p.op("dve", lambda e, hsx=hsx: e.max_index(ti16[:, hsx, 8:16], ts16[:, hsx, 8:16], s_wk[:, :]),
                 reads=[s_wk, ts16], writes=[ti16])
            yield
        p.copy(ti16f, ti16f.ap[:, :, :].rearrange("p a b -> p (a b)"), ti16, ti16.ap[:, :, :].rearrange("p a b -> p (a b)"))
        ts4 = ts16.ap[:, :, :].rearrange("p (h s) k -> p h s k", s=2)
        ti4 = ti16f.ap[:, :, :].rearrange("p (h s) k -> p h s k", s=2)
        c4 = cand.ap[:, :, :].rearrange("p h (a b) -> p h a b", a=TK)
        e4 = eid.ap[:, :, :].rearrange("p h (a b) -> p h a b", a=TK)
        for h in range(H8):
            a_b = ts4[:, h, 0, :].unsqueeze(2).broadcast_to([128, TK, TK])
            b_b = ts4[:, h, 1, :].unsqueeze(1).broadcast_to([128, TK, TK])
            p.tt(cand, c4[:, h, :, :], ts16, a_b, ts16, b_b, ALU.add)
            yield
        for h in range(H8):
            src_ = cand[:, h, :]
            p.op("dve", lambda e, h=h, src_=src_: e.max(out=best[:, h, 0:8], in_=src_), reads=[cand], writes=[best])
            p.op("dve", lambda e, h=h, src_=src_: e.match_replace(out=cwk[:, :], in_to_replace=best[:, h, 0:8],
                                                                  in_values=src_, imm_value=NEG),
                 reads=[cand, best], writes=[cwk])
            p.op("dve", lambda e, h=h, src_=src_: e.max_index(posu[:, h, 0:8], best[:, h, 0:8], src_),
                 reads=[cand, best], writes=[posu])
            p.op("dve", lambda e, h=h: e.max(out=best[:, h, 8:16], in_=cwk[:, :]), reads=[cwk], writes=[best])
            p.op("dve", lambda e, h=h: e.max_index(posu[:, h, 8:16], best[:, h, 8:16], cwk[:, :]),
                 reads=[cwk, best], writes=[posu])
            yield
        mx_b = best.ap[:, :, 0:1].broadcast_to([128, H8, TK])
        p.tt(gate_, gate_[:, :, :], best, best[:, :, :], best, mx_b, ALU.subtract)
        p.act(gate_, gate_[:, :, :], gate_, gate_[:, :, :], AF.Exp)
        p.op("dve", lambda e: e.tensor_reduce(out=gsum[:, :], in_=gate_[:, :, :], axis=AX.X, op=ALU.add),
             reads=[gate_], writes=[gsum])
        p.op("dve", lambda e: e.reciprocal(gsum[:, :], gsum[:, :]), reads=[gsum], writes=[gsum])
        p.tt(gate_, gate_[:, :, :], gate_, gate_[:, :, :], gsum, gsum.ap[:, :].unsqueeze(2).broadcast_to([128, H8, TK]), ALU.mult)
        yield
        pflat = posu.ap[:, :, :].rearrange("p a b -> p (a b)")
        p.op("dve", lambda e: e.tensor_single_scalar(abu[:, 0, :], pflat, 4, op=ALU.logical_shift_right),
             reads=[posu], writes=[abu])
        p.op("dve", lambda e: e.tensor_single_scalar(abu[:, 1, :], pflat, 15, op=ALU.bitwise_and),
             reads=[posu], writes=[abu])
        p.copy(abf, abf.ap[:, :, :].rearrange("p a b -> p (a b)"), abu, abu.ap[:, :, :].rearrange("p a b -> p (a b)"))
        yield
        oh4 = eid.ap[:, :, :].rearrange("p h (k a) -> p h k a", k=TK)
        oh3 = eid.ap[:, :, :].rearrange("p h (k a) -> p (h k) a", k=TK)
        io16 = iota16.ap[:, :].unsqueeze(1).unsqueeze(1).broadcast_to([128, H8, TK, TK])
        for half in range(2):
            sel_ = abf.ap[:, half, :].rearrange("p (h k) -> p h k", h=H8).unsqueeze(3).broadcast_to([128, H8, TK, TK])
            tab_ = ti4[:, :, half, :].unsqueeze(2).broadcast_to([128, H8, TK, TK])
            p.tt(eid, oh4, abf, sel_, iota16, io16, ALU.is_equal)
            p.tt(eid, oh4, eid, oh4, ti16f, tab_, ALU.mult)
            yield
            p.op("dve", lambda e, half=half: e.tensor_reduce(out=ijf[:, half, :], in_=oh3, axis=AX.X, op=ALU.add),
                 reads=[eid], writes=[ijf])
            yield
        p.stt(idxf, idxf[:, :], ijf, ijf[:, 0, :], float(NK), ijf, ijf[:, 1, :], ALU.mult, ALU.add)
        p.ts(idxf, idxf[:, :], idxf, idxf[:, :], 0.0, float(NK * NK - 1), ALU.max, ALU.min)
        p.copy(idxu_, idxu_[:, :], idxf, idxf[:, :])
        yield

    def drain(g, n=None):
        if g is None:
            return None
        try:
            if n is None:
                while True:
                    next(g)
            for _ in range(n):
                next(g)
        except StopIteration:
            return None
        return g

    gi = 0
    drain(producer(0))
    for ti in range(ntiles):
        nxt = producer(ti + 1) if ti + 1 < ntiles else None
        idxu_, gate_ = idxus[ti % 2], gates[ti % 2]
        slot = ti % 4
        for hf in range(2):
            p.mm(pHM, pHM[:, hf * 512:(hf + 1) * 512], ident, ident[:, :], hm, hm[:, slot, hf * 512:(hf + 1) * 512])
        gflat = gate_.ap[:, :, :].rearrange("p a b -> p (a b)")
        for s0 in range(0, nslot, GRP):
            bufs = []
            for s in range(s0, s0 + GRP):
                u = ub[gi % NUB]
                gi += 1
                bufs.append(u)
                p.dma("pool", u, u[:, :], UVB, None, extra_reads=[idxu_],
                      fn=lambda e, u=u, s=s, idxu_=idxu_: e.indirect_dma_start(
                          out=u[:, :], out_offset=None, in_=UVB.ap,
                          in_offset=bass.IndirectOffsetOnAxis(ap=idxu_[:, s:s + 1], axis=0)))
                p.stt(junk, junk[:, :], u, u[:, 0:D], 1.0, pHM, pHM[:, :], ALU.mult, ALU.mult,
                      accum=aval[:, s:s + 1], accum_b=aval)
            sl = slice(s0, s0 + GRP)
            p.stt(tmpa, tmpa[:, sl], aval, aval[:, sl], 0.044715, aval, aval[:, sl], ALU.mult, ALU.mult)
            p.stt(tmpa, tmpa[:, sl], tmpa, tmpa[:, sl], 1.0, aval, aval[:, sl], ALU.add, ALU.mult)
            p.act(tmpa, tmpa[:, sl], tmpa, tmpa[:, sl], AF.Sigmoid, scale=float(2.0 * (2.0 / np.pi) ** 0.5))
            p.tt(xg, xg[:, sl], aval, aval[:, sl], gate_, gflat[:, sl], ALU.mult)
            p.tt(coef, coef[:, sl], tmpa, tmpa[:, sl], xg, xg[:, sl], ALU.mult)
            for k_, s in enumerate(range(s0, s0 + GRP)):
                u = bufs[k_]
                dg = dgs[s % 8]
                p.act(dg, dg[:, :], identb, identb[:, :], AF.Identity, scale=coef[:, s:s + 1], reads=[coef])
                for hf in range(2):
                    p.mm(pACC[hf], pACC[hf][:, :], dg, dg[:, :], u, u[:, D + hf * 512:D + (hf + 1) * 512],
                         start=(s == 0), stop=(s == nslot - 1))
            nxt = drain(nxt, 4)
        drain(nxt)
        p.copy(osb, osb[:, 0:512], pACC[0], pACC[0][:, :], eng="act")
        p.copy(osb, osb[:, 512:1024], pACC[1], pACC[1][:, :], eng="dve")
        p.store(G["PEERO"], G["PEERO"].ap[ti * 128:(ti + 1) * 128, :], osb, osb[:, :], q="sp")
    p.close_scope()


def stage_ssd(p, cfg, G, io, l):
    Tb, CTX = cfg.tb, cfg.ctx
    j = l // 2
    YT = G["YT"]
    XSF = G["XSF"]
    YPRE = G["YPRE"]
    nt = Tb // 128
    segs = [(0, CTX), (CTX, Tb)]
    p.open_scope()
    identb, ident = G["ident_bf"], G["ident"]
    cw = p.sbuf([128, 8, 5], F32, "convw")
    p.load(cw, cw.ap[:, :, :].rearrange("p a b -> p (a b)"), G["ext"], io["ssd_conv_wT"].ap[j])
    cb = p.sbuf([128, 8], F32, "convb")
    p.load(cb, cb[:, :], G["ext"], io["ssd_conv_bT"].ap[j])
    dtb = p.sbuf([8, 2], F32, "dtb")
    p.load(dtb, dtb[:, :], G["ext"], io["ssd_dt_biasT"].ap[j])
    nA = p.sbuf([8, 2], F32, "nA")
    p.load(nA, nA[:, :], G["ext"], io["ssd_a_logT"].ap[j])
    p.act(nA, nA[:, :], nA, nA[:, :], AF.Exp)
    p.ts(nA, nA[:, :], nA, nA[:, :], -1.0, None, ALU.mult)
    dsk = p.sbuf([128, 4], F32, "dskip")
    p.load(dsk, dsk[:, :], G["ext"], io["ssd_dT"].ap[j])
    gnm = p.sbuf([128, 4], F32, "ssd_g")
    p.load(gnm, gnm[:, :], G["ext"], io["ssd_norm_gT"].ap[j])
    sel = p.sbuf([8, 8, 128], F32, "sel")
    p.load(sel, sel.ap[:, :, :].rearrange("p a b -> p (a b)"), G["ext"], io["selT"].ap[:, :])
    mbias = p.sbuf([128, 128], F32, "mbias")
    p.load(mbias, mbias[:, :], G["ext"], io["mbias"].ap[:, :])
    rm128 = p.sbuf([8, Tb], F32, "rm128")
    p.load(rm128, rm128[:, :], G["ext"], io["rmask128"].ap[:, :])
    ones256 = p.sbuf([128, 128], F32, "ones256")
    p.memset(ones256, ones256[:, :], 1.0 / 256.0)
    pTrX = p.psum([128, 1024], BF16, "pTrX")
    pTrB = p.psum([128, 1024], BF16, "pTrB")
    pTrD = p.psum([128, 512], F32, "pTrD")
    pCR = p.psum([128, 2, 256], F32, "pCR")
    pSc = p.psum([128, 512], F32, "pSc")
    pY = p.psum([128, 512], F32, "pY")
    pKV = p.psum([128, 512], F32, "pKV")
    pN = p.psum([128, 512], F32, "pN")
    XCb = p.sbuf([128, 8, Tb], BF16, "XCb")
    raw = [p.sbuf([128, Tb], F32, f"sraw{i}") for i in range(2)]
    cacc = p.sbuf([128, Tb], F32, "cacc")
    dtT = [p.sbuf([8, Tb], F32, f"dtT{d}") for d in range(2)]
    cumT = [p.sbuf([8, Tb], F32, f"cumT{d}") for d in range(2)]
    t8a = p.sbuf([8, Tb], F32, "t8a")
    t8b = p.sbuf([8, Tb], F32, "t8b")
    Xd = p.sbuf([128, 3, Tb], BF16, "Xd")
    yacc = p.sbuf([128, Tb], F32, "yacc")
    xs_tok = [p.sbuf([128, 128], BF16, f"xs_tok{i}") for i in range(2)]
    B_tok = [p.sbuf([128, 128], BF16, f"B_tok{i}") for i in range(2)]
    dc_tok = [p.sbuf([128, 16], F32, f"dc_tok{i}") for i in range(2)]
    crow = [p.sbuf([128, 2, 128], F32, f"crow{i}") for i in range(2)]
    sc_sb = [p.sbuf([128, 128], F32, f"sc_sb{i}") for i in range(2)]
    D1 = [p.sbuf([128, 128], F32, f"D1{i}") for i in range(2)]
    attT = [p.sbuf([128, 128], BF16, f"attT{i}") for i in range(2)]
    ec = [p.sbuf([128, 128], F32, f"ec{i}") for i in range(2)]
    Chat = [p.sbuf([128, 128], BF16, f"Chat{i}") for i in range(2)]
    w2 = p.sbuf([128, 2], F32, "w2")
    el2 = p.sbuf([128, 2], F32, "el2")
    xhat = [p.sbuf([128, 2, 64], BF16, f"xhat{i}") for i in range(2)]
    S2 = p.sbuf([128, 2, 64], F32, "S2")
    Sbf = [p.sbuf([128, 2, 64], BF16, f"S2bf{i}") for i in range(2)]
    obf = p.sbuf([128, Tb], BF16, "ssd_obf")
    for b in range(cfg.nb):
        c0 = b * Tb
        for ch in range(8):
            r0 = OD["xs"] + ch * 128
            rw = raw[ch % 2]
            p.load(rw, rw[:, :], YT, YT.ap[r0:r0 + 128, c0:c0 + Tb], q="sp" if ch % 2 else "act")
            for (s0, s1) in segs:
                p.ts(cacc, cacc[:, s0:s1], rw, rw[:, s0:s1], cw[:, ch, 2:3], None, ALU.mult, reads=[cw])
                for k in (0, 1, 3, 4):
                    sh = k - 2
                    o0, o1 = max(s0, s0 - sh), min(s1, s1 - sh)
                    p.stt(cacc, cacc[:, o0:o1], rw, rw[:, o0 + sh:o1 + sh], cw[:, ch, k:k + 1], cacc, cacc[:, o0:o1],
                          ALU.mult, ALU.add, reads=[cw])
            if ch < 4:
                p.act(cacc, cacc[:, :], cacc, cacc[:, :], AF.Silu, bias=cb[:, ch:ch + 1], reads=[cb])
                p.copy(XCb, XCb[:, ch, :], cacc, cacc[:, :], eng="pool")
                p.store(XSF, XSF.ap[ch * 128:(ch + 1) * 128, c0:c0 + Tb], cacc, cacc[:, :], q="sp")
            else:
                p.act(XCb, XCb[:, ch, :], cacc, cacc[:, :], AF.Silu, bias=cb[:, ch:ch + 1], reads=[cb])
        for d in range(2):
            r0 = OD["dtf"] if d == 0 else OD["dtb"]
            p.load(t8a, t8a[:, :], YT, YT.ap[r0:r0 + 8, c0:c0 + Tb], q="sp")
            for (s0, s1) in segs:
                p.ts(t8b, t8b[:, s0:s1], t8a, nat_ap(t8a.ap, cfg, s0, s1, d), dtb[:, d:d + 1], None, ALU.add, reads=[dtb])
            p.act(t8a, t8a[:, :], t8b, t8b[:, :], AF.Abs)
            p.act(t8a, t8a[:, :], t8a, t8a[:, :], AF.Exp, scale=-1.0)
            p.act(t8a, t8a[:, :], t8a, t8a[:, :], AF.Ln, bias=1.0)
            p.ts(t8b, t8b[:, :], t8b, t8b[:, :], 0.0, None, ALU.max)
            p.tt(dtT[d], dtT[d][:, :], t8a, t8a[:, :], t8b, t8b[:, :], ALU.add)
            p.ts(t8a, t8a[:, :], dtT[d], dtT[d][:, :], nA[:, d:d + 1], None, ALU.mult, reads=[nA])
            p.op("dve", lambda e, d=d: e.tensor_tensor_scan(cumT[d][:, :], rm128[:, :], t8a[:, :], 0.0, ALU.mult, ALU.add),
                 reads=[rm128, t8a], writes=[cumT[d]])
        for cc in range(4):
            g = cc // 2
            for d in range(2):
                for (s0, s1) in segs:
                    for k_, src_ch in enumerate((cc, 4 + g, 6 + g)):
                        p.copy(Xd, Xd[:, k_, s0:s1], XCb, nat_ap(XCb.ap[:, src_ch, :], cfg, s0, s1, d),
                               eng=("pool", "act", "dve")[k_])
                p.memset(S2, S2.ap[:, :, :].rearrange("p a b -> p (a b)"), 0.0)
                p.memset(Sbf[0], Sbf[0].ap[:, :, :].rearrange("p a b -> p (a b)"), 0.0)
                cur = 0
                for ti in range(nt):
                    cs = slice(ti * 128, (ti + 1) * 128)
                    i2 = ti % 2
                    p.transpose(pTrX, pTrX[:, 0:128], Xd, Xd[:, 0, cs], identb, identb[:, :])
                    p.transpose(pTrB, pTrB[:, 0:128], Xd, Xd[:, 1, cs], identb, identb[:, :])
                    p.copy(xs_tok[i2], xs_tok[i2][:, :], pTrX, pTrX[:, 0:128], eng="act")
                    p.copy(B_tok[i2], B_tok[i2][:, :], pTrB, pTrB[:, 0:128], eng="dve")
                    p.transpose(pTrD, pTrD[:, 0:8], dtT[d], dtT[d][:, cs], ident, ident[0:8, 0:8])
                    p.transpose(pTrD, pTrD[:, 8:16], cumT[d], cumT[d][:, cs], ident, ident[0:8, 0:8])
                    dc = dc_tok[i2]
                    p.copy(dc, dc[:, :], pTrD, pTrD[:, 0:16], eng="dve")
                    for hh in range(2):
                        p.mm(pCR, pCR[:, hh, 0:128], sel, sel[:, 2 * cc + hh, :], cumT[d], cumT[d][:, cs])
                    cr = crow[i2]
                    p.copy(cr, cr[:, :, :], pCR, pCR[:, :, 0:128], eng="act")
                    p.mm(pSc, pSc[:, 0:128], Xd, Xd[:, 1, cs], Xd, Xd[:, 2, cs])
                    sc = sc_sb[i2]
                    p.copy(sc, sc[:, :], pSc, pSc[:, 0:128], eng="dve")
                    for hh in range(2):
                        h = 2 * cc + hh
                        p.stt(D1[hh], D1[hh][:, :], cr, cr[:, hh, :], dc[:, 8 + h:9 + h], mbias, mbias[:, :],
                              ALU.subtract, ALU.add, reads=[dc])
                        p.act(D1[hh], D1[hh][:, :], D1[hh], D1[hh][:, :], AF.Exp)
                        p.stt(attT[hh], attT[hh][:, :], D1[hh], D1[hh][:, :], dc[:, h:h + 1], sc, sc[:, :],
                              ALU.mult, ALU.mult, reads=[dc])
                        p.act(ec[hh], ec[hh][:, :], cr, cr[:, hh, :], AF.Exp)
                        p.tt(Chat[hh], Chat[hh][:, :], Xd, Xd[:, 2, cs], ec[hh], ec[hh][:, :], ALU.mult, eng="pool")
                        p.mm(pY, pY[hh * 64:(hh + 1) * 64, 0:128], xs_tok[i2], xs_tok[i2][:, hh * 64:(hh + 1) * 64],
                             attT[hh], attT[hh][:, :], start=True, stop=False)
                        p.mm(pY, pY[hh * 64:(hh + 1) * 64, 0:128], Sbf[cur], Sbf[cur][:, hh, :],
                             Chat[hh], Chat[hh][:, :], start=False, stop=True)
                    lastb = cr[:, :, 127]
                    p.tt(w2, w2[:, :], cr, lastb, dc, dc[:, 8 + 2 * cc:10 + 2 * cc], ALU.subtract)
                    p.act(w2, w2[:, :], w2, w2[:, :], AF.Exp)
                    p.tt(w2, w2[:, :], w2, w2[:, :], dc, dc[:, 2 * cc:2 * cc + 2], ALU.mult)
                    p.act(el2, el2[:, :], cr, lastb, AF.Exp)
                    xh = xhat[i2]
                    p.tt(xh, xh[:, :, :], xs_tok[i2], xs_tok[i2].ap[:, :].rearrange("p (a b) -> p a b", a=2),
                         w2, w2.ap[:, :].unsqueeze(2).broadcast_to([128, 2, 64]), ALU.mult)
                    p.mm(pKV, pKV[:, 0:128], B_tok[i2], B_tok[i2][:, :], xh, xh.ap[:, :, :].rearrange("p a b -> p (a b)"))
                    p.tt(S2, S2[:, :, :], S2, S2[:, :, :], el2, el2.ap[:, :].unsqueeze(2).broadcast_to([128, 2, 64]), ALU.mult)
                    S2f = S2.ap[:, :, :].rearrange("p a b -> p (a b)")
                    p.tt(S2, S2f, S2, S2f, pKV, pKV[:, 0:128], ALU.add)
                    nxt = 1 - cur
                    p.copy(Sbf[nxt], Sbf[nxt].ap[:, :, :].rearrange("p a b -> p (a b)"), S2, S2f, eng="act")
                    cur = nxt
                    sl, rev = nat_slice(cfg, ti * 128, (ti + 1) * 128, d)
                    if d == 0:
                        p.copy(yacc, yacc[:, sl], pY, pY[:, 0:128], eng="act")
                    else:
                        ya = yacc.ap[:, sl][:, ::-1]
                        p.tt(yacc, ya, yacc, ya, pY, pY[:, 0:128], ALU.add)
            p.load(raw[0], raw[0][:, :], XSF, XSF.ap[cc * 128:(cc + 1) * 128, c0:c0 + Tb], q="sp")
            p.load(raw[1], raw[1][:, :], YT, YT.ap[OD["z"] + cc * 128:OD["z"] + (cc + 1) * 128, c0:c0 + Tb], q="act")
            p.stt(yacc, yacc[:, :], raw[0], raw[0][:, :], dsk[:, cc:cc + 1], yacc, yacc[:, :], ALU.mult, ALU.add, reads=[dsk])
            p.act(raw[1], raw[1][:, :], raw[1], raw[1][:, :], AF.Silu)
            p.tt(yacc, yacc[:, :], yacc, yacc[:, :], raw[1], raw[1][:, :], ALU.mult)
            p.store(YPRE, YPRE.ap[cc * 128:(cc + 1) * 128, c0:c0 + Tb], yacc, yacc[:, :], q="sp")
        for g in range(2):
            for k_ in range(2):
                p.load(raw[k_], raw[k_][:, :], YPRE, YPRE.ap[(2 * g + k_) * 128:(2 * g + k_ + 1) * 128, c0:c0 + Tb],
                       q="sp" if k_ else "act")
            p.act(cacc, cacc[:, :], raw[0], raw[0][:, :], AF.Square)
            p.act(yacc, yacc[:, :], raw[1], raw[1][:, :], AF.Square)
            for (a0, a1) in blocks(Tb, 512):
                p.mm(pN, pN[:, 0:a1 - a0], ones256, ones256[:, :], cacc, cacc[:, a0:a1], start=True, stop=False)
                p.mm(pN, pN[:, 0:a1 - a0], ones256, ones256[:, :], yacc, yacc[:, a0:a1], start=False, stop=True)
                p.act(cacc, cacc[:, a0:a1], pN, pN[:, 0:a1 - a0], AF.Ln, bias=float(NORM_EPS))
            p.act(cacc, cacc[:, :], cacc, cacc[:, :], AF.Exp, scale=-0.5)
            for k_ in range(2):
                p.stt(obf, obf[:, :], raw[k_], raw[k_][:, :], gnm[:, 2 * g + k_:2 * g + k_ + 1], cacc, cacc[:, :],
                      ALU.mult, ALU.mult, reads=[gnm])
                p.store(G["MIXT"], G["MIXT"].ap[512 + (2 * g + k_) * 128:512 + (2 * g + k_ + 1) * 128, c0:c0 + Tb],
                        obf, obf[:, :], q="pool")
    p.close_scope()


def kernel(**inputs):
    x = np.asarray(inputs["x"])
    B, SEQ, _ = x.shape
    CTX = np.asarray(inputs["ctx"]).shape[1]
    depth = np.asarray(inputs["mod_w"]).shape[0]
    ncores = NCORES if B % NCORES == 0 else 1
    cfg = Cfg(B // ncores, SEQ, CTX, depth)
    p = build_program(cfg)
    nc = p.finish()
    maps = prep_inputs(inputs, cfg, ncores)
    res = run_bass_kernel_spmd(nc, maps, core_ids=list(range(ncores))).results
    out = np.empty((B, SEQ, D), np.float32)
    for ci in range(ncores):
        o = res[ci]["out"].reshape(cfg.nb, cfg.tb, D)
        for bl in range(cfg.nb):
            out[ci * cfg.nb + bl] = o[bl, CTX:, :]
    return out
```

```python
import numpy as np
from contextlib import ExitStack
import concourse.bass as bass
import concourse.mybir as mybir
from concourse.bass_utils import run_bass_kernel_spmd

F32 = mybir.dt.float32
BF16 = mybir.dt.bfloat16
U32 = mybir.dt.uint32
I32 = mybir.dt.int32
AF = mybir.ActivationFunctionType
ALU = mybir.AluOpType
AX = mybir.AxisListType

NCORES = 8
ENGS = ("pe", "dve", "act", "pool", "sp")


class Buf:
    __slots__ = ("ap", "name", "w", "r", "sem", "is_dram")

    def __init__(self, ap, name, is_dram=False):
        self.ap = ap
        self.name = name
        self.w = None
        self.r = []
        self.sem = None
        self.is_dram = is_dram

    def __getitem__(self, idx):
        return self.ap[idx]


SEM_LIMIT = 30000


class P:
    def __init__(self, name="k"):
        self.nc = bass.Bass("TRN2", target_bir_lowering=False)
        self.es = ExitStack()
        self.scopes = []
        nc_ = self.nc
        self.engs = {"pe": nc_.tensor, "dve": nc_.vector, "act": nc_.scalar, "pool": nc_.gpsimd, "sp": nc_.sync}
        self.known = {e: {} for e in ENGS}
        self.nsem = 0
        self.esem = {}
        self.ecnt = {}
        self.retired = []
        for e in ENGS:
            self._new_esem(e)
        self.dpool = {'sw': [], 'hw': []}
        self.dlive = []
        self.nbuf = 0
        self.out_events = []

    def _alloc_sem(self, tag):
        self.nsem += 1
        return self.es.enter_context(self.nc.semaphore(f"{tag}{self.nsem}"))

    def _new_esem(self, e):
        if e in self.esem:
            self.retired.append((id(self.esem[e]), self.ecnt[e], self.esem[e], e == "pe"))
        self.esem[e] = self._alloc_sem(f"se_{e}_")
        self.ecnt[e] = 0

    def _stack(self):
        return self.scopes[-1][0] if self.scopes else self.es

    def dram(self, name, shape, dtype, kind="Internal"):
        t = self.nc.dram_tensor(name, list(shape), dtype, kind=kind)
        return Buf(t.ap(), name, is_dram=True)

    def sbuf(self, shape, dtype, name=None):
        self.nbuf += 1
        name = f"{name or 'sb'}_{self.nbuf}"
        t = self._stack().enter_context(self.nc.sbuf_tensor(name, list(shape), dtype))
        return Buf(t, name)

    def psum(self, shape, dtype=F32, name=None):
        self.nbuf += 1
        name = f"{name or 'ps'}_{self.nbuf}"
        t = self._stack().enter_context(self.nc.psum_tensor(name, list(shape), dtype))
        return Buf(t, name)

    def view(self, ap, name="v"):
        return Buf(ap, name)

    def open_scope(self):
        self.scopes.append((ExitStack(), []))

    def close_scope(self):
        self.barrier()
        st, bufs = self.scopes.pop()
        for (b, kind, cur) in bufs:
            self.dpool[kind].append(cur)
            if b.sem is not None and b.sem.get(kind) is cur:
                del b.sem[kind]
        st.close()

    def barrier(self):
        evs = []
        for e in ENGS:
            if self.ecnt[e] > 0:
                evs.append((id(self.esem[e]), self.ecnt[e], self.esem[e], False))
        for s in self.dlive:
            if s[1] > 0:
                evs.append((id(s[0]), s[1], s[0], False))
        for e in ENGS:
            waits = []
            for sid, val, semh, _ in evs:
                if self.known[e].get(sid, 0) >= val:
                    continue
                if sid == id(self.esem[e]) and e == "pe":
                    pass
                self.known[e][sid] = val
                waits.append((semh, val))
            if waits:
                for s, v in waits:
                    self.engs[e].wait_ge(s, v)

    def _deps(self, eng, reads, writes):
        deps = []
        for b in reads:
            if b.w is not None:
                deps.append(b.w)
        for b in writes:
            if b.r:
                deps.extend(b.r)
            elif b.w is not None:
                deps.append(b.w)
        need = {}
        for ev in deps:
            sid, val, semh, is_pe = ev
            if is_pe and eng == "pe":
                continue
            if self.known[eng].get(sid, 0) >= val:
                continue
            if sid not in need or need[sid][0] < val:
                need[sid] = (val, semh)
        waits = []
        for sid, (val, semh) in need.items():
            self.known[eng][sid] = val
            waits.append((semh, val))
        return waits

    def _commit(self, ev, reads, writes):
        for b in reads:
            b.r.append(ev)
            if len(b.r) > 48:
                b.r = b.r[-48:]
        for b in writes:
            b.w = ev
            b.r = []

    def op(self, eng, fn, reads=(), writes=()):
        if self.ecnt[eng] >= SEM_LIMIT:
            self._new_esem(eng)
        waits = self._deps(eng, reads, writes)
        self.ecnt[eng] += 1
        semh = self.esem[eng]
        ev = (id(semh), self.ecnt[eng], semh, eng == "pe")

        def emit(e, waits=waits, fn=fn, semh=semh):
            for s, v in waits:
                e.wait_ge(s, v)
            fn(e).then_inc(semh, 1)

        emit(self.engs[eng])
        self._commit(ev, reads, writes)
        return ev

    def dma(self, q, out_b, out_ap, in_b, in_ap, fn=None, extra_reads=(), **kw):
        owner = out_b if not out_b.is_dram else in_b
        kind = "sw" if q == "pool" else "hw"
        if owner.sem is None:
            owner.sem = {}
        cur = owner.sem.get(kind)
        if cur is None or cur[1] >= SEM_LIMIT:
            cand = [s for s in self.dpool[kind] if s[1] < SEM_LIMIT]
            if cand:
                cur = cand[0]
                self.dpool[kind].remove(cur)
            else:
                cur = [self._alloc_sem("sd_"), 0]
                self.dlive.append(cur)
            owner.sem[kind] = cur
            if self.scopes:
                self.scopes[-1][1].append((owner, kind, cur))
        waits = self._deps(q, [in_b] + list(extra_reads), [out_b])
        cur[1] += 16
        semh = cur[0]
        ev = (id(semh), cur[1], semh, False)

        def emit(e, waits=waits, semh=semh):
            for s, v in waits:
                e.wait_ge(s, v)
            if fn is None:
                e.dma_start(out=out_ap, in_=in_ap, **kw).then_inc(semh, 16)
            else:
                fn(e).then_inc(semh, 16)

        emit(self.engs[q])
        self._commit(ev, [in_b] + list(extra_reads), [out_b])
        return ev

    def finish(self):
        while self.scopes:
            self.close_scope()
        self.barrier()
        nc = self.nc
        self.es.close()
        return nc

    def load(self, dst, dst_ap, src, src_ap, q="sp", **kw):
        return self.dma(q, dst, dst_ap, src, src_ap, **kw)

    def store(self, dst, dst_ap, src, src_ap, q="pool", **kw):
        return self.dma(q, dst, dst_ap, src, src_ap, **kw)

    def mm(self, out_b, out_ap, lhsT_b, lhsT_ap, rhs_b, rhs_ap, start=True, stop=True, extra_reads=()):
        return self.op("pe", lambda e: e.matmul(out_ap, lhsT_ap, rhs_ap, start=start, stop=stop),
                       reads=[lhsT_b, rhs_b] + list(extra_reads), writes=[out_b])

    def transpose(self, out_b, out_ap, in_b, in_ap, ident_b, ident_ap):
        return self.op("pe", lambda e: e.transpose(out_ap, in_ap, ident_ap),
                       reads=[in_b, ident_b], writes=[out_b])

    def act(self, out_b, out_ap, in_b, in_ap, func, bias=None, scale=None, reads=(), accum=None, accum_b=None):
        kw = {}
        if bias is not None:
            kw["bias"] = bias
        if scale is not None:
            kw["scale"] = scale
        if accum is not None:
            kw["accum_out"] = accum
        w = [out_b] + ([accum_b] if accum_b is not None else [])
        return self.op("act", lambda e: e.activation(out_ap, in_ap, func, **kw),
                       reads=[in_b] + list(reads), writes=w)

    def tt(self, out_b, out_ap, a_b, a_ap, b_b, b_ap, op, eng="dve"):
        return self.op(eng, lambda e: e.tensor_tensor(out_ap, a_ap, b_ap, op),
                       reads=[a_b, b_b], writes=[out_b])

    def ts(self, out_b, out_ap, in_b, in_ap, s1, s2, op0, op1=None, reads=(), eng="dve", accum=None, accum_b=None):
        kw = {}
        if accum is not None:
            kw["accum_out"] = accum
        w = [out_b] + ([accum_b] if accum_b is not None else [])
        if op1 is None:
            return self.op(eng, lambda e: e.tensor_scalar(out_ap, in_ap, s1, None, op0, **kw),
                           reads=[in_b] + list(reads), writes=w)
        return self.op(eng, lambda e: e.tensor_scalar(out_ap, in_ap, s1, s2, op0, op1, **kw),
                       reads=[in_b] + list(reads), writes=w)

    def stt(self, out_b, out_ap, a_b, a_ap, scalar, b_b, b_ap, op0, op1, reads=(), accum=None, accum_b=None):
        kw = {}
        if accum is not None:
            kw["accum_out"] = accum
        w = [out_b] + ([accum_b] if accum_b is not None else [])
        return self.op("dve", lambda e: e.scalar_tensor_tensor(out_ap, a_ap, scalar, b_ap, op0, op1, **kw),
                       reads=[a_b, b_b] + list(reads), writes=w)

    def copy(self, out_b, out_ap, in_b, in_ap, eng="dve"):
        if eng == "act":
            return self.op("act", lambda e: e.activation(out_ap, in_ap, AF.Copy), reads=[in_b], writes=[out_b])
        return self.op(eng, lambda e: e.tensor_copy(out_ap, in_ap), reads=[in_b], writes=[out_b])

    def memset(self, out_b, out_ap, val, eng="dve"):
        return self.op(eng, lambda e: e.memset(out_ap, val), reads=[], writes=[out_b])


def run(prog, in_maps):
    nc = prog.finish()
    res = run_bass_kernel_spmd(nc, in_maps, core_ids=list(range(NCORES)))
    return res.results


D = 1024
KC = D // 128
N_MOD = 6
NORM_EPS = 1e-6
GRID_W = 64
LB_FLOOR = 1e-30
CH = 32
EV = dict(q=0, i=512, zf=1024, zb=1536, g=2048, aq=2560, ak=3072, av=3328, aqs=3584, aks=4096)
EV_NF = 4352
OD = dict(q=0, k=256, v=512, g=1024, z=1536, xs=2048, bm=2560, cm=2816, lrf=3072, lrb=3088, dtf=3104, dtb=3112)
OD_NF = 3200
PEER_HEADS, PEER_NKEYS, PEER_TOPK = 8, 128, 16


class Cfg:
    def __init__(self, nb, seq, ctx, depth):
        self.nb, self.seq, self.ctx, self.depth = nb, seq, ctx, depth
        self.tb = seq + ctx
        self.T = nb * self.tb
        self.R = nb + 1
        self.alpha = (2 * depth) ** 0.25
        assert seq % 128 == 0 and ctx % 128 == 0

    def segs(self):
        out = []
        for b in range(self.nb):
            o = b * self.tb
            out.append((o, o + self.ctx, self.nb))
            out.append((o + self.ctx, o + self.tb, b))
        return out

    def seg_of_tile(self, ti):
        t = ti * 128
        for (a, b, r) in self.segs():
            if a <= t < b:
                return r
        raise AssertionError


def blocks(n, step):
    return [(a, min(a + step, n)) for a in range(0, n, step)]


def stage_mod(p, cfg, io, G):
    R = cfg.R
    NM = N_MOD * D
    p.open_scope()
    cT = p.sbuf([128, KC * R], F32, "cT")
    p.load(cT, cT[:, :], io["cT"], io["cT"].ap[:, :])
    sc = p.sbuf([128, KC, R], F32, "silu_c")
    p.act(sc, sc.ap[:, :, :].rearrange("p k r -> p (k r)"), cT, cT[:, :], AF.Silu)
    mbT = p.sbuf([128, cfg.depth * 48], F32, "mbT")
    p.load(mbT, mbT[:, :], io["mod_bT"], io["mod_bT"].ap[:, :])
    CB = 768
    wst = [p.sbuf([128, KC, CB], F32, f"mw{i}") for i in range(2)]
    psF = [p.psum([128, 512], F32, f"psF{i}") for i in range(2)]
    psT = [p.psum([128, 512], F32, f"psT{i}") for i in range(2)]
    mbrow = p.sbuf([R, NM], F32, "mbrow")
    mtok = p.sbuf([R, NM], F32, "mtok")
    modT = G["modT"]
    it = 0
    for l in range(cfg.depth):
        p.load(mbrow, mbrow[:, :], io["mod_b"], io["mod_b"].ap[l:l + 1, :].partition_broadcast(R))
        for cb in range(NM // CB):
            w = wst[it % 2]
            src = io["mod_w"].ap[l, :, cb * CB:(cb + 1) * CB].rearrange("(kc p) n -> p kc n", p=128)
            p.load(w, w[:, :, :], io["mod_w"], src, q="sp" if it % 2 else "act")
            pf = psF[it % 2]
            for dcl in range(CB // 128):
                dc = cb * (CB // 128) + dcl
                for kc in range(KC):
                    p.mm(pf, pf[:, dcl * R:(dcl + 1) * R], w, w[:, kc, dcl * 128:(dcl + 1) * 128],
                         sc, sc[:, kc, :], start=(kc == 0), stop=(kc == KC - 1))
                p.ts(modT, modT[:, l, dc, :], pf, pf[:, dcl * R:(dcl + 1) * R],
                     mbT[:, l * 48 + dc:l * 48 + dc + 1], None, ALU.add, reads=[mbT])
            pt = psT[it % 2]
            for (n0, n1) in blocks(CB, 512):
                for kc in range(KC):
                    p.mm(pt, pt[0:R, 0:n1 - n0], sc, sc[:, kc, :], w, w[:, kc, n0:n1],
                         start=(kc == 0), stop=(kc == KC - 1))
                p.tt(mtok, mtok[:, cb * CB + n0:cb * CB + n1], pt, pt[0:R, 0:n1 - n0],
                     mbrow, mbrow[:, cb * CB + n0:cb * CB + n1], ALU.add)
            it += 1
        p.store(G["modtok"], G["modtok"].ap[l, :, :], mtok, mtok[:, :])
    p.close_scope()


def stage_inproj(p, cfg, G, l, W_ap, NF, YT):
    T = cfg.T
    p.open_scope()
    ident = G["ident"]
    modT = G["modT"]
    w_bf = p.sbuf([128, KC, NF], BF16, "w_bf")
    wst = [p.sbuf([128, NF], F32, f"wst{i}") for i in range(2)]
    for kc in range(KC):
        st = wst[kc % 2]
        p.load(st, st[:, :], G["ext"], W_ap[kc * 128:(kc + 1) * 128, :], q="sp" if kc % 2 else "act")
        p.copy(w_bf, w_bf[:, kc, :], st, st[:, :], eng="pool" if kc % 2 else "dve")
    scale1 = p.sbuf([128, KC, cfg.R], F32, "scale1")
    p.ts(scale1, scale1[:, :, :], modT, modT[:, l, 8:16, :], 1.0, None, ALU.add)
    hst = [p.sbuf([128, D], F32, f"hst{i}") for i in range(2)]
    uT = [p.sbuf([128, KC, 512], BF16, f"uT{i}") for i in range(2)]
    pT = [p.psum([128, 4, 128], F32, f"pT{i}") for i in range(4)]
    pY = [p.psum([128, 512], F32, f"pY{i}") for i in range(3)]
    ysb = [p.sbuf([128, 512], F32, f"ysb{i}") for i in range(4)]
    j = 0
    for bi, (t0, t1) in enumerate(blocks(T, 512)):
        u = uT[bi % 2]
        nt = (t1 - t0) // 128
        for tl in range(nt):
            ti = t0 // 128 + tl
            r = cfg.seg_of_tile(ti)
            hs = hst[ti % 2]
            p.load(hs, hs[:, :], G["H"], G["H"].ap[ti * 128:(ti + 1) * 128, :], q="sp")
            for half in range(2):
                pt = pT[(2 * ti + half) % 4]
                for kk in range(4):
                    kc = half * 4 + kk
                    p.transpose(pt, pt[:, kk, :], hs, hs[:, kc * 128:(kc + 1) * 128], ident, ident[:, :])
                for kk in range(4):
                    kc = half * 4 + kk
                    p.act(u, u[:, kc, tl * 128:(tl + 1) * 128], pt, pt[:, kk, :], AF.Identity,
                          bias=modT[:, l, kc, r:r + 1], scale=scale1[:, kc, r:r + 1], reads=[modT, scale1])
        w = t1 - t0
        for fc in range(NF // 128):
            py = pY[j % 3]
            for kc in range(KC):
                p.mm(py, py[:, 0:w], w_bf, w_bf[:, kc, fc * 128:(fc + 1) * 128], u, u[:, kc, 0:w],
                     start=(kc == 0), stop=(kc == KC - 1))
            ys = ysb[j % 4]
            p.copy(ys, ys[:, 0:w], py, py[:, 0:w], eng="dve" if j % 2 else "act")
            p.store(YT, YT.ap[fc * 128:(fc + 1) * 128, t0:t1], ys, ys[:, 0:w], q="pool" if j % 2 else "sp")
            j += 1
    p.close_scope()


def layernorm_tile(p, cfg, G, r_b, out_b, lng, lnb, scr):
    stats, mv, rstd, tmp = scr
    for c in range(2):
        p.op("dve", lambda e, c=c: e.bn_stats(stats[:, c, :], r_b[:, c * 512:(c + 1) * 512]),
             reads=[r_b], writes=[stats])
    p.op("dve", lambda e: e.bn_aggr(mv[:, :], stats.ap[:, :, :].rearrange("p a b -> p (a b)")),
         reads=[stats], writes=[mv])
    p.act(rstd, rstd[:, 0:1], mv, mv[:, 1:2], AF.Ln, bias=float(NORM_EPS))
    p.act(rstd, rstd[:, 1:2], rstd, rstd[:, 0:1], AF.Exp, scale=-0.5)
    p.ts(tmp, tmp[:, :], r_b, r_b[:, :], mv[:, 0:1], rstd[:, 1:2], ALU.subtract, ALU.mult, reads=[mv, rstd])
    p.tt(tmp, tmp[:, :], tmp, tmp[:, :], lng, lng[:, :], ALU.mult)
    p.tt(out_b, out_b[:, :], tmp, tmp[:, :], lnb, lnb[:, :], ALU.add, eng="pool")


def stage_resid_ln(p, cfg, G, io, l, which, W_ap, mix_dram):
    T = cfg.T
    p.open_scope()
    gate_off = (2 if which == 0 else 5) * D
    lng = p.sbuf([128, D], F32, "lng")
    lnb = p.sbuf([128, D], F32, "lnb")
    p.load(lng, lng[:, :], io["ln_g"], io["ln_g"].ap[l, which:which + 1, :].partition_broadcast(128))
    p.load(lnb, lnb[:, :], io["ln_b"], io["ln_b"].ap[l, which:which + 1, :].partition_broadcast(128))
    gb = []
    for r in range(cfg.R):
        g = p.sbuf([128, D], F32, f"gate{r}")
        p.load(g, g[:, :], G["modtok"], G["modtok"].ap[l, r:r + 1, gate_off:gate_off + D].partition_broadcast(128))
        gb.append(g)
    if which == 0:
        w_bf = p.sbuf([128, KC, D], BF16, "wo_bf")
        wst = [p.sbuf([128, D], F32, f"wost{i}") for i in range(2)]
        for kc in range(KC):
            st = wst[kc % 2]
            p.load(st, st[:, :], G["ext"], W_ap[kc * 128:(kc + 1) * 128, :], q="act")
            p.copy(w_bf, w_bf[:, kc, :], st, st[:, :], eng="pool")
        mx = [p.sbuf([128, KC, 128], BF16, f"mx{i}") for i in range(2)]
        po = [p.psum([128, 2, 512], F32, f"po{i}") for i in range(2)]
    else:
        br = [p.sbuf([128, D], F32, f"br{i}") for i in range(2)]
    hs = [p.sbuf([128, D], F32, f"h{i}") for i in range(2)]
    rb = [p.sbuf([128, D], F32, f"r{i}") for i in range(2)]
    ob = [p.sbuf([128, D], F32, f"o{i}") for i in range(2)]
    scrs = [(p.sbuf([128, 2, 6], F32, f"stats{i}"), p.sbuf([128, 2], F32, f"mv{i}"), p.sbuf([128, 2], F32, f"rstd{i}"),
             p.sbuf([128, D], F32, f"lntmp{i}")) for i in range(2)]
    for ti in range(T // 128):
        r = cfg.seg_of_tile(ti)
        h = hs[ti % 2]
        rr = rb[ti % 2]
        p.load(h, h[:, :], G["H"], G["H"].ap[ti * 128:(ti + 1) * 128, :], q="sp")
        if which == 0:
            m = mx[ti % 2]
            p.load(m, m[:, :, :], mix_dram,
                   mix_dram.ap[:, ti * 128:(ti + 1) * 128].rearrange("(kc p) t -> p kc t", p=128), q="act")
            ps = po[ti % 2]
            for nb in range(2):
                for kc in range(KC):
                    p.mm(ps, ps[:, nb, :], m, m[:, kc, :], w_bf, w_bf[:, kc, nb * 512:(nb + 1) * 512],
                         start=(kc == 0), stop=(kc == KC - 1))
            p.tt(rr, rr.ap[:, :], ps, ps.ap[:, :, :].rearrange("p a b -> p (a b)"), gb[r], gb[r][:, :], ALU.mult)
        else:
            b_ = br[ti % 2]
            p.load(b_, b_[:, :], mix_dram, mix_dram.ap[ti * 128:(ti + 1) * 128, :], q="act")
            p.tt(rr, rr[:, :], b_, b_[:, :], gb[r], gb[r][:, :], ALU.mult, eng="pool")
        p.stt(rr, rr[:, :], h, h[:, :], float(cfg.alpha), rr, rr[:, :], ALU.mult, ALU.add)
        o = ob[ti % 2]
        layernorm_tile(p, cfg, G, rr, o, lng, lnb, scrs[ti % 2])
        p.store(G["H"], G["H"].ap[ti * 128:(ti + 1) * 128, :], o, o[:, :], q="pool")
    p.close_scope()


def build_program(cfg, stop_after=None, debug=()):
    p = P("mk")
    io = {}
    G = {}

    def ext(name, shape, dtype=F32):
        io[name] = p.dram(name, shape, dtype, "ExternalInput")
        return io[name]

    T, R, L = cfg.T, cfg.R, cfg.depth
    ne, no = (L + 1) // 2, L // 2
    ext("h0", [T, D])
    ext("cT", [128, KC * R])
    ext("mod_w", [L, D, N_MOD * D])
    ext("mod_b", [L, N_MOD * D])
    ext("mod_bT", [128, L * 48])
    ext("ln_g", [L, 2, D])
    ext("ln_b", [L, 2, D])
    ext("ev_w_in", [ne, D, EV_NF])
    ext("ev_w_out", [ne, D, D])
    if no:
        ext("od_w_in", [no, D, OD_NF])
        ext("od_w_out", [no, D, D])
    ext("ident", [128, 128])
    ext("peer_wq", [L, D, 2048])
    ext("peer_subkT", [L, 128, 2048])
    for l_ in range(L):
        ext(f"peer_u{l_}", [PEER_NKEYS * PEER_NKEYS, D])
        ext(f"peer_v{l_}", [PEER_NKEYS * PEER_NKEYS, D])
    ext("cmask", [128, 128])
    ext("iota16", [128, 16])
    ext("rm3", [128, 1])
    ext("rmask", [128, cfg.tb])
    ext("cosT", [128, cfg.tb])
    ext("sinT", [128, cfg.tb])
    ext("hg_lbT", [128, 2 * ne * 4])
    ext("hg_norm_gT", [ne, 128, 1])
    ext("at_normT", [ne, 128, 4])
    if no:
        ext("ssd_conv_wT", [no, 128, 40])
        ext("ssd_conv_bT", [no, 128, 8])
        ext("ssd_dt_biasT", [no, 8, 2])
        ext("ssd_a_logT", [no, 8, 2])
        ext("ssd_dT", [no, 128, 4])
        ext("ssd_norm_gT", [no, 128, 4])
        ext("selT", [8, 8 * 128])
        ext("mbias", [128, 128])
        ext("rmask128", [8, cfg.tb])
        ext("gla_gate_w", [no, 2, 16, 256])
        ext("gla_gate_bT", [no, 64, 8])
        ext("gla_norm_gT", [no, 128, 1])
    G["ext"] = Buf(None, "ext", is_dram=True)
    out = p.dram("out", [T, D], F32, "ExternalOutput")
    G["H"] = out
    kind = "ExternalOutput" if debug else "Internal"
    G["modtok"] = p.dram("modtok", [L, R, N_MOD * D], F32, kind)
    G["YT"] = p.dram("YT", [EV_NF, T], F32, kind)
    G["MIXT"] = p.dram("MIXT", [D, T], BF16, kind)
    G["PEERO"] = p.dram("PEERO", [T, D], F32, kind)
    G["UVB"] = p.dram("UVB", [PEER_NKEYS * PEER_NKEYS, 2 * D], BF16, "Internal")
    G["XSF"] = p.dram("XSF", [512, T], F32, "Internal")
    G["YPRE"] = p.dram("YPRE", [512, T], F32, "Internal")
    G["ident"] = p.sbuf([128, 128], F32, "ident")
    p.load(G["ident"], G["ident"][:, :], io["ident"], io["ident"].ap[:, :])
    G["ident_bf"] = p.sbuf([128, 128], BF16, "ident_bf")
    p.copy(G["ident_bf"], G["ident_bf"][:, :], G["ident"], G["ident"][:, :])
    G["modT"] = p.sbuf([128, L, 48, R], F32, "modT")
    G["ones_mean"] = p.sbuf([128, 128], F32, "ones_mean")
    p.memset(G["ones_mean"], G["ones_mean"][:, :], 1.0 / 128.0)
    for nm in ("cmask", "rm3"):
        G[nm] = p.sbuf(list(io[nm].ap.shape), F32, nm)
        p.load(G[nm], G[nm][:, :], io[nm], io[nm].ap[:, :])

    p.open_scope()
    cp = [p.sbuf([128, D], F32, f"cp{i}") for i in range(3)]
    for ti in range(T // 128):
        c = cp[ti % 3]
        p.load(c, c[:, :], io["h0"], io["h0"].ap[ti * 128:(ti + 1) * 128, :], q="sp")
        p.store(G["H"], G["H"].ap[ti * 128:(ti + 1) * 128, :], c, c[:, :], q="act")
    p.close_scope()
    stage_mod(p, cfg, io, G)
    if stop_after == "mod":
        return p
    for l in range(L):
        j = l // 2
        if l % 2 == 0:
            stage_inproj(p, cfg, G, l, io["ev_w_in"].ap[j], EV_NF, G["YT"])
        else:
            stage_inproj(p, cfg, G, l, io["od_w_in"].ap[j], OD_NF, G["YT"])
        if stop_after == f"inproj{l}":
            return p
        if l % 2 == 0:
            stage_vscan(p, cfg, G, io, l, "hgrn")
            if stop_after == f"scan{l}":
                return p
            stage_attn(p, cfg, G, io, l)
            if stop_after == f"attn{l}":
                return p
            stage_resid_ln(p, cfg, G, io, l, 0, io["ev_w_out"].ap[j], G["MIXT"])
        else:
            stage_vscan(p, cfg, G, io, l, "gla")
            if stop_after == f"scan{l}":
                return p
            stage_ssd(p, cfg, G, io, l)
            if stop_after == f"ssd{l}":
                return p
            stage_resid_ln(p, cfg, G, io, l, 0, io["od_w_out"].ap[j], G["MIXT"])
        if stop_after == f"mix{l}":
            return p
        stage_peer(p, cfg, G, io, l)
        if stop_after == f"peer{l}":
            return p
        stage_resid_ln(p, cfg, G, io, l, 1, None, G["PEERO"])
        if stop_after == f"layer{l}":
            return p
    return p


def _pair_swap(n):
    idx = np.arange(n)
    return idx ^ 1


def prep_inputs(inp, cfg, ncores):
    L = cfg.depth
    f32 = np.float32
    x, c, ctx, c_ctx = (np.asarray(inp[k], f32) for k in ("x", "c", "ctx", "c_ctx"))
    ev_w_in = np.asarray(inp["ev_w_in"], f32)
    aq = ev_w_in[:, :, EV["aq"]:EV["aq"] + 512][:, :, _pair_swap(512)]
    ak = ev_w_in[:, :, EV["ak"]:EV["ak"] + 256][:, :, _pair_swap(256)]
    ev_ext = np.ascontiguousarray(np.concatenate([ev_w_in, aq, ak], axis=2))
    od_w_in = np.asarray(inp["od_w_in"], f32)
    no = od_w_in.shape[0]
    if no:
        q, k, v, g, lrf, lrb, z, xs, bm, cm, dtf, dtb = np.split(
            od_w_in, np.cumsum([256, 256, 512, 512, 16, 16, 512, 512, 256, 256, 8, 8])[:-1].tolist(), axis=2)
        pad = np.zeros((no, D, OD_NF - 3120), f32)
        od_ext = np.ascontiguousarray(np.concatenate([q, k, v, g, z, xs, bm, cm, lrf, lrb, dtf, dtb, pad], axis=2))
    mod_b = np.asarray(inp["mod_b"], f32)
    mod_bT = np.ascontiguousarray(mod_b.reshape(L, 48, 128).transpose(2, 0, 1).reshape(128, L * 48))
    shared = {
        "mod_w": np.asarray(inp["mod_w"], f32), "mod_b": mod_b, "mod_bT": mod_bT,
        "ln_g": np.asarray(inp["ln_g"], f32), "ln_b": np.asarray(inp["ln_b"], f32),
        "ev_w_in": ev_ext, "ev_w_out": np.asarray(inp["ev_w_out"], f32),
        "ident": np.eye(128, dtype=f32),
    }
    shared["peer_wq"] = np.asarray(inp["peer_wq"], f32)
    sk = np.asarray(inp["peer_subkeys"], f32)
    shared["peer_subkT"] = np.ascontiguousarray(sk.transpose(0, 4, 1, 2, 3).reshape(L, 128, 2048))
    for l_ in range(L):
        shared[f"peer_u{l_}"] = np.asarray(inp["peer_u"][l_], f32)
        shared[f"peer_v{l_}"] = np.asarray(inp["peer_v"][l_], f32)
    jj = np.arange(128)
    shared["cmask"] = ((jj[:, None] // CH == jj[None, :] // CH) & (jj[:, None] <= jj[None, :])).astype(f32)
    shared["iota16"] = np.tile(np.arange(16, dtype=f32), (128, 1))
    shared["rm3"] = (jj >= 96).astype(f32)[:, None].copy()
    rm = np.ones((128, cfg.tb), f32)
    rm[:, ::CH] = 0.0
    shared["rmask"] = rm
    rows_ = cfg.seq // GRID_W
    trow = np.repeat(np.arange(rows_), GRID_W).astype(np.float64)
    tcol = np.tile(np.arange(GRID_W), rows_).astype(np.float64)
    inv = 10000.0 ** (-np.arange(32, dtype=np.float64) / 32)
    ang = np.concatenate([trow[:, None] * inv, tcol[:, None] * inv], axis=-1)
    angd = np.repeat(ang, 2, axis=1).T
    cosT = np.ones((128, cfg.tb)); sinT = np.zeros((128, cfg.tb))
    cosT[:, cfg.ctx:] = np.cos(angd)
    sgn = np.where(np.arange(128) % 2 == 0, -1.0, 1.0)[:, None]
    sinT[:, cfg.ctx:] = np.sin(angd) * sgn
    shared["cosT"] = cosT.astype(f32); shared["sinT"] = sinT.astype(f32)
    ne = ev_w_in.shape[0]
    lbl = np.asarray(inp["hg_lb_logits"], f32)
    shared["hg_lbT"] = np.ascontiguousarray(lbl.reshape(2, ne, 4, 128).transpose(3, 0, 1, 2).reshape(128, 2 * ne * 4))
    shared["hg_norm_gT"] = np.ascontiguousarray(np.asarray(inp["hg_norm_g"], f32)[:, :, None])
    gqn = np.asarray(inp["at_q_norm_g"], f32); gkn = np.asarray(inp["at_k_norm_g"], f32)
    sw = _pair_swap(128)
    shared["at_normT"] = np.ascontiguousarray(np.stack([gqn, gqn[:, sw], gkn, gkn[:, sw]], axis=2))
    if no:
        cwt = np.asarray(inp["ssd_conv_w"], f32)
        shared["ssd_conv_wT"] = np.ascontiguousarray(cwt.reshape(no, 5, 8, 128).transpose(0, 3, 2, 1).reshape(no, 128, 40))
        shared["ssd_conv_bT"] = np.ascontiguousarray(np.asarray(inp["ssd_conv_b"], f32).reshape(no, 8, 128).transpose(0, 2, 1))
        shared["ssd_dt_biasT"] = np.ascontiguousarray(np.asarray(inp["ssd_dt_bias"], f32).transpose(0, 2, 1))
        shared["ssd_a_logT"] = np.ascontiguousarray(np.asarray(inp["ssd_a_log"], f32).transpose(0, 2, 1))
        dd = np.asarray(inp["ssd_d"], f32)
        shared["ssd_dT"] = np.ascontiguousarray(np.repeat(dd, 64, axis=1).reshape(no, 4, 128).transpose(0, 2, 1))
        shared["ssd_norm_gT"] = np.ascontiguousarray(np.asarray(inp["ssd_norm_g"], f32).reshape(no, 4, 128).transpose(0, 2, 1))
        selT = np.zeros((8, 8, 128), f32)
        for h_ in range(8):
            selT[h_, h_, :] = 1.0
        shared["selT"] = selT.reshape(8, 8 * 128)
        shared["mbias"] = np.where(jj[:, None] <= jj[None, :], 0.0, -30000.0).astype(f32)
        rm128 = np.ones((8, cfg.tb), f32)
        rm128[:, ::128] = 0.0
        shared["rmask128"] = rm128
        shared["gla_gate_w"] = np.asarray(inp["gla_gate_w"], f32)
        gb_ = np.asarray(inp["gla_gate_b"], f32)
        shared["gla_gate_bT"] = np.ascontiguousarray(gb_.reshape(no, 2, 4, 64).transpose(0, 3, 1, 2).reshape(no, 64, 8))
        shared["gla_norm_gT"] = np.ascontiguousarray(np.asarray(inp["gla_norm_g"], f32)[:, :, None])
    if no:
        shared["od_w_in"] = od_ext
        shared["od_w_out"] = np.asarray(inp["od_w_out"], f32)
    maps = []
    for ci in range(ncores):
        bs = range(ci * cfg.nb, (ci + 1) * cfg.nb)
        h0 = np.concatenate([np.concatenate([ctx[b], x[b]], axis=0) for b in bs], axis=0)
        cv = np.stack([c[b] for b in bs] + [c_ctx], axis=0)
        cT = np.ascontiguousarray(cv.T.reshape(KC, 128, cfg.R).transpose(1, 0, 2).reshape(128, KC * cfg.R))
        m = dict(shared)
        m["h0"] = np.ascontiguousarray(h0)
        m["cT"] = cT
        maps.append(m)
    return maps


def nat_slice(cfg, s0, s1, d):
    if d == 0:
        return slice(s0, s1), False
    if s1 <= cfg.ctx:
        return slice(cfg.ctx - s1, cfg.ctx - s0), True
    assert s0 >= cfg.ctx
    return slice(cfg.tb - (s1 - cfg.ctx), cfg.tb - (s0 - cfg.ctx)), True


def nat_ap(ap, cfg, s0, s1, d, rows=None):
    sl, rev = nat_slice(cfg, s0, s1, d)
    a = ap[:, sl] if rows is None else ap[rows, sl]
    return a[:, ::-1] if rev else a


def stage_vscan(p, cfg, G, io, l, kind):
    Tb, CTX = cfg.tb, cfg.ctx
    j = l // 2
    hgrn = kind == "hgrn"
    dk = 128 if hgrn else 64
    dv = 128
    NH = 4
    YT = G["YT"]
    p.open_scope()
    segs = [(0, CTX), (CTX, Tb)]
    identb = G["ident_bf"]
    cmask = G["cmask"]
    onesm = G["ones_mean"]
    pTrK = p.psum([128, 1024], BF16, "pTrK")
    pTrV = p.psum([128, 1024], BF16, "pTrV")
    pAtt = [p.psum([128, 512], F32, f"pAtt{i}") for i in range(1)]
    pO = [p.psum([128, 512], F32, f"pO{i}") for i in range(2)]
    pKV = [p.psum([128, 512], F32, f"pKV{i}") for i in range(2)]
    pX = p.psum([128, 512], F32, "pX")
    if hgrn:
        ne = (cfg.depth + 1) // 2
        lg = p.sbuf([128, 2, ne, NH], F32, "lb_logits")
        p.load(lg, lg.ap[:, :, :, :].rearrange("p a b c -> p (a b c)"), io["hg_lbT"], io["hg_lbT"].ap[:, :])
        p.act(lg, lg[:, :, :, :], lg, lg[:, :, :, :], AF.Exp)
        ssum = p.sbuf([128, 2, NH], F32, "lb_sum")
        p.copy(ssum, ssum[:, :, :], lg, lg[:, :, 0, :])
        for i in range(1, ne):
            p.tt(ssum, ssum[:, :, :], ssum, ssum[:, :, :], lg, lg[:, :, i, :], ALU.add)
        ssf = ssum.ap[:, :, :].rearrange("p a b -> p (a b)")
        p.op("dve", lambda e: e.reciprocal(ssf, ssf), reads=[ssum], writes=[ssum])
        lb = p.sbuf([128, 2, NH], F32, "lb")
        p.memset(lb, lb[:, :, :], 0.0)
        for i in range(1, j + 1):
            p.tt(lb, lb[:, :, :], lb, lb[:, :, :], lg, lg[:, :, i, :], ALU.add)
        p.tt(lb, lb[:, :, :], lb, lb[:, :, :], ssum, ssum[:, :, :], ALU.mult)
        oml = p.sbuf([128, 2, NH], F32, "oml")
        p.ts(oml, oml[:, :, :], lb, lb[:, :, :], -1.0, 1.0, ALU.mult, ALU.add)
        noml = p.sbuf([128, 2, NH], F32, "noml")
        p.ts(noml, noml[:, :, :], oml, oml[:, :, :], -1.0, None, ALU.mult)
        lbf = p.sbuf([128, 2, NH], F32, "lbf")
        p.ts(lbf, lbf[:, :, :], lb, lb[:, :, :], float(LB_FLOOR), None, ALU.max)
        gn = p.sbuf([128, 1], F32, "gn")
        p.load(gn, gn[:, :], io["hg_norm_gT"], io["hg_norm_gT"].ap[j])
        rows = dict(q=EV["q"], k=None, v=EV["i"], g=EV["g"], z=(EV["zf"], EV["zb"]))
        qscale = 128 ** -0.5
    else:
        gw = p.sbuf([16, 2, 256], F32, "gw")
        p.load(gw, gw[:, :, :], io["gla_gate_w"], io["gla_gate_w"].ap[j].rearrange("d r c -> r d c"))
        gw_bf = p.sbuf([16, 2, 256], BF16, "gw_bf")
        p.copy(gw_bf, gw_bf[:, :, :], gw, gw[:, :, :])
        gbT = p.sbuf([64, 2 * NH], F32, "gbT")
        p.load(gbT, gbT[:, :], io["gla_gate_bT"], io["gla_gate_bT"].ap[j])
        gn = p.sbuf([128, 1], F32, "gn")
        p.load(gn, gn[:, :], io["gla_norm_gT"], io["gla_norm_gT"].ap[j])
        rows = dict(q=OD["q"], k=OD["k"], v=OD["v"], g=OD["g"], z=(OD["lrf"], OD["lrb"]))
        qscale = 64 ** -0.5
    q_raw = p.sbuf([dk, Tb], F32, "q_raw")
    k_raw = None if hgrn else p.sbuf([dk, Tb], F32, "k_raw")
    v_raw = p.sbuf([dv, Tb], F32, "v_raw")
    g_raw = p.sbuf([dv, Tb], F32, "g_raw")
    z_raw = [p.sbuf([dk if hgrn else 16, Tb], F32, f"z_raw{i}") for i in range(2)]
    A1 = p.sbuf([dk, Tb], F32, "A1")
    A2 = p.sbuf([dk, Tb], F32, "A2")
    A3 = p.sbuf([dk, Tb], F32, "A3")
    A4 = p.sbuf([dk, Tb], F32, "A4")
    oacc = p.sbuf([dv, Tb], F32, "oacc")
    Qh = p.sbuf([dk, Tb], BF16, "Qh")
    Kh = p.sbuf([dk, Tb], BF16, "Kh")
    vb = p.sbuf([dv, Tb], BF16, "vb")
    lr_bf = None if hgrn else p.sbuf([16, Tb], BF16, "lr_bf")
    outb = p.sbuf([dv, Tb], BF16, "outb")
    Ktok = [p.sbuf([128, dk], BF16, f"Ktok{i}") for i in range(2)]
    Vtok = [p.sbuf([128, dv], BF16, f"Vtok{i}") for i in range(2)]
    Ktok3 = [p.sbuf([128, dk], BF16, f"Ktok3{i}") for i in range(2)]
    att_sb = [p.sbuf([128, 128], BF16, f"att{i}") for i in range(2)]
    S = p.sbuf([dk, dv], F32, "S")
    Stmp = p.sbuf([dk, dv], F32, "Stmp")
    Sbf = [p.sbuf([dk, dv], BF16, f"Sbf{i}") for i in range(2)]
    rmask = p.sbuf([128, Tb], F32, "rmask")
    p.load(rmask, rmask[:, :], io["rmask"], io["rmask"].ap[:, :])
    tabgen = peer_tables_gen(p, cfg, G, io, l)
    n_tile_iters = cfg.nb * NH * 2 * (Tb // 128)
    tab_per_iter = -(-(PEER_NKEYS * PEER_NKEYS // 128) // n_tile_iters)
    import os
    VCUT = int(os.environ.get("VCUT", "0"))
    if VCUT == 1:
        p.close_scope(); return
    nt = Tb // 128
    NCK = 128 // CH
    for b in range(cfg.nb):
        c0 = b * Tb
        for h in range(NH):
            p.load(q_raw, q_raw[:, :], YT, YT.ap[rows["q"] + h * dk:rows["q"] + (h + 1) * dk, c0:c0 + Tb], q="sp")
            if not hgrn:
                p.load(k_raw, k_raw[:, :], YT, YT.ap[rows["k"] + h * dk:rows["k"] + (h + 1) * dk, c0:c0 + Tb], q="act")
            p.load(v_raw, v_raw[:, :], YT, YT.ap[rows["v"] + h * dv:rows["v"] + (h + 1) * dv, c0:c0 + Tb], q="act")
            p.load(g_raw, g_raw[:, :], YT, YT.ap[rows["g"] + h * dv:rows["g"] + (h + 1) * dv, c0:c0 + Tb], q="sp")
            for d in range(2):
                zr = z_raw[d]
                if hgrn:
                    p.load(zr, zr[:, :], YT, YT.ap[rows["z"][d] + h * dk:rows["z"][d] + (h + 1) * dk, c0:c0 + Tb], q="sp")
                else:
                    p.load(zr, zr[:, :], YT, YT.ap[rows["z"][d]:rows["z"][d] + 16, c0:c0 + Tb], q="sp")
                if VCUT == 2:
                    p.close_scope(); return
                if hgrn:
                    for (s0, s1) in segs:
                        p.act(A1, A1[:, s0:s1], zr, nat_ap(zr.ap, cfg, s0, s1, d), AF.Sigmoid)
                    p.ts(A2, A2[:, :], A1, A1[:, :], oml[:, d, h:h + 1], lbf[:, d, h:h + 1], ALU.mult, ALU.add,
                         reads=[oml, lbf])
                    p.ts(A4, A4[:, :], A1, A1[:, :], noml[:, d, h:h + 1], oml[:, d, h:h + 1], ALU.mult, ALU.add,
                         reads=[oml, noml], eng="pool")
                    p.act(A2, A2[:, :], A2, A2[:, :], AF.Ln)
                    esc = 1.0
                else:
                    for (s0, s1) in segs:
                        p.copy(lr_bf, lr_bf[:, s0:s1], zr, nat_ap(zr.ap, cfg, s0, s1, d), eng="pool")
                    for (t0, t1) in blocks(Tb, 512):
                        p.mm(pX, pX[0:dk, 0:t1 - t0], gw_bf, gw_bf[:, d, h * dk:(h + 1) * dk], lr_bf, lr_bf[:, t0:t1])
                        p.act(A2, A2[:, t0:t1], pX, pX[0:dk, 0:t1 - t0], AF.Sigmoid,
                              bias=gbT[:, d * NH + h:d * NH + h + 1], reads=[gbT])
                    p.act(A2, A2[:, :], A2, A2[:, :], AF.Ln)
                    esc = 1.0 / 16.0
                p.op("dve", lambda e: e.tensor_tensor_scan(A3[:, :], rmask[0:dk, :], A2[:, :], 0.0, ALU.mult, ALU.add),
                     reads=[rmask, A2], writes=[A3])
                p.act(A1, A1[:, :], A3, A3[:, :], AF.Exp, scale=-esc)
                p.act(A3, A3[:, :], A3, A3[:, :], AF.Exp, scale=esc)
                if hgrn:
                    for (s0, s1) in segs:
                        p.act(A2, A2[:, s0:s1], q_raw, nat_ap(q_raw.ap, cfg, s0, s1, d), AF.Silu)
                    p.stt(Qh, Qh[:, :], A2, A2[:, :], float(qscale), A3, A3[:, :], ALU.mult, ALU.mult)
                    p.tt(Kh, Kh[:, :], A4, A4[:, :], A1, A1[:, :], ALU.mult, eng="pool")
                else:
                    for (s0, s1) in segs:
                        p.stt(Qh, Qh[:, s0:s1], q_raw, nat_ap(q_raw.ap, cfg, s0, s1, d), float(qscale),
                              A3, A3[:, s0:s1], ALU.mult, ALU.mult)
                        p.tt(Kh, Kh[:, s0:s1], k_raw, nat_ap(k_raw.ap, cfg, s0, s1, d), A1, A1[:, s0:s1], ALU.mult)
                for (s0, s1) in segs:
                    p.copy(vb, vb[:, s0:s1], v_raw, nat_ap(v_raw.ap, cfg, s0, s1, d), eng="pool")
                if VCUT == 3:
                    p.close_scope(); return
                p.memset(S, S[:, :], 0.0)
                p.memset(Sbf[0], Sbf[0][:, :], 0.0)
                cur = 0
                dprev = A3[:, 0:1]
                for ti in range(nt):
                    cs = slice(ti * 128, (ti + 1) * 128)
                    kt, vt = Ktok[ti % 2], Vtok[ti % 2]
                    kt3 = Ktok3[ti % 2]
                    p.transpose(pTrK, pTrK[:, 0:dk], Kh, Kh[:, cs], identb, identb[0:dk, 0:dk])
                    p.transpose(pTrV, pTrV[:, 0:dv], vb, vb[:, cs], identb, identb[:, :])
                    p.copy(kt, kt[:, :], pTrK, pTrK[:, 0:dk], eng="act")
                    p.copy(vt, vt[:, :], pTrV, pTrV[:, 0:dv], eng="dve")
                    p.ts(kt3, kt3[:, :], kt, kt[:, :], G["rm3"][:, 0:1], None, ALU.mult, reads=[G["rm3"]], eng="pool")
                    if VCUT == 4:
                        p.close_scope(); return
                    pa = pAtt[0]
                    p.mm(pa, pa[:, 0:128], Kh, Kh[:, cs], Qh, Qh[:, cs])
                    at = att_sb[ti % 2]
                    p.tt(at, at[:, :], pa, pa[:, 0:128], cmask, cmask[:, :], ALU.mult)
                    if VCUT == 5:
                        p.close_scope(); return
                    po = pO[ti % 2]
                    p.mm(po, po[0:dv, 0:128], vt, vt[:, :], at, at[:, :], start=True, stop=False)
                    for c in range(NCK):
                        cc = slice(ti * 128 + c * CH, ti * 128 + (c + 1) * CH)
                        p.mm(po, po[0:dv, c * CH:(c + 1) * CH], Sbf[cur], Sbf[cur][:, :], Qh, Qh[:, cc],
                             start=False, stop=(c == NCK - 1))
                        pk = pKV[c % 2]
                        if c * CH < 96:
                            p.mm(pk, pk[0:dk, 0:dv], kt, kt[c * CH:(c + 1) * CH, :], vt, vt[c * CH:(c + 1) * CH, :])
                        else:
                            p.mm(pk, pk[0:dk, 0:dv], kt3, kt3[64:128, :], vt, vt[64:128, :])
                        dcol = A3[:, ti * 128 + (c + 1) * CH - 1:ti * 128 + (c + 1) * CH]
                        p.stt(S, S[:, :], S, S[:, :], dprev, pk, pk[0:dk, 0:dv], ALU.mult, ALU.add, reads=[A3])
                        nxt = 1 - cur
                        p.act(Sbf[nxt], Sbf[nxt][:, :], S, S[:, :], AF.Identity, scale=dcol, reads=[A3])
                        cur = nxt
                        dprev = dcol
                    if VCUT == 6:
                        p.close_scope(); return
                    tabgen = drain_gen(tabgen, tab_per_iter)
                    sl, rev = nat_slice(cfg, ti * 128, (ti + 1) * 128, d)
                    if d == 0:
                        p.copy(oacc, oacc[:, sl], po, po[0:dv, 0:128], eng="act")
                    else:
                        oa = oacc.ap[:, sl][:, ::-1]
                        p.tt(oacc, oa, oacc, oa, po, po[0:dv, 0:128], ALU.add)
            if VCUT == 7:
                p.close_scope(); return
            if hgrn:
                p.act(g_raw, g_raw[:, :], g_raw, g_raw[:, :], AF.Sigmoid)
                p.tt(oacc, oacc[:, :], oacc, oacc[:, :], g_raw, g_raw[:, :], ALU.mult)
            else:
                p.act(g_raw, g_raw[:, :], g_raw, g_raw[:, :], AF.Silu)
            p.act(v_raw, v_raw[:, :], oacc, oacc[:, :], AF.Square)
            for (t0, t1) in blocks(Tb, 512):
                p.mm(pX, pX[:, 0:t1 - t0], onesm, onesm[:, :], v_raw, v_raw[:, t0:t1])
                p.act(v_raw, v_raw[:, t0:t1], pX, pX[:, 0:t1 - t0], AF.Ln, bias=float(NORM_EPS))
            p.act(v_raw, v_raw[:, :], v_raw, v_raw[:, :], AF.Exp, scale=-0.5)
            if hgrn:
                p.stt(outb, outb[:, :], oacc, oacc[:, :], gn[:, 0:1], v_raw, v_raw[:, :], ALU.mult, ALU.mult, reads=[gn])
            else:
                p.stt(oacc, oacc[:, :], oacc, oacc[:, :], gn[:, 0:1], v_raw, v_raw[:, :], ALU.mult, ALU.mult, reads=[gn])
                p.tt(outb, outb[:, :], oacc, oacc[:, :], g_raw, g_raw[:, :], ALU.mult)
            p.store(G["MIXT"], G["MIXT"].ap[h * dv:(h + 1) * dv, c0:c0 + Tb], outb, outb[:, :], q="pool")
    drain_gen(tabgen)
    p.close_scope()


def stage_attn(p, cfg, G, io, l):
    Tb, CTX, SEQ = cfg.tb, cfg.ctx, cfg.seq
    j = l // 2
    YT = G["YT"]
    p.open_scope()
    identb = G["ident_bf"]
    onesm = G["ones_mean"]
    cosT = p.sbuf([128, Tb], F32, "cosT")
    sinT = p.sbuf([128, Tb], F32, "sinT")
    p.load(cosT, cosT[:, :], io["cosT"], io["cosT"].ap[:, :])
    p.load(sinT, sinT[:, :], io["sinT"], io["sinT"].ap[:, :])
    onesb = p.sbuf([128, 128], BF16, "onesb")
    p.memset(onesb, onesb[:, :], 1.0)
    gq = p.sbuf([128, 4], F32, "gq")
    p.load(gq, gq[:, :], io["at_normT"], io["at_normT"].ap[j])
    gab = p.sbuf([128, 2], F32, "gab")
    p.act(gab, gab[:, 0:1], gq, gq[:, 0:1], AF.Abs)
    p.act(gab, gab[:, 1:2], gq, gq[:, 2:3], AF.Abs)
    pS = [p.psum([128, 512], F32, f"pS{i}") for i in range(3)]
    pOa = p.psum([128, 512], F32, "pOa")
    pSum = p.psum([128, 512], F32, "pSum")
    pM = p.psum([128, 512], F32, "pM")
    trs = [p.psum([128, 4, 256], BF16, f"pTrA{s}") for s in range(2)]
    ident = G["ident"]
    p.transpose(pM, pM[0:2, 0:128], gab, gab[:, 0:2], ident, ident[:, :])
    gmx = p.sbuf([2, 2], F32, "gmx")
    p.op("dve", lambda e: e.reduce_max(gmx[0:2, 0:1], pM[0:2, 0:128], axis=AX.X), reads=[pM], writes=[gmx])
    gmx2 = p.sbuf([2, 2], F32, "gmx2")
    p.memset(gmx2, gmx2[:, :], 0.0)
    p.tt(gmx2, gmx2[0:2, 0:2], ident, ident[0:2, 0:2], gmx, gmx.ap[0:2, 0:1].broadcast_to([2, 2]), ALU.mult)
    ones2 = p.sbuf([2, 128], F32, "ones2")
    p.memset(ones2, ones2[:, :], 1.0)
    p.mm(pM, pM[:, 0:2], ones2, ones2[:, :], gmx2, gmx2[:, :])
    nshift = p.sbuf([128, 1], F32, "nshift")
    p.copy(gab, gab[:, 0:2], pM, pM[:, 0:2])
    p.stt(nshift, nshift[:, 0:1], gab, gab[:, 0:1], float(-(128 ** 0.5)), gab, gab[:, 1:2], ALU.mult, ALU.mult)
    x_raw = p.sbuf([128, Tb], F32, "x_raw")
    xs_raw = p.sbuf([128, Tb], F32, "xs_raw")
    t1 = p.sbuf([128, Tb], F32, "t1")
    t2 = p.sbuf([128, Tb], F32, "t2")
    qr = p.sbuf([128, 4, Tb], BF16, "qr")
    kr = p.sbuf([128, 2, Tb], BF16, "kr")
    vbf = p.sbuf([128, Tb], BF16, "vbf")
    nkt = Tb // 128
    Vtok = p.sbuf([128, 2, nkt, 128], BF16, "Vtok")
    Pt = [p.sbuf([128, 512], BF16, f"Pt{i}") for i in range(3)]
    osb = p.sbuf([128, 512], F32, "osb")
    rs = p.sbuf([128, 512], F32, "rs")
    obf = [p.sbuf([128, 512], BF16, f"obf{i}") for i in range(2)]
    scale = 128 ** -0.5
    for b in range(cfg.nb):
        c0 = b * Tb
        heads = [("q", h, EV["aq"] + h * 128, EV["aqs"] + h * 128, 0) for h in range(4)] + \
                [("k", g, EV["ak"] + g * 128, EV["aks"] + g * 128, 2) for g in range(2)]
        for (kind, hi, r0, rs0, gc) in heads:
            p.load(x_raw, x_raw[:, :], YT, YT.ap[r0:r0 + 128, c0:c0 + Tb], q="sp")
            p.load(xs_raw, xs_raw[:, :], YT, YT.ap[rs0:rs0 + 128, c0:c0 + Tb], q="act")
            p.act(t1, t1[:, :], x_raw, x_raw[:, :], AF.Square)
            for (a0, a1) in blocks(Tb, 512):
                p.mm(pM, pM[:, 0:a1 - a0], onesm, onesm[:, :], t1, t1[:, a0:a1])
                p.act(t2, t2[:, a0:a1], pM, pM[:, 0:a1 - a0], AF.Ln, bias=float(NORM_EPS))
            p.act(t2, t2[:, :], t2, t2[:, :], AF.Exp, scale=-0.5)
            p.stt(t1, t1[:, :], x_raw, x_raw[:, :], gq[:, gc:gc + 1], cosT, cosT[:, :], ALU.mult, ALU.mult, reads=[gq])
            p.stt(x_raw, x_raw[:, :], xs_raw, xs_raw[:, :], gq[:, gc + 1:gc + 2], sinT, sinT[:, :], ALU.mult, ALU.mult,
                  reads=[gq])
            p.tt(t1, t1[:, :], t1, t1[:, :], x_raw, x_raw[:, :], ALU.add, eng="pool")
            dst = qr if kind == "q" else kr
            p.tt(dst, dst[:, hi, :], t1, t1[:, :], t2, t2[:, :], ALU.mult)
        for g in range(2):
            p.load(x_raw, x_raw[:, :], YT, YT.ap[EV["av"] + g * 128:EV["av"] + (g + 1) * 128, c0:c0 + Tb], q="sp")
            p.copy(vbf, vbf[:, :], x_raw, x_raw[:, :], eng="pool")
            for kt in range(nkt):
                tr = trs[kt % 2]
                p.transpose(tr, tr[:, 0, 0:128], vbf, vbf[:, kt * 128:(kt + 1) * 128], identb, identb[:, :])
                p.copy(Vtok, Vtok[:, g, kt, :], tr, tr[:, 0, 0:128], eng="act" if kt % 2 else "dve")
        it = 0
        for h in range(4):
            g = h // 2
            qblocks = [(a0, a1, 0, CTX // 128) for (a0, a1) in blocks(CTX, 512)] + \
                      [(CTX + a0, CTX + a1, 0, nkt) for (a0, a1) in blocks(SEQ, 512)]
            for (q0, q1, k0, k1) in qblocks:
                w = q1 - q0
                for kt in range(k0, k1):
                    ps = pS[it % 3]
                    pt = Pt[it % 3]
                    it += 1
                    p.mm(ps, ps[:, 0:w], kr, kr[:, g, kt * 128:(kt + 1) * 128], qr, qr[:, h, q0:q1])
                    p.act(pt, pt[:, 0:w], ps, ps[:, 0:w], AF.Exp, bias=nshift[:, 0:1], scale=float(scale), reads=[nshift])
                    p.mm(pOa, pOa[:, 0:w], Vtok, Vtok[:, g, kt, :], pt, pt[:, 0:w], start=(kt == k0), stop=(kt == k1 - 1))
                    p.mm(pSum, pSum[:, 0:w], onesb, onesb[:, :], pt, pt[:, 0:w], start=(kt == k0), stop=(kt == k1 - 1))
                p.op("dve", lambda e, w=w: e.reciprocal(rs[:, 0:w], pSum[:, 0:w]), reads=[pSum], writes=[rs])
                ob = obf[(it // 1) % 2]
                p.tt(ob, ob[:, 0:w], pOa, pOa[:, 0:w], rs, rs[:, 0:w], ALU.mult)
                p.store(G["MIXT"], G["MIXT"].ap[512 + h * 128:512 + (h + 1) * 128, c0 + q0:c0 + q1], ob, ob[:, 0:w], q="pool")
    p.close_scope()


NEG = -1.0e30

def peer_tables_gen(p, cfg, G, io, l):
    NE = PEER_NKEYS * PEER_NKEYS
    su = [p.sbuf([128, D], F32, f"tsu{i}") for i in range(2)]
    sv = [p.sbuf([128, D], F32, f"tsv{i}") for i in range(2)]
    ob = [p.sbuf([128, 2 * D], BF16, f"tob{i}") for i in range(2)]
    u_tab = io[f"peer_u{l}"].ap
    v_tab = io[f"peer_v{l}"].ap
    for rb in range(NE // 128):
        a, b_, o = su[rb % 2], sv[rb % 2], ob[rb % 2]
        p.load(a, a[:, :], G["ext"], u_tab[rb * 128:(rb + 1) * 128, :], q="sp")
        p.load(b_, b_[:, :], G["ext"], v_tab[rb * 128:(rb + 1) * 128, :], q="act")
        p.copy(o, o[:, 0:D], a, a[:, :], eng="act")
        p.copy(o, o[:, D:2 * D], b_, b_[:, :], eng="dve" if rb % 2 else "pool")
        p.store(G["UVB"], G["UVB"].ap[rb * 128:(rb + 1) * 128, :], o, o[:, :], q="sp")
        yield


def drain_gen(g, n=None):
    if g is None:
        return None
    try:
        if n is None:
            while True:
                next(g)
        for _ in range(n):
            next(g)
    except StopIteration:
        return None
    return g


def stage_peer(p, cfg, G, io, l):
    import os
    T = cfg.T
    H8, NK, TK = PEER_HEADS, PEER_NKEYS, PEER_TOPK
    NQ = H8 * 2 * NK
    p.open_scope()
    ident = G["ident"]
    wq_bf = p.sbuf([128, KC, NQ], BF16, "wq_bf")
    p.open_scope()
    wst = [p.sbuf([128, NQ], F32, f"wqst{i}") for i in range(2)]
    for kc in range(KC):
        st = wst[kc % 2]
        p.load(st, st[:, :], G["ext"], io["peer_wq"].ap[l, kc * 128:(kc + 1) * 128, :], q="sp" if kc % 2 else "act")
        p.copy(wq_bf, wq_bf[:, kc, :], st, st[:, :], eng="pool" if kc % 2 else "dve")
    sk = p.sbuf([128, 16 * NK], F32, "sk")
    p.load(sk, sk[:, :], G["ext"], io["peer_subkT"].ap[l])
    p.close_scope()
    sk_bf = p.sbuf([128, 16, NK], BF16, "sk_bf")
    p.open_scope()
    sk2 = p.sbuf([128, 16 * NK], F32, "sk2")
    p.load(sk2, sk2[:, :], G["ext"], io["peer_subkT"].ap[l])
    p.copy(sk_bf, sk_bf.ap[:, :, :].rearrange("p a b -> p (a b)"), sk2, sk2[:, :])
    p.close_scope()
    scb, shb = [], []
    for r in range(cfg.R):
        a = p.sbuf([128, D], F32, f"sc2b{r}")
        b_ = p.sbuf([128, D], F32, f"sh2b{r}")
        p.load(a, a[:, :], G["modtok"], G["modtok"].ap[l, r:r + 1, 4 * D:5 * D].partition_broadcast(128))
        p.load(b_, b_[:, :], G["modtok"], G["modtok"].ap[l, r:r + 1, 3 * D:4 * D].partition_broadcast(128))
        p.ts(a, a[:, :], a, a[:, :], 1.0, None, ALU.add)
        scb.append(a)
        shb.append(b_)
    TB = 2
    hst = [p.sbuf([128, D], F32, f"phst{i}") for i in range(2)]
    hm = p.sbuf([128, 4, D], F32, "hm_tok")
    hmT = p.sbuf([128, KC, TB * 128], BF16, "hmT")
    qT = p.sbuf([128, 16, TB * 128], BF16, "qT")
    pT = [p.psum([128, 4, 128], F32, "ppT")] * 2
    pQ = [p.psum([128, 512], F32, "ppQ")] * 2
    pS = [p.psum([128, 4, 128], F32, f"ppS{i}") for i in range(2)] * 2
    pHM = p.psum([128, 1024], F32, "pHM")
    pACC = [p.psum([128, 512], F32, f"pACC{i}") for i in range(2)]
    s_sb = p.sbuf([128, 16, NK], F32, "s_sb")
    s_wk = p.sbuf([128, NK], F32, "s_wk")
    ts16 = p.sbuf([128, 16, TK], F32, "ts16")
    ti16 = p.sbuf([128, 16, TK], U32, "ti16")
    ti16f = p.sbuf([128, 16, TK], F32, "ti16f")
    cand = p.sbuf([128, H8, TK * TK], F32, "cand")
    cwk = p.sbuf([128, TK * TK], F32, "cwk")
    eid = p.sbuf([128, H8, TK * TK], F32, "eid")
    best = p.sbuf([128, H8, TK], F32, "best")
    gate = p.sbuf([128, H8, TK], F32, "gate")
    gsum = p.sbuf([128, H8], F32, "gsum")
    idxf = p.sbuf([128, H8 * TK], F32, "idxf")
    idxu = p.sbuf([128, H8 * TK], U32, "idxu")
    aval = p.sbuf([128, H8 * TK], F32, "aval")
    coef = p.sbuf([128, H8 * TK], F32, "coef")
    tmpa = p.sbuf([128, H8 * TK], F32, "tmpa")
    junk = p.sbuf([128, D], BF16, "junk")
    NUB = int(os.environ.get("NUB", "12"))
    GRP = 4
    ub = [p.sbuf([128, 2 * D], BF16, f"ub{i}") for i in range(NUB)]
    dgs = [p.sbuf([128, 128], BF16, f"dg{i}") for i in range(8)]
    osb = p.sbuf([128, D], F32, "posb")
    identb = G["ident_bf"]
    UVB = G["UVB"]
    nslot = H8 * TK
    ntiles = T // 128
    idxus = [idxu, p.sbuf([128, H8 * TK], U32, "idxu_b")]
    gates = [gate, p.sbuf([128, H8, TK], F32, "gate_b")]
    posu = p.sbuf([128, H8, TK], U32, "posu")
    xg = p.sbuf([128, H8 * TK], F32, "xg")
    abu = p.sbuf([128, 2, H8 * TK], U32, "abu")
    abf = p.sbuf([128, 2, H8 * TK], F32, "abf")
    ijf = p.sbuf([128, 2, H8 * TK], F32, "ijf")
    iota16 = p.sbuf([128, TK], F32, "iota16")
    p.load(iota16, iota16[:, :], G["ext"], io["iota16"].ap[:, :])

    def producer(ti):
        t0 = (ti // TB) * TB * 128
        t1 = min(t0 + TB * 128, T)
        ntl = (t1 - t0) // 128
        w = t1 - t0
        tl = ti % TB
        if tl == 0:
            for tl2 in range(ntl):
                tj = ti + tl2
                r = cfg.seg_of_tile(tj)
                hs = hst[tj % 2]
                slot = tj % 4
                p.load(hs, hs[:, :], G["H"], G["H"].ap[tj * 128:(tj + 1) * 128, :], q="sp")
                p.tt(hm, hm[:, slot, :], hs, hs[:, :], scb[r], scb[r][:, :], ALU.mult)
                p.tt(hm, hm[:, slot, :], hm, hm[:, slot, :], shb[r], shb[r][:, :], ALU.add)
                yield
                for half in range(2):
                    pt = pT[half]
                    for kk in range(4):
                        kc = half * 4 + kk
                        p.transpose(pt, pt[:, kk, :], hm, hm[:, slot, kc * 128:(kc + 1) * 128], ident, ident[:, :])
                    p.copy(hmT, hmT[:, half * 4:(half + 1) * 4, tl2 * 128:(tl2 + 1) * 128], pt, pt[:, :, :],
                           eng="act" if half else "dve")
                    yield
            for hsx in range(16):
                pq = pQ[hsx % 2]
                for kc in range(KC):
                    p.mm(pq, pq[:, 0:w], wq_bf, wq_bf[:, kc, hsx * NK:(hsx + 1) * NK], hmT, hmT[:, kc, 0:w],
                         start=(kc == 0), stop=(kc == KC - 1))
                p.copy(qT, qT[:, hsx, 0:w], pq, pq[:, 0:w], eng="act" if hsx % 2 else "dve")
                yield
        idxu_, gate_ = idxus[ti % 2], gates[ti % 2]
        for q4 in range(4):
            ps = pS[q4]
            for k4 in range(4):
                hsx = q4 * 4 + k4
                p.mm(ps, ps[:, k4, :], qT, qT[:, hsx, tl * 128:(tl + 1) * 128], sk_bf, sk_bf[:, hsx, :])
            p.copy(s_sb, s_sb[:, q4 * 4:(q4 + 1) * 4, :], ps, ps[:, :, :], eng="act" if q4 % 2 else "dve")
            yield
        for hsx in range(16):
            src_ = s_sb[:, hsx, :]
            p.op("dve", lambda e, hsx=hsx, src_=src_: e.max(out=ts16[:, hsx, 0:8], in_=src_), reads=[s_sb], writes=[ts16])
            p.op("dve", lambda e, hsx=hsx, src_=src_: e.max_index(ti16[:, hsx, 0:8], ts16[:, hsx, 0:8], src_),
                 reads=[s_sb, ts16], writes=[ti16])
            p.op("dve", lambda e, hsx=hsx, src_=src_: e.match_replace(out=s_wk[:, :], in_to_replace=ts16[:, hsx, 0:8],
                                                                      in_values=src_, imm_value=NEG),
                 reads=[s_sb, ts16], writes=[s_wk])
            p.op("dve", lambda e, hsx=hsx: e.max(out=ts16[:, hsx, 8:16], in_=s_wk[:, :]), reads=[s_wk], writes=[ts16])
            p.op("dve", lambda e, hsx=hsx: e.max_index(ti16[:, hsx, 8:16], ts16[:, hsx, 8:16], s_wk[:, :]),
                 reads=[s_wk, ts16], writes=[ti16])
            yield
        p.copy(ti16f, ti16f.ap[:, :, :].rearrange("p a b -> p (a b)"), ti16, ti16.ap[:, :, :].rearrange("p a b -> p (a b)"))
        ts4 = ts16.ap[:, :, :].rearrange("p (h s) k -> p h s k", s=2)
        ti4 = ti16f.ap[:, :, :].rearrange("p (h s) k -> p h s k", s=2)
        c4 = cand.ap[:, :, :].rearrange("p h (a b) -> p h a b", a=TK)
        e4 = eid.ap[:, :, :].rearrange("p h (a b) -> p h a b", a=TK)
        for h in range(H8):
            a_b = ts4[:, h, 0, :].unsqueeze(2).broadcast_to([128, TK, TK])
            b_b = ts4[:, h, 1, :].unsqueeze(1).broadcast_to([128, TK, TK])
            p.tt(cand, c4[:, h, :, :], ts16, a_b, ts16, b_b, ALU.add)
            yield
        for h in range(H8):
            src_ = cand[:, h, :]
            p.op("dve", lambda e, h=h, src_=src_: e.max(out=best[:, h, 0:8], in_=src_), reads=[cand], writes=[best])
            p.op("dve", lambda e, h=h, src_=src_: e.match_replace(out=cwk[:, :], in_to_replace=best[:, h, 0:8],
                                                                  in_values=src_, imm_value=NEG),
                 reads=[cand, best], writes=[cwk])
            p.op("dve", lambda e, h=h, src_=src_: e.max_index(posu[:, h, 0:8], best[:, h, 0:8], src_),
                 reads=[cand, best], writes=[posu])
            p.op("dve", lambda e, h=h: e.max(out=best[:, h, 8:16], in_=cwk[:, :]), reads=[cwk], writes=[best])
            p.op("dve", lambda e, h=h: e.max_index(posu[:, h, 8:16], best[:, h, 8:16], cwk[:, :]),
                 reads=[cwk, best], writes=[posu])
            yield
        mx_b = best.ap[:, :, 0:1].broadcast_to([128, H8, TK])
        p.tt(gate_, gate_[:, :, :], best, best[:, :, :], best, mx_b, ALU.subtract)
        p.act(gate_, gate_[:, :, :], gate_, gate_[:, :, :], AF.Exp)
        p.op("dve", lambda e: e.tensor_reduce(out=gsum[:, :], in_=gate_[:, :, :], axis=AX.X, op=ALU.add),
             reads=[gate_], writes=[gsum])
        p.op("dve", lambda e: e.reciprocal(gsum[:, :], gsum[:, :]), reads=[gsum], writes=[gsum])
        p.tt(gate_, gate_[:, :, :], gate_, gate_[:, :, :], gsum, gsum.ap[:, :].unsqueeze(2).broadcast_to([128, H8, TK]), ALU.mult)
        yield
        pflat = posu.ap[:, :, :].rearrange("p a b -> p (a b)")
        p.op("dve", lambda e: e.tensor_single_scalar(abu[:, 0, :], pflat, 4, op=ALU.logical_shift_right),
             reads=[posu], writes=[abu])
        p.op("dve", lambda e: e.tensor_single_scalar(abu[:, 1, :], pflat, 15, op=ALU.bitwise_and),
             reads=[posu], writes=[abu])
        p.copy(abf, abf.ap[:, :, :].rearrange("p a b -> p (a b)"), abu, abu.ap[:, :, :].rearrange("p a b -> p (a b)"))
        yield
        oh4 = eid.ap[:, :, :].rearrange("p h (k a) -> p h k a", k=TK)
        oh3 = eid.ap[:, :, :].rearrange("p h (k a) -> p (h k) a", k=TK)
        io16 = iota16.ap[:, :].unsqueeze(1).unsqueeze(1).broadcast_to([128, H8, TK, TK])
        for half in range(2):
            sel_ = abf.ap[:, half, :].rearrange("p (h k) -> p h k", h=H8).unsqueeze(3).broadcast_to([128, H8, TK, TK])
            tab_ = ti4[:, :, half, :].unsqueeze(2).broadcast_to([128, H8, TK, TK])
            p.tt(eid, oh4, abf, sel_, iota16, io16, ALU.is_equal)
            p.tt(eid, oh4, eid, oh4, ti16f, tab_, ALU.mult)
            yield
            p.op("dve", lambda e, half=half: e.tensor_reduce(out=ijf[:, half, :], in_=oh3, axis=AX.X, op=ALU.add),
                 reads=[eid], writes=[ijf])
            yield
        p.stt(idxf, idxf[:, :], ijf, ijf[:, 0, :], float(NK), ijf, ijf[:, 1, :], ALU.mult, ALU.add)
        p.ts(idxf, idxf[:, :], idxf, idxf[:, :], 0.0, float(NK * NK - 1), ALU.max, ALU.min)
        p.copy(idxu_, idxu_[:, :], idxf, idxf[:, :])
        yield

    def drain(g, n=None):
        if g is None:
            return None
        try:
            if n is None:
                while True:
                    next(g)
            for _ in range(n):
                next(g)
        except StopIteration:
            return None
        return g

    gi = 0
    drain(producer(0))
    for ti in range(ntiles):
        nxt = producer(ti + 1) if ti + 1 < ntiles else None
        idxu_, gate_ = idxus[ti % 2], gates[ti % 2]
        slot = ti % 4
        for hf in range(2):
            p.mm(pHM, pHM[:, hf * 512:(hf + 1) * 512], ident, ident[:, :], hm, hm[:, slot, hf * 512:(hf + 1) * 512])
        gflat = gate_.ap[:, :, :].rearrange("p a b -> p (a b)")
        for s0 in range(0, nslot, GRP):
            bufs = []
            for s in range(s0, s0 + GRP):
                u = ub[gi % NUB]
                gi += 1
                bufs.append(u)
                p.dma("pool", u, u[:, :], UVB, None, extra_reads=[idxu_],
                      fn=lambda e, u=u, s=s, idxu_=idxu_: e.indirect_dma_start(
                          out=u[:, :], out_offset=None, in_=UVB.ap,
                          in_offset=bass.IndirectOffsetOnAxis(ap=idxu_[:, s:s + 1], axis=0)))
                p.stt(junk, junk[:, :], u, u[:, 0:D], 1.0, pHM, pHM[:, :], ALU.mult, ALU.mult,
                      accum=aval[:, s:s + 1], accum_b=aval)
            sl = slice(s0, s0 + GRP)
            p.stt(tmpa, tmpa[:, sl], aval, aval[:, sl], 0.044715, aval, aval[:, sl], ALU.mult, ALU.mult)
            p.stt(tmpa, tmpa[:, sl], tmpa, tmpa[:, sl], 1.0, aval, aval[:, sl], ALU.add, ALU.mult)
            p.act(tmpa, tmpa[:, sl], tmpa, tmpa[:, sl], AF.Sigmoid, scale=float(2.0 * (2.0 / np.pi) ** 0.5))
            p.tt(xg, xg[:, sl], aval, aval[:, sl], gate_, gflat[:, sl], ALU.mult)
            p.tt(coef, coef[:, sl], tmpa, tmpa[:, sl], xg, xg[:, sl], ALU.mult)
            for k_, s in enumerate(range(s0, s0 + GRP)):
                u = bufs[k_]
                dg = dgs[s % 8]
                p.act(dg, dg[:, :], identb, identb[:, :], AF.Identity, scale=coef[:, s:s + 1], reads=[coef])
                for hf in range(2):
                    p.mm(pACC[hf], pACC[hf][:, :], dg, dg[:, :], u, u[:, D + hf * 512:D + (hf + 1) * 512],
                         start=(s == 0), stop=(s == nslot - 1))
            nxt = drain(nxt, 4)
        drain(nxt)
        p.copy(osb, osb[:, 0:512], pACC[0], pACC[0][:, :], eng="act")
        p.copy(osb, osb[:, 512:1024], pACC[1], pACC[1][:, :], eng="dve")
        p.store(G["PEERO"], G["PEERO"].ap[ti * 128:(ti + 1) * 128, :], osb, osb[:, :], q="sp")
    p.close_scope()


def stage_ssd(p, cfg, G, io, l):
    Tb, CTX = cfg.tb, cfg.ctx
    j = l // 2
    YT = G["YT"]
    XSF = G["XSF"]
    YPRE = G["YPRE"]
    nt = Tb // 128
    segs = [(0, CTX), (CTX, Tb)]
    p.open_scope()
    identb, ident = G["ident_bf"], G["ident"]
    cw = p.sbuf([128, 8, 5], F32, "convw")
    p.load(cw, cw.ap[:, :, :].rearrange("p a b -> p (a b)"), G["ext"], io["ssd_conv_wT"].ap[j])
    cb = p.sbuf([128, 8], F32, "convb")
    p.load(cb, cb[:, :], G["ext"], io["ssd_conv_bT"].ap[j])
    dtb = p.sbuf([8, 2], F32, "dtb")
    p.load(dtb, dtb[:, :], G["ext"], io["ssd_dt_biasT"].ap[j])
    nA = p.sbuf([8, 2], F32, "nA")
    p.load(nA, nA[:, :], G["ext"], io["ssd_a_logT"].ap[j])
    p.act(nA, nA[:, :], nA, nA[:, :], AF.Exp)
    p.ts(nA, nA[:, :], nA, nA[:, :], -1.0, None, ALU.mult)
    dsk = p.sbuf([128, 4], F32, "dskip")
    p.load(dsk, dsk[:, :], G["ext"], io["ssd_dT"].ap[j])
    gnm = p.sbuf([128, 4], F32, "ssd_g")
    p.load(gnm, gnm[:, :], G["ext"], io["ssd_norm_gT"].ap[j])
    sel = p.sbuf([8, 8, 128], F32, "sel")
    p.load(sel, sel.ap[:, :, :].rearrange("p a b -> p (a b)"), G["ext"], io["selT"].ap[:, :])
    mbias = p.sbuf([128, 128], F32, "mbias")
    p.load(mbias, mbias[:, :], G["ext"], io["mbias"].ap[:, :])
    rm128 = p.sbuf([8, Tb], F32, "rm128")
    p.load(rm128, rm128[:, :], G["ext"], io["rmask128"].ap[:, :])
    ones256 = p.sbuf([128, 128], F32, "ones256")
    p.memset(ones256, ones256[:, :], 1.0 / 256.0)
    pTrX = p.psum([128, 1024], BF16, "pTrX")
    pTrB = p.psum([128, 1024], BF16, "pTrB")
    pTrD = p.psum([128, 512], F32, "pTrD")
    pCR = p.psum([128, 2, 256], F32, "pCR")
    pSc = p.psum([128, 512], F32, "pSc")
    pY = p.psum([128, 512], F32, "pY")
    pKV = p.psum([128, 512], F32, "pKV")
    pN = p.psum([128, 512], F32, "pN")
    XCb = p.sbuf([128, 8, Tb], BF16, "XCb")
    raw = [p.sbuf([128, Tb], F32, f"sraw{i}") for i in range(2)]
    cacc = p.sbuf([128, Tb], F32, "cacc")
    dtT = [p.sbuf([8, Tb], F32, f"dtT{d}") for d in range(2)]
    cumT = [p.sbuf([8, Tb], F32, f"cumT{d}") for d in range(2)]
    t8a = p.sbuf([8, Tb], F32, "t8a")
    t8b = p.sbuf([8, Tb], F32, "t8b")
    Xd = p.sbuf([128, 3, Tb], BF16, "Xd")
    yacc = p.sbuf([128, Tb], F32, "yacc")
    xs_tok = [p.sbuf([128, 128], BF16, f"xs_tok{i}") for i in range(2)]
    B_tok = [p.sbuf([128, 128], BF16, f"B_tok{i}") for i in range(2)]
    dc_tok = [p.sbuf([128, 16], F32, f"dc_tok{i}") for i in range(2)]
    crow = [p.sbuf([128, 2, 128], F32, f"crow{i}") for i in range(2)]
    sc_sb = [p.sbuf([128, 128], F32, f"sc_sb{i}") for i in range(2)]
    D1 = [p.sbuf([128, 128], F32, f"D1{i}") for i in range(2)]
    attT = [p.sbuf([128, 128], BF16, f"attT{i}") for i in range(2)]
    ec = [p.sbuf([128, 128], F32, f"ec{i}") for i in range(2)]
    Chat = [p.sbuf([128, 128], BF16, f"Chat{i}") for i in range(2)]
    w2 = p.sbuf([128, 2], F32, "w2")
    el2 = p.sbuf([128, 2], F32, "el2")
    xhat = [p.sbuf([128, 2, 64], BF16, f"xhat{i}") for i in range(2)]
    S2 = p.sbuf([128, 2, 64], F32, "S2")
    Sbf = [p.sbuf([128, 2, 64], BF16, f"S2bf{i}") for i in range(2)]
    obf = p.sbuf([128, Tb], BF16, "ssd_obf")
    for b in range(cfg.nb):
        c0 = b * Tb
        for ch in range(8):
            r0 = OD["xs"] + ch * 128
            rw = raw[ch % 2]
            p.load(rw, rw[:, :], YT, YT.ap[r0:r0 + 128, c0:c0 + Tb], q="sp" if ch % 2 else "act")
            for (s0, s1) in segs:
                p.ts(cacc, cacc[:, s0:s1], rw, rw[:, s0:s1], cw[:, ch, 2:3], None, ALU.mult, reads=[cw])
                for k in (0, 1, 3, 4):
                    sh = k - 2
                    o0, o1 = max(s0, s0 - sh), min(s1, s1 - sh)
                    p.stt(cacc, cacc[:, o0:o1], rw, rw[:, o0 + sh:o1 + sh], cw[:, ch, k:k + 1], cacc, cacc[:, o0:o1],
                          ALU.mult, ALU.add, reads=[cw])
            if ch < 4:
                p.act(cacc, cacc[:, :], cacc, cacc[:, :], AF.Silu, bias=cb[:, ch:ch + 1], reads=[cb])
                p.copy(XCb, XCb[:, ch, :], cacc, cacc[:, :], eng="pool")
                p.store(XSF, XSF.ap[ch * 128:(ch + 1) * 128, c0:c0 + Tb], cacc, cacc[:, :], q="sp")
            else:
                p.act(XCb, XCb[:, ch, :], cacc, cacc[:, :], AF.Silu, bias=cb[:, ch:ch + 1], reads=[cb])
        for d in range(2):
            r0 = OD["dtf"] if d == 0 else OD["dtb"]
            p.load(t8a, t8a[:, :], YT, YT.ap[r0:r0 + 8, c0:c0 + Tb], q="sp")
            for (s0, s1) in segs:
                p.ts(t8b, t8b[:, s0:s1], t8a, nat_ap(t8a.ap, cfg, s0, s1, d), dtb[:, d:d + 1], None, ALU.add, reads=[dtb])
            p.act(t8a, t8a[:, :], t8b, t8b[:, :], AF.Abs)
            p.act(t8a, t8a[:, :], t8a, t8a[:, :], AF.Exp, scale=-1.0)
            p.act(t8a, t8a[:, :], t8a, t8a[:, :], AF.Ln, bias=1.0)
            p.ts(t8b, t8b[:, :], t8b, t8b[:, :], 0.0, None, ALU.max)
            p.tt(dtT[d], dtT[d][:, :], t8a, t8a[:, :], t8b, t8b[:, :], ALU.add)
            p.ts(t8a, t8a[:, :], dtT[d], dtT[d][:, :], nA[:, d:d + 1], None, ALU.mult, reads=[nA])
            p.op("dve", lambda e, d=d: e.tensor_tensor_scan(cumT[d][:, :], rm128[:, :], t8a[:, :], 0.0, ALU.mult, ALU.add),
                 reads=[rm128, t8a], writes=[cumT[d]])
        for cc in range(4):
            g = cc // 2
            for d in range(2):
                for (s0, s1) in segs:
                    for k_, src_ch in enumerate((cc, 4 + g, 6 + g)):
                        p.copy(Xd, Xd[:, k_, s0:s1], XCb, nat_ap(XCb.ap[:, src_ch, :], cfg, s0, s1, d),
                               eng=("pool", "act", "dve")[k_])
                p.memset(S2, S2.ap[:, :, :].rearrange("p a b -> p (a b)"), 0.0)
                p.memset(Sbf[0], Sbf[0].ap[:, :, :].rearrange("p a b -> p (a b)"), 0.0)
                cur = 0
                for ti in range(nt):
                    cs = slice(ti * 128, (ti + 1) * 128)
                    i2 = ti % 2
                    p.transpose(pTrX, pTrX[:, 0:128], Xd, Xd[:, 0, cs], identb, identb[:, :])
                    p.transpose(pTrB, pTrB[:, 0:128], Xd, Xd[:, 1, cs], identb, identb[:, :])
                    p.copy(xs_tok[i2], xs_tok[i2][:, :], pTrX, pTrX[:, 0:128], eng="act")
                    p.copy(B_tok[i2], B_tok[i2][:, :], pTrB, pTrB[:, 0:128], eng="dve")
                    p.transpose(pTrD, pTrD[:, 0:8], dtT[d], dtT[d][:, cs], ident, ident[0:8, 0:8])
                    p.transpose(pTrD, pTrD[:, 8:16], cumT[d], cumT[d][:, cs], ident, ident[0:8, 0:8])
                    dc = dc_tok[i2]
                    p.copy(dc, dc[:, :], pTrD, pTrD[:, 0:16], eng="dve")
                    for hh in range(2):
                        p.mm(pCR, pCR[:, hh, 0:128], sel, sel[:, 2 * cc + hh, :], cumT[d], cumT[d][:, cs])
                    cr = crow[i2]
                    p.copy(cr, cr[:, :, :], pCR, pCR[:, :, 0:128], eng="act")
                    p.mm(pSc, pSc[:, 0:128], Xd, Xd[:, 1, cs], Xd, Xd[:, 2, cs])
                    sc = sc_sb[i2]
                    p.copy(sc, sc[:, :], pSc, pSc[:, 0:128], eng="dve")
                    for hh in range(2):
                        h = 2 * cc + hh
                        p.stt(D1[hh], D1[hh][:, :], cr, cr[:, hh, :], dc[:, 8 + h:9 + h], mbias, mbias[:, :],
                              ALU.subtract, ALU.add, reads=[dc])
                        p.act(D1[hh], D1[hh][:, :], D1[hh], D1[hh][:, :], AF.Exp)
                        p.stt(attT[hh], attT[hh][:, :], D1[hh], D1[hh][:, :], dc[:, h:h + 1], sc, sc[:, :],
                              ALU.mult, ALU.mult, reads=[dc])
                        p.act(ec[hh], ec[hh][:, :], cr, cr[:, hh, :], AF.Exp)
                        p.tt(Chat[hh], Chat[hh][:, :], Xd, Xd[:, 2, cs], ec[hh], ec[hh][:, :], ALU.mult, eng="pool")
                        p.mm(pY, pY[hh * 64:(hh + 1) * 64, 0:128], xs_tok[i2], xs_tok[i2][:, hh * 64:(hh + 1) * 64],
                             attT[hh], attT[hh][:, :], start=True, stop=False)
                        p.mm(pY, pY[hh * 64:(hh + 1) * 64, 0:128], Sbf[cur], Sbf[cur][:, hh, :],
                             Chat[hh], Chat[hh][:, :], start=False, stop=True)
                    lastb = cr[:, :, 127]
                    p.tt(w2, w2[:, :], cr, lastb, dc, dc[:, 8 + 2 * cc:10 + 2 * cc], ALU.subtract)
                    p.act(w2, w2[:, :], w2, w2[:, :], AF.Exp)
                    p.tt(w2, w2[:, :], w2, w2[:, :], dc, dc[:, 2 * cc:2 * cc + 2], ALU.mult)
                    p.act(el2, el2[:, :], cr, lastb, AF.Exp)
                    xh = xhat[i2]
                    p.tt(xh, xh[:, :, :], xs_tok[i2], xs_tok[i2].ap[:, :].rearrange("p (a b) -> p a b", a=2),
                         w2, w2.ap[:, :].unsqueeze(2).broadcast_to([128, 2, 64]), ALU.mult)
                    p.mm(pKV, pKV[:, 0:128], B_tok[i2], B_tok[i2][:, :], xh, xh.ap[:, :, :].rearrange("p a b -> p (a b)"))
                    p.tt(S2, S2[:, :, :], S2, S2[:, :, :], el2, el2.ap[:, :].unsqueeze(2).broadcast_to([128, 2, 64]), ALU.mult)
                    S2f = S2.ap[:, :, :].rearrange("p a b -> p (a b)")
                    p.tt(S2, S2f, S2, S2f, pKV, pKV[:, 0:128], ALU.add)
                    nxt = 1 - cur
                    p.copy(Sbf[nxt], Sbf[nxt].ap[:, :, :].rearrange("p a b -> p (a b)"), S2, S2f, eng="act")
                    cur = nxt
                    sl, rev = nat_slice(cfg, ti * 128, (ti + 1) * 128, d)
                    if d == 0:
                        p.copy(yacc, yacc[:, sl], pY, pY[:, 0:128], eng="act")
                    else:
                        ya = yacc.ap[:, sl][:, ::-1]
                        p.tt(yacc, ya, yacc, ya, pY, pY[:, 0:128], ALU.add)
            p.load(raw[0], raw[0][:, :], XSF, XSF.ap[cc * 128:(cc + 1) * 128, c0:c0 + Tb], q="sp")
            p.load(raw[1], raw[1][:, :], YT, YT.ap[OD["z"] + cc * 128:OD["z"] + (cc + 1) * 128, c0:c0 + Tb], q="act")
            p.stt(yacc, yacc[:, :], raw[0], raw[0][:, :], dsk[:, cc:cc + 1], yacc, yacc[:, :], ALU.mult, ALU.add, reads=[dsk])
            p.act(raw[1], raw[1][:, :], raw[1], raw[1][:, :], AF.Silu)
            p.tt(yacc, yacc[:, :], yacc, yacc[:, :], raw[1], raw[1][:, :], ALU.mult)
            p.store(YPRE, YPRE.ap[cc * 128:(cc + 1) * 128, c0:c0 + Tb], yacc, yacc[:, :], q="sp")
        for g in range(2):
            for k_ in range(2):
                p.load(raw[k_], raw[k_][:, :], YPRE, YPRE.ap[(2 * g + k_) * 128:(2 * g + k_ + 1) * 128, c0:c0 + Tb],
                       q="sp" if k_ else "act")
            p.act(cacc, cacc[:, :], raw[0], raw[0][:, :], AF.Square)
            p.act(yacc, yacc[:, :], raw[1], raw[1][:, :], AF.Square)
            for (a0, a1) in blocks(Tb, 512):
                p.mm(pN, pN[:, 0:a1 - a0], ones256, ones256[:, :], cacc, cacc[:, a0:a1], start=True, stop=False)
                p.mm(pN, pN[:, 0:a1 - a0], ones256, ones256[:, :], yacc, yacc[:, a0:a1], start=False, stop=True)
                p.act(cacc, cacc[:, a0:a1], pN, pN[:, 0:a1 - a0], AF.Ln, bias=float(NORM_EPS))
            p.act(cacc, cacc[:, :], cacc, cacc[:, :], AF.Exp, scale=-0.5)
            for k_ in range(2):
                p.stt(obf, obf[:, :], raw[k_], raw[k_][:, :], gnm[:, 2 * g + k_:2 * g + k_ + 1], cacc, cacc[:, :],
                      ALU.mult, ALU.mult, reads=[gnm])
                p.store(G["MIXT"], G["MIXT"].ap[512 + (2 * g + k_) * 128:512 + (2 * g + k_ + 1) * 128, c0:c0 + Tb],
                        obf, obf[:, :], q="pool")
    p.close_scope()


def kernel(**inputs):
    x = np.asarray(inputs["x"])
    B, SEQ, _ = x.shape
    CTX = np.asarray(inputs["ctx"]).shape[1]
    depth = np.asarray(inputs["mod_w"]).shape[0]
    ncores = NCORES if B % NCORES == 0 else 1
    cfg = Cfg(B // ncores, SEQ, CTX, depth)
    p = build_program(cfg)
    nc = p.finish()
    maps = prep_inputs(inputs, cfg, ncores)
    res = run_bass_kernel_spmd(nc, maps, core_ids=list(range(ncores))).results
    out = np.empty((B, SEQ, D), np.float32)
    for ci in range(ncores):
        o = res[ci]["out"].reshape(cfg.nb, cfg.tb, D)
        for bl in range(cfg.nb):
            out[ci * cfg.nb + bl] = o[bl, CTX:, :]
    return out
```

```python
import numpy as np
from contextlib import ExitStack
import concourse.bass as bass
import concourse.mybir as mybir
from concourse.bass_utils import run_bass_kernel_spmd

F32 = mybir.dt.float32
BF16 = mybir.dt.bfloat16
U32 = mybir.dt.uint32
I32 = mybir.dt.int32
AF = mybir.ActivationFunctionType
ALU = mybir.AluOpType
AX = mybir.AxisListType

NCORES = 8
ENGS = ("pe", "dve", "act", "pool", "sp")


class Buf:
    __slots__ = ("ap", "name", "w", "r", "sem", "is_dram")

    def __init__(self, ap, name, is_dram=False):
        self.ap = ap
        self.name = name
        self.w = None
        self.r = []
        self.sem = None
        self.is_dram = is_dram

    def __getitem__(self, idx):
        return self.ap[idx]


SEM_LIMIT = 30000


class P:
    def __init__(self, name="k"):
        self.nc = bass.Bass("TRN2", target_bir_lowering=False)
        self.es = ExitStack()
        self.scopes = []
        nc_ = self.nc
        self.engs = {"pe": nc_.tensor, "dve": nc_.vector, "act": nc_.scalar, "pool": nc_.gpsimd, "sp": nc_.sync}
        self.known = {e: {} for e in ENGS}
        self.nsem = 0
        self.esem = {}
        self.ecnt = {}
        self.retired = []
        for e in ENGS:
            self._new_esem(e)
        self.dpool = {'sw': [], 'hw': []}
        self.dlive = []
        self.nbuf = 0
        self.out_events = []

    def _alloc_sem(self, tag):
        self.nsem += 1
        return self.es.enter_context(self.nc.semaphore(f"{tag}{self.nsem}"))

    def _new_esem(self, e):
        if e in self.esem:
            self.retired.append((id(self.esem[e]), self.ecnt[e], self.esem[e], e == "pe"))
        self.esem[e] = self._alloc_sem(f"se_{e}_")
        self.ecnt[e] = 0

    def _stack(self):
        return self.scopes[-1][0] if self.scopes else self.es

    def dram(self, name, shape, dtype, kind="Internal"):
        t = self.nc.dram_tensor(name, list(shape), dtype, kind=kind)
        return Buf(t.ap(), name, is_dram=True)

    def sbuf(self, shape, dtype, name=None):
        self.nbuf += 1
        name = f"{name or 'sb'}_{self.nbuf}"
        t = self._stack().enter_context(self.nc.sbuf_tensor(name, list(shape), dtype))
        return Buf(t, name)

    def psum(self, shape, dtype=F32, name=None):
        self.nbuf += 1
        name = f"{name or 'ps'}_{self.nbuf}"
        t = self._stack().enter_context(self.nc.psum_tensor(name, list(shape), dtype))
        return Buf(t, name)

    def view(self, ap, name="v"):
        return Buf(ap, name)

    def open_scope(self):
        self.scopes.append((ExitStack(), []))

    def close_scope(self):
        self.barrier()
        st, bufs = self.scopes.pop()
        for (b, kind, cur) in bufs:
            self.dpool[kind].append(cur)
            if b.sem is not None and b.sem.get(kind) is cur:
                del b.sem[kind]
        st.close()

    def barrier(self):
        evs = []
        for e in ENGS:
            if self.ecnt[e] > 0:
                evs.append((id(self.esem[e]), self.ecnt[e], self.esem[e], False))
        for s in self.dlive:
            if s[1] > 0:
                evs.append((id(s[0]), s[1], s[0], False))
        for e in ENGS:
            waits = []
            for sid, val, semh, _ in evs:
                if self.known[e].get(sid, 0) >= val:
                    continue
                if sid == id(self.esem[e]) and e == "pe":
                    pass
                self.known[e][sid] = val
                waits.append((semh, val))
            if waits:
                for s, v in waits:
                    self.engs[e].wait_ge(s, v)

    def _deps(self, eng, reads, writes):
        deps = []
        for b in reads:
            if b.w is not None:
                deps.append(b.w)
        for b in writes:
            if b.r:
                deps.extend(b.r)
            elif b.w is not None:
                deps.append(b.w)
        need = {}
        for ev in deps:
            sid, val, semh, is_pe = ev
            if is_pe and eng == "pe":
                continue
            if self.known[eng].get(sid, 0) >= val:
                continue
            if sid not in need or need[sid][0] < val:
                need[sid] = (val, semh)
        waits = []
        for sid, (val, semh) in need.items():
            self.known[eng][sid] = val
            waits.append((semh, val))
        return waits

    def _commit(self, ev, reads, writes):
        for b in reads:
            b.r.append(ev)
            if len(b.r) > 48:
                b.r = b.r[-48:]
        for b in writes:
            b.w = ev
            b.r = []

    def op(self, eng, fn, reads=(), writes=()):
        if self.ecnt[eng] >= SEM_LIMIT:
            self._new_esem(eng)
        waits = self._deps(eng, reads, writes)
        self.ecnt[eng] += 1
        semh = self.esem[eng]
        ev = (id(semh), self.ecnt[eng], semh, eng == "pe")

        def emit(e, waits=waits, fn=fn, semh=semh):
            for s, v in waits:
                e.wait_ge(s, v)
            fn(e).then_inc(semh, 1)

        emit(self.engs[eng])
        self._commit(ev, reads, writes)
        return ev

    def dma(self, q, out_b, out_ap, in_b, in_ap, fn=None, extra_reads=(), **kw):
        owner = out_b if not out_b.is_dram else in_b
        kind = "sw" if q == "pool" else "hw"
        if owner.sem is None:
            owner.sem = {}
        cur = owner.sem.get(kind)
        if cur is None or cur[1] >= SEM_LIMIT:
            cand = [s for s in self.dpool[kind] if s[1] < SEM_LIMIT]
            if cand:
                cur = cand[0]
                self.dpool[kind].remove(cur)
            else:
                cur = [self._alloc_sem("sd_"), 0]
                self.dlive.append(cur)
            owner.sem[kind] = cur
            if self.scopes:
                self.scopes[-1][1].append((owner, kind, cur))
        waits = self._deps(q, [in_b] + list(extra_reads), [out_b])
        cur[1] += 16
        semh = cur[0]
        ev = (id(semh), cur[1], semh, False)

        def emit(e, waits=waits, semh=semh):
            for s, v in waits:
                e.wait_ge(s, v)
            if fn is None:
                e.dma_start(out=out_ap, in_=in_ap, **kw).then_inc(semh, 16)
            else:
                fn(e).then_inc(semh, 16)

        emit(self.engs[q])
        self._commit(ev, [in_b] + list(extra_reads), [out_b])
        return ev

    def finish(self):
        while self.scopes:
            self.close_scope()
        self.barrier()
        nc = self.nc
        self.es.close()
        return nc

    def load(self, dst, dst_ap, src, src_ap, q="sp", **kw):
        return self.dma(q, dst, dst_ap, src, src_ap, **kw)

    def store(self, dst, dst_ap, src, src_ap, q="pool", **kw):
        return self.dma(q, dst, dst_ap, src, src_ap, **kw)

    def mm(self, out_b, out_ap, lhsT_b, lhsT_ap, rhs_b, rhs_ap, start=True, stop=True, extra_reads=()):
        return self.op("pe", lambda e: e.matmul(out_ap, lhsT_ap, rhs_ap, start=start, stop=stop),
                       reads=[lhsT_b, rhs_b] + list(extra_reads), writes=[out_b])

    def transpose(self, out_b, out_ap, in_b, in_ap, ident_b, ident_ap):
        return self.op("pe", lambda e: e.transpose(out_ap, in_ap, ident_ap),
                       reads=[in_b, ident_b], writes=[out_b])

    def act(self, out_b, out_ap, in_b, in_ap, func, bias=None, scale=None, reads=(), accum=None, accum_b=None):
        kw = {}
        if bias is not None:
            kw["bias"] = bias
        if scale is not None:
            kw["scale"] = scale
        if accum is not None:
            kw["accum_out"] = accum
        w = [out_b] + ([accum_b] if accum_b is not None else [])
        return self.op("act", lambda e: e.activation(out_ap, in_ap, func, **kw),
                       reads=[in_b] + list(reads), writes=w)

    def tt(self, out_b, out_ap, a_b, a_ap, b_b, b_ap, op, eng="dve"):
        return self.op(eng, lambda e: e.tensor_tensor(out_ap, a_ap, b_ap, op),
                       reads=[a_b, b_b], writes=[out_b])

    def ts(self, out_b, out_ap, in_b, in_ap, s1, s2, op0, op1=None, reads=(), eng="dve", accum=None, accum_b=None):
        kw = {}
        if accum is not None:
            kw["accum_out"] = accum
        w = [out_b] + ([accum_b] if accum_b is not None else [])
        if op1 is None:
            return self.op(eng, lambda e: e.tensor_scalar(out_ap, in_ap, s1, None, op0, **kw),
                           reads=[in_b] + list(reads), writes=w)
        return self.op(eng, lambda e: e.tensor_scalar(out_ap, in_ap, s1, s2, op0, op1, **kw),
                       reads=[in_b] + list(reads), writes=w)

    def stt(self, out_b, out_ap, a_b, a_ap, scalar, b_b, b_ap, op0, op1, reads=(), accum=None, accum_b=None):
        kw = {}
        if accum is not None:
            kw["accum_out"] = accum
        w = [out_b] + ([accum_b] if accum_b is not None else [])
        return self.op("dve", lambda e: e.scalar_tensor_tensor(out_ap, a_ap, scalar, b_ap, op0, op1, **kw),
                       reads=[a_b, b_b] + list(reads), writes=w)

    def copy(self, out_b, out_ap, in_b, in_ap, eng="dve"):
        if eng == "act":
            return self.op("act", lambda e: e.activation(out_ap, in_ap, AF.Copy), reads=[in_b], writes=[out_b])
        return self.op(eng, lambda e: e.tensor_copy(out_ap, in_ap), reads=[in_b], writes=[out_b])

    def memset(self, out_b, out_ap, val, eng="dve"):
        return self.op(eng, lambda e: e.memset(out_ap, val), reads=[], writes=[out_b])


def run(prog, in_maps):
    nc = prog.finish()
    res = run_bass_kernel_spmd(nc, in_maps, core_ids=list(range(NCORES)))
    return res.results


D = 1024
KC = D // 128
N_MOD = 6
NORM_EPS = 1e-6
GRID_W = 64
LB_FLOOR = 1e-30
CH = 32
EV = dict(q=0, i=512, zf=1024, zb=1536, g=2048, aq=2560, ak=3072, av=3328, aqs=3584, aks=4096)
EV_NF = 4352
OD = dict(q=0, k=256, v=512, g=1024, z=1536, xs=2048, bm=2560, cm=2816, lrf=3072, lrb=3088, dtf=3104, dtb=3112)
OD_NF = 3200
PEER_HEADS, PEER_NKEYS, PEER_TOPK = 8, 128, 16


class Cfg:
    def __init__(self, nb, seq, ctx, depth):
        self.nb, self.seq, self.ctx, self.depth = nb, seq, ctx, depth
        self.tb = seq + ctx
        self.T = nb * self.tb
        self.R = nb + 1
        self.alpha = (2 * depth) ** 0.25
        assert seq % 128 == 0 and ctx % 128 == 0

    def segs(self):
        out = []
        for b in range(self.nb):
            o = b * self.tb
            out.append((o, o + self.ctx, self.nb))
            out.append((o + self.ctx, o + self.tb, b))
        return out

    def seg_of_tile(self, ti):
        t = ti * 128
        for (a, b, r) in self.segs():
            if a <= t < b:
                return r
        raise AssertionError


def blocks(n, step):
    return [(a, min(a + step, n)) for a in range(0, n, step)]


def stage_mod(p, cfg, io, G):
    R = cfg.R
    NM = N_MOD * D
    p.open_scope()
    cT = p.sbuf([128, KC * R], F32, "cT")
    p.load(cT, cT[:, :], io["cT"], io["cT"].ap[:, :])
    sc = p.sbuf([128, KC, R], F32, "silu_c")
    p.act(sc, sc.ap[:, :, :].rearrange("p k r -> p (k r)"), cT, cT[:, :], AF.Silu)
    mbT = p.sbuf([128, cfg.depth * 48], F32, "mbT")
    p.load(mbT, mbT[:, :], io["mod_bT"], io["mod_bT"].ap[:, :])
    CB = 768
    wst = [p.sbuf([128, KC, CB], F32, f"mw{i}") for i in range(2)]
    psF = [p.psum([128, 512], F32, f"psF{i}") for i in range(2)]
    psT = [p.psum([128, 512], F32, f"psT{i}") for i in range(2)]
    mbrow = p.sbuf([R, NM], F32, "mbrow")
    mtok = p.sbuf([R, NM], F32, "mtok")
    modT = G["modT"]
    it = 0
    for l in range(cfg.depth):
        p.load(mbrow, mbrow[:, :], io["mod_b"], io["mod_b"].ap[l:l + 1, :].partition_broadcast(R))
        for cb in range(NM // CB):
            w = wst[it % 2]
            src = io["mod_w"].ap[l, :, cb * CB:(cb + 1) * CB].rearrange("(kc p) n -> p kc n", p=128)
            p.load(w, w[:, :, :], io["mod_w"], src, q="sp" if it % 2 else "act")
            pf = psF[it % 2]
            for dcl in range(CB // 128):
                dc = cb * (CB // 128) + dcl
                for kc in range(KC):
                    p.mm(pf, pf[:, dcl * R:(dcl + 1) * R], w, w[:, kc, dcl * 128:(dcl + 1) * 128],
                         sc, sc[:, kc, :], start=(kc == 0), stop=(kc == KC - 1))
                p.ts(modT, modT[:, l, dc, :], pf, pf[:, dcl * R:(dcl + 1) * R],
                     mbT[:, l * 48 + dc:l * 48 + dc + 1], None, ALU.add, reads=[mbT])
            pt = psT[it % 2]
            for (n0, n1) in blocks(CB, 512):
                for kc in range(KC):
                    p.mm(pt, pt[0:R, 0:n1 - n0], sc, sc[:, kc, :], w, w[:, kc, n0:n1],
                         start=(kc == 0), stop=(kc == KC - 1))
                p.tt(mtok, mtok[:, cb * CB + n0:cb * CB + n1], pt, pt[0:R, 0:n1 - n0],
                     mbrow, mbrow[:, cb * CB + n0:cb * CB + n1], ALU.add)
            it += 1
        p.store(G["modtok"], G["modtok"].ap[l, :, :], mtok, mtok[:, :])
    p.close_scope()


def stage_inproj(p, cfg, G, l, W_ap, NF, YT):
    T = cfg.T
    p.open_scope()
    ident = G["ident"]
    modT = G["modT"]
    w_bf = p.sbuf([128, KC, NF], BF16, "w_bf")
    wst = [p.sbuf([128, NF], F32, f"wst{i}") for i in range(2)]
    for kc in range(KC):
        st = wst[kc % 2]
        p.load(st, st[:, :], G["ext"], W_ap[kc * 128:(kc + 1) * 128, :], q="sp" if kc % 2 else "act")
        p.copy(w_bf, w_bf[:, kc, :], st, st[:, :], eng="pool" if kc % 2 else "dve")
    scale1 = p.sbuf([128, KC, cfg.R], F32, "scale1")
    p.ts(scale1, scale1[:, :, :], modT, modT[:, l, 8:16, :], 1.0, None, ALU.add)
    hst = [p.sbuf([128, D], F32, f"hst{i}") for i in range(2)]
    uT = [p.sbuf([128, KC, 512], BF16, f"uT{i}") for i in range(2)]
    pT = [p.psum([128, 4, 128], F32, f"pT{i}") for i in range(4)]
    pY = [p.psum([128, 512], F32, f"pY{i}") for i in range(3)]
    ysb = [p.sbuf([128, 512], F32, f"ysb{i}") for i in range(4)]
    j = 0
    for bi, (t0, t1) in enumerate(blocks(T, 512)):
        u = uT[bi % 2]
        nt = (t1 - t0) // 128
        for tl in range(nt):
            ti = t0 // 128 + tl
            r = cfg.seg_of_tile(ti)
            hs = hst[ti % 2]
            p.load(hs, hs[:, :], G["H"], G["H"].ap[ti * 128:(ti + 1) * 128, :], q="sp")
            for half in range(2):
                pt = pT[(2 * ti + half) % 4]
                for kk in range(4):
                    kc = half * 4 + kk
                    p.transpose(pt, pt[:, kk, :], hs, hs[:, kc * 128:(kc + 1) * 128], ident, ident[:, :])
                for kk in range(4):
                    kc = half * 4 + kk
                    p.act(u, u[:, kc, tl * 128:(tl + 1) * 128], pt, pt[:, kk, :], AF.Identity,
                          bias=modT[:, l, kc, r:r + 1], scale=scale1[:, kc, r:r + 1], reads=[modT, scale1])
        w = t1 - t0
        for fc in range(NF // 128):
            py = pY[j % 3]
            for kc in range(KC):
                p.mm(py, py[:, 0:w], w_bf, w_bf[:, kc, fc * 128:(fc + 1) * 128], u, u[:, kc, 0:w],
                     start=(kc == 0), stop=(kc == KC - 1))
            ys = ysb[j % 4]
            p.copy(ys, ys[:, 0:w], py, py[:, 0:w], eng="dve" if j % 2 else "act")
            p.store(YT, YT.ap[fc * 128:(fc + 1) * 128, t0:t1], ys, ys[:, 0:w], q="pool" if j % 2 else "sp")
            j += 1
    p.close_scope()


def layernorm_tile(p, cfg, G, r_b, out_b, lng, lnb, scr):
    stats, mv, rstd, tmp = scr
    for c in range(2):
        p.op("dve", lambda e, c=c: e.bn_stats(stats[:, c, :], r_b[:, c * 512:(c + 1) * 512]),
             reads=[r_b], writes=[stats])
    p.op("dve", lambda e: e.bn_aggr(mv[:, :], stats.ap[:, :, :].rearrange("p a b -> p (a b)")),
         reads=[stats], writes=[mv])
    p.act(rstd, rstd[:, 0:1], mv, mv[:, 1:2], AF.Ln, bias=float(NORM_EPS))
    p.act(rstd, rstd[:, 1:2], rstd, rstd[:, 0:1], AF.Exp, scale=-0.5)
    p.ts(tmp, tmp[:, :], r_b, r_b[:, :], mv[:, 0:1], rstd[:, 1:2], ALU.subtract, ALU.mult, reads=[mv, rstd])
    p.tt(tmp, tmp[:, :], tmp, tmp[:, :], lng, lng[:, :], ALU.mult)
    p.tt(out_b, out_b[:, :], tmp, tmp[:, :], lnb, lnb[:, :], ALU.add, eng="pool")


def stage_resid_ln(p, cfg, G, io, l, which, W_ap, mix_dram):
    T = cfg.T
    p.open_scope()
    gate_off = (2 if which == 0 else 5) * D
    lng = p.sbuf([128, D], F32, "lng")
    lnb = p.sbuf([128, D], F32, "lnb")
    p.load(lng, lng[:, :], io["ln_g"], io["ln_g"].ap[l, which:which + 1, :].partition_broadcast(128))
    p.load(lnb, lnb[:, :], io["ln_b"], io["ln_b"].ap[l, which:which + 1, :].partition_broadcast(128))
    gb = []
    for r in range(cfg.R):
        g = p.sbuf([128, D], F32, f"gate{r}")
        p.load(g, g[:, :], G["modtok"], G["modtok"].ap[l, r:r + 1, gate_off:gate_off + D].partition_broadcast(128))
        gb.append(g)
    if which == 0:
        w_bf = p.sbuf([128, KC, D], BF16, "wo_bf")
        wst = [p.sbuf([128, D], F32, f"wost{i}") for i in range(2)]
        for kc in range(KC):
            st = wst[kc % 2]
            p.load(st, st[:, :], G["ext"], W_ap[kc * 128:(kc + 1) * 128, :], q="act")
            p.copy(w_bf, w_bf[:, kc, :], st, st[:, :], eng="pool")
        mx = [p.sbuf([128, KC, 128], BF16, f"mx{i}") for i in range(2)]
        po = [p.psum([128, 2, 512], F32, f"po{i}") for i in range(2)]
    else:
        br = [p.sbuf([128, D], F32, f"br{i}") for i in range(2)]
    hs = [p.sbuf([128, D], F32, f"h{i}") for i in range(2)]
    rb = [p.sbuf([128, D], F32, f"r{i}") for i in range(2)]
    ob = [p.sbuf([128, D], F32, f"o{i}") for i in range(2)]
    scrs = [(p.sbuf([128, 2, 6], F32, f"stats{i}"), p.sbuf([128, 2], F32, f"mv{i}"), p.sbuf([128, 2], F32, f"rstd{i}"),
             p.sbuf([128, D], F32, f"lntmp{i}")) for i in range(2)]
    for ti in range(T // 128):
        r = cfg.seg_of_tile(ti)
        h = hs[ti % 2]
        rr = rb[ti % 2]
        p.load(h, h[:, :], G["H"], G["H"].ap[ti * 128:(ti + 1) * 128, :], q="sp")
        if which == 0:
            m = mx[ti % 2]
            p.load(m, m[:, :, :], mix_dram,
                   mix_dram.ap[:, ti * 128:(ti + 1) * 128].rearrange("(kc p) t -> p kc t", p=128), q="act")
            ps = po[ti % 2]
            for nb in range(2):
                for kc in range(KC):
                    p.mm(ps, ps[:, nb, :], m, m[:, kc, :], w_bf, w_bf[:, kc, nb * 512:(nb + 1) * 512],
                         start=(kc == 0), stop=(kc == KC - 1))
            p.tt(rr, rr.ap[:, :], ps, ps.ap[:, :, :].rearrange("p a b -> p (a b)"), gb[r], gb[r][:, :], ALU.mult)
        else:
            b_ = br[ti % 2]
            p.load(b_, b_[:, :], mix_dram, mix_dram.ap[ti * 128:(ti + 1) * 128, :], q="act")
            p.tt(rr, rr[:, :], b_, b_[:, :], gb[r], gb[r][:, :], ALU.mult, eng="pool")
        p.stt(rr, rr[:, :], h, h[:, :], float(cfg.alpha), rr, rr[:, :], ALU.mult, ALU.add)
        o = ob[ti % 2]
        layernorm_tile(p, cfg, G, rr, o, lng, lnb, scrs[ti % 2])
        p.store(G["H"], G["H"].ap[ti * 128:(ti + 1) * 128, :], o, o[:, :], q="pool")
    p.close_scope()


def build_program(cfg, stop_after=None, debug=()):
    p = P("mk")
    io = {}
    G = {}

    def ext(name, shape, dtype=F32):
        io[name] = p.dram(name, shape, dtype, "ExternalInput")
        return io[name]

    T, R, L = cfg.T, cfg.R, cfg.depth
    ne, no = (L + 1) // 2, L // 2
    ext("h0", [T, D])
    ext("cT", [128, KC * R])
    ext("mod_w", [L, D, N_MOD * D])
    ext("mod_b", [L, N_MOD * D])
    ext("mod_bT", [128, L * 48])
    ext("ln_g", [L, 2, D])
    ext("ln_b", [L, 2, D])
    ext("ev_w_in", [ne, D, EV_NF])
    ext("ev_w_out", [ne, D, D])
    if no:
        ext("od_w_in", [no, D, OD_NF])
        ext("od_w_out", [no, D, D])
    ext("ident", [128, 128])
    ext("peer_wq", [L, D, 2048])
    ext("peer_subkT", [L, 128, 2048])
    for l_ in range(L):
        ext(f"peer_u{l_}", [PEER_NKEYS * PEER_NKEYS, D])
        ext(f"peer_v{l_}", [PEER_NKEYS * PEER_NKEYS, D])
    ext("cmask", [128, 128])
    ext("iota16", [128, 16])
    ext("rm3", [128, 1])
    ext("rmask", [128, cfg.tb])
    ext("cosT", [128, cfg.tb])
    ext("sinT", [128, cfg.tb])
    ext("hg_lbT", [128, 2 * ne * 4])
    ext("hg_norm_gT", [ne, 128, 1])
    ext("at_normT", [ne, 128, 4])
    if no:
        ext("ssd_conv_wT", [no, 128, 40])
        ext("ssd_conv_bT", [no, 128, 8])
        ext("ssd_dt_biasT", [no, 8, 2])
        ext("ssd_a_logT", [no, 8, 2])
        ext("ssd_dT", [no, 128, 4])
        ext("ssd_norm_gT", [no, 128, 4])
        ext("selT", [8, 8 * 128])
        ext("mbias", [128, 128])
        ext("rmask128", [8, cfg.tb])
        ext("gla_gate_w", [no, 2, 16, 256])
        ext("gla_gate_bT", [no, 64, 8])
        ext("gla_norm_gT", [no, 128, 1])
    G["ext"] = Buf(None, "ext", is_dram=True)
    out = p.dram("out", [T, D], F32, "ExternalOutput")
    G["H"] = out
    kind = "ExternalOutput" if debug else "Internal"
    G["modtok"] = p.dram("modtok", [L, R, N_MOD * D], F32, kind)
    G["YT"] = p.dram("YT", [EV_NF, T], F32, kind)
    G["MIXT"] = p.dram("MIXT", [D, T], BF16, kind)
    G["PEERO"] = p.dram("PEERO", [T, D], F32, kind)
    G["UVB"] = p.dram("UVB", [PEER_NKEYS * PEER_NKEYS, 2 * D], BF16, "Internal")
    G["XSF"] = p.dram("XSF", [512, T], F32, "Internal")
    G["YPRE"] = p.dram("YPRE", [512, T], F32, "Internal")
    G["ident"] = p.sbuf([128, 128], F32, "ident")
    p.load(G["ident"], G["ident"][:, :], io["ident"], io["ident"].ap[:, :])
    G["ident_bf"] = p.sbuf([128, 128], BF16, "ident_bf")
    p.copy(G["ident_bf"], G["ident_bf"][:, :], G["ident"], G["ident"][:, :])
    G["modT"] = p.sbuf([128, L, 48, R], F32, "modT")
    G["ones_mean"] = p.sbuf([128, 128], F32, "ones_mean")
    p.memset(G["ones_mean"], G["ones_mean"][:, :], 1.0 / 128.0)
    for nm in ("cmask", "rm3"):
        G[nm] = p.sbuf(list(io[nm].ap.shape), F32, nm)
        p.load(G[nm], G[nm][:, :], io[nm], io[nm].ap[:, :])

    p.open_scope()
    cp = [p.sbuf([128, D], F32, f"cp{i}") for i in range(3)]
    for ti in range(T // 128):
        c = cp[ti % 3]
        p.load(c, c[:, :], io["h0"], io["h0"].ap[ti * 128:(ti + 1) * 128, :], q="sp")
        p.store(G["H"], G["H"].ap[ti * 128:(ti + 1) * 128, :], c, c[:, :], q="act")
    p.close_scope()
    stage_mod(p, cfg, io, G)
    if stop_after == "mod":
        return p
    for l in range(L):
        j = l // 2
        if l % 2 == 0:
            stage_inproj(p, cfg, G, l, io["ev_w_in"].ap[j], EV_NF, G["YT"])
        else:
            stage_inproj(p, cfg, G, l, io["od_w_in"].ap[j], OD_NF, G["YT"])
        if stop_after == f"inproj{l}":
            return p
        if l % 2 == 0:
            stage_vscan(p, cfg, G, io, l, "hgrn")
            if stop_after == f"scan{l}":
                return p
            stage_attn(p, cfg, G, io, l)
            if stop_after == f"attn{l}":
                return p
            stage_resid_ln(p, cfg, G, io, l, 0, io["ev_w_out"].ap[j], G["MIXT"])
        else:
            stage_vscan(p, cfg, G, io, l, "gla")
            if stop_after == f"scan{l}":
                return p
            stage_ssd(p, cfg, G, io, l)
            if stop_after == f"ssd{l}":
                return p
            stage_resid_ln(p, cfg, G, io, l, 0, io["od_w_out"].ap[j], G["MIXT"])
        if stop_after == f"mix{l}":
            return p
        stage_peer(p, cfg, G, io, l)
        if stop_after == f"peer{l}":
            return p
        stage_resid_ln(p, cfg, G, io, l, 1, None, G["PEERO"])
        if stop_after == f"layer{l}":
            return p
    return p


def _pair_swap(n):
    idx = np.arange(n)
    return idx ^ 1


def prep_inputs(inp, cfg, ncores):
    L = cfg.depth
    f32 = np.float32
    x, c, ctx, c_ctx = (np.asarray(inp[k], f32) for k in ("x", "c", "ctx", "c_ctx"))
    ev_w_in = np.asarray(inp["ev_w_in"], f32)
    aq = ev_w_in[:, :, EV["aq"]:EV["aq"] + 512][:, :, _pair_swap(512)]
    ak = ev_w_in[:, :, EV["ak"]:EV["ak"] + 256][:, :, _pair_swap(256)]
    ev_ext = np.ascontiguousarray(np.concatenate([ev_w_in, aq, ak], axis=2))
    od_w_in = np.asarray(inp["od_w_in"], f32)
    no = od_w_in.shape[0]
    if no:
        q, k, v, g, lrf, lrb, z, xs, bm, cm, dtf, dtb = np.split(
            od_w_in, np.cumsum([256, 256, 512, 512, 16, 16, 512, 512, 256, 256, 8, 8])[:-1].tolist(), axis=2)
        pad = np.zeros((no, D, OD_NF - 3120), f32)
        od_ext = np.ascontiguousarray(np.concatenate([q, k, v, g, z, xs, bm, cm, lrf, lrb, dtf, dtb, pad], axis=2))
    mod_b = np.asarray(inp["mod_b"], f32)
    mod_bT = np.ascontiguousarray(mod_b.reshape(L, 48, 128).transpose(2, 0, 1).reshape(128, L * 48))
    shared = {
        "mod_w": np.asarray(inp["mod_w"], f32), "mod_b": mod_b, "mod_bT": mod_bT,
        "ln_g": np.asarray(inp["ln_g"], f32), "ln_b": np.asarray(inp["ln_b"], f32),
        "ev_w_in": ev_ext, "ev_w_out": np.asarray(inp["ev_w_out"], f32),
        "ident": np.eye(128, dtype=f32),
    }
    shared["peer_wq"] = np.asarray(inp["peer_wq"], f32)
    sk = np.asarray(inp["peer_subkeys"], f32)
    shared["peer_subkT"] = np.ascontiguousarray(sk.transpose(0, 4, 1, 2, 3).reshape(L, 128, 2048))
    for l_ in range(L):
        shared[f"peer_u{l_}"] = np.asarray(inp["peer_u"][l_], f32)
        shared[f"peer_v{l_}"] = np.asarray(inp["peer_v"][l_], f32)
    jj = np.arange(128)
    shared["cmask"] = ((jj[:, None] // CH == jj[None, :] // CH) & (jj[:, None] <= jj[None, :])).astype(f32)
    shared["iota16"] = np.tile(np.arange(16, dtype=f32), (128, 1))
    shared["rm3"] = (jj >= 96).astype(f32)[:, None].copy()
    rm = np.ones((128, cfg.tb), f32)
    rm[:, ::CH] = 0.0
    shared["rmask"] = rm
    rows_ = cfg.seq // GRID_W
    trow = np.repeat(np.arange(rows_), GRID_W).astype(np.float64)
    tcol = np.tile(np.arange(GRID_W), rows_).astype(np.float64)
    inv = 10000.0 ** (-np.arange(32, dtype=np.float64) / 32)
    ang = np.concatenate([trow[:, None] * inv, tcol[:, None] * inv], axis=-1)
    angd = np.repeat(ang, 2, axis=1).T
    cosT = np.ones((128, cfg.tb)); sinT = np.zeros((128, cfg.tb))
    cosT[:, cfg.ctx:] = np.cos(angd)
    sgn = np.where(np.arange(128) % 2 == 0, -1.0, 1.0)[:, None]
    sinT[:, cfg.ctx:] = np.sin(angd) * sgn
    shared["cosT"] = cosT.astype(f32); shared["sinT"] = sinT.astype(f32)
    ne = ev_w_in.shape[0]
    lbl = np.asarray(inp["hg_lb_logits"], f32)
    shared["hg_lbT"] = np.ascontiguousarray(lbl.reshape(2, ne, 4, 128).transpose(3, 0, 1, 2).reshape(128, 2 * ne * 4))
    shared["hg_norm_gT"] = np.ascontiguousarray(np.asarray(inp["hg_norm_g"], f32)[:, :, None])
    gqn = np.asarray(inp["at_q_norm_g"], f32); gkn = np.asarray(inp["at_k_norm_g"], f32)
    sw = _pair_swap(128)
    shared["at_normT"] = np.ascontiguousarray(np.stack([gqn, gqn[:, sw], gkn, gkn[:, sw]], axis=2))
    if no:
        cwt = np.asarray(inp["ssd_conv_w"], f32)
        shared["ssd_conv_wT"] = np.ascontiguousarray(cwt.reshape(no, 5, 8, 128).transpose(0, 3, 2, 1).reshape(no, 128, 40))
        shared["ssd_conv_bT"] = np.ascontiguousarray(np.asarray(inp["ssd_conv_b"], f32).reshape(no, 8, 128).transpose(0, 2, 1))
        shared["ssd_dt_biasT"] = np.ascontiguousarray(np.asarray(inp["ssd_dt_bias"], f32).transpose(0, 2, 1))
        shared["ssd_a_logT"] = np.ascontiguousarray(np.asarray(inp["ssd_a_log"], f32).transpose(0, 2, 1))
        dd = np.asarray(inp["ssd_d"], f32)
        shared["ssd_dT"] = np.ascontiguousarray(np.repeat(dd, 64, axis=1).reshape(no, 4, 128).transpose(0, 2, 1))
        shared["ssd_norm_gT"] = np.ascontiguousarray(np.asarray(inp["ssd_norm_g"], f32).reshape(no, 4, 128).transpose(0, 2, 1))
        selT = np.zeros((8, 8, 128), f32)
        for h_ in range(8):
            selT[h_, h_, :] = 1.0
        shared["selT"] = selT.reshape(8, 8 * 128)
        shared["mbias"] = np.where(jj[:, None] <= jj[None, :], 0.0, -30000.0).astype(f32)
        rm128 = np.ones((8, cfg.tb), f32)
        rm128[:, ::128] = 0.0
        shared["rmask128"] = rm128
        shared["gla_gate_w"] = np.asarray(inp["gla_gate_w"], f32)
        gb_ = np.asarray(inp["gla_gate_b"], f32)
        shared["gla_gate_bT"] = np.ascontiguousarray(gb_.reshape(no, 2, 4, 64).transpose(0, 3, 1, 2).reshape(no, 64, 8))
        shared["gla_norm_gT"] = np.ascontiguousarray(np.asarray(inp["gla_norm_g"], f32)[:, :, None])
    if no:
        shared["od_w_in"] = od_ext
        shared["od_w_out"] = np.asarray(inp["od_w_out"], f32)
    maps = []
    for ci in range(ncores):
        bs = range(ci * cfg.nb, (ci + 1) * cfg.nb)
        h0 = np.concatenate([np.concatenate([ctx[b], x[b]], axis=0) for b in bs], axis=0)
        cv = np.stack([c[b] for b in bs] + [c_ctx], axis=0)
        cT = np.ascontiguousarray(cv.T.reshape(KC, 128, cfg.R).transpose(1, 0, 2).reshape(128, KC * cfg.R))
        m = dict(shared)
        m["h0"] = np.ascontiguousarray(h0)
        m["cT"] = cT
        maps.append(m)
    return maps


def nat_slice(cfg, s0, s1, d):
    if d == 0:
        return slice(s0, s1), False
    if s1 <= cfg.ctx:
        return slice(cfg.ctx - s1, cfg.ctx - s0), True
    assert s0 >= cfg.ctx
    return slice(cfg.tb - (s1 - cfg.ctx), cfg.tb - (s0 - cfg.ctx)), True


def nat_ap(ap, cfg, s0, s1, d, rows=None):
    sl, rev = nat_slice(cfg, s0, s1, d)
    a = ap[:, sl] if rows is None else ap[rows, sl]
    return a[:, ::-1] if rev else a


def stage_vscan(p, cfg, G, io, l, kind):
    Tb, CTX = cfg.tb, cfg.ctx
    j = l // 2
    hgrn = kind == "hgrn"
    dk = 128 if hgrn else 64
    dv = 128
    NH = 4
    YT = G["YT"]
    p.open_scope()
    segs = [(0, CTX), (CTX, Tb)]
    identb = G["ident_bf"]
    cmask = G["cmask"]
    onesm = G["ones_mean"]
    pTrK = p.psum([128, 1024], BF16, "pTrK")
    pTrV = p.psum([128, 1024], BF16, "pTrV")
    pAtt = [p.psum([128, 512], F32, f"pAtt{i}") for i in range(1)]
    pO = [p.psum([128, 512], F32, f"pO{i}") for i in range(2)]
    pKV = [p.psum([128, 512], F32, f"pKV{i}") for i in range(2)]
    pX = p.psum([128, 512], F32, "pX")
    if hgrn:
        ne = (cfg.depth + 1) // 2
        lg = p.sbuf([128, 2, ne, NH], F32, "lb_logits")
        p.load(lg, lg.ap[:, :, :, :].rearrange("p a b c -> p (a b c)"), io["hg_lbT"], io["hg_lbT"].ap[:, :])
        p.act(lg, lg[:, :, :, :], lg, lg[:, :, :, :], AF.Exp)
        ssum = p.sbuf([128, 2, NH], F32, "lb_sum")
        p.copy(ssum, ssum[:, :, :], lg, lg[:, :, 0, :])
        for i in range(1, ne):
            p.tt(ssum, ssum[:, :, :], ssum, ssum[:, :, :], lg, lg[:, :, i, :], ALU.add)
        ssf = ssum.ap[:, :, :].rearrange("p a b -> p (a b)")
        p.op("dve", lambda e: e.reciprocal(ssf, ssf), reads=[ssum], writes=[ssum])
        lb = p.sbuf([128, 2, NH], F32, "lb")
        p.memset(lb, lb[:, :, :], 0.0)
        for i in range(1, j + 1):
            p.tt(lb, lb[:, :, :], lb, lb[:, :, :], lg, lg[:, :, i, :], ALU.add)
        p.tt(lb, lb[:, :, :], lb, lb[:, :, :], ssum, ssum[:, :, :], ALU.mult)
        oml = p.sbuf([128, 2, NH], F32, "oml")
        p.ts(oml, oml[:, :, :], lb, lb[:, :, :], -1.0, 1.0, ALU.mult, ALU.add)
        noml = p.sbuf([128, 2, NH], F32, "noml")
        p.ts(noml, noml[:, :, :], oml, oml[:, :, :], -1.0, None, ALU.mult)
        lbf = p.sbuf([128, 2, NH], F32, "lbf")
        p.ts(lbf, lbf[:, :, :], lb, lb[:, :, :], float(LB_FLOOR), None, ALU.max)
        gn = p.sbuf([128, 1], F32, "gn")
        p.load(gn, gn[:, :], io["hg_norm_gT"], io["hg_norm_gT"].ap[j])
        rows = dict(q=EV["q"], k=None, v=EV["i"], g=EV["g"], z=(EV["zf"], EV["zb"]))
        qscale = 128 ** -0.5
    else:
        gw = p.sbuf([16, 2, 256], F32, "gw")
        p.load(gw, gw[:, :, :], io["gla_gate_w"], io["gla_gate_w"].ap[j].rearrange("d r c -> r d c"))
        gw_bf = p.sbuf([16, 2, 256], BF16, "gw_bf")
        p.copy(gw_bf, gw_bf[:, :, :], gw, gw[:, :, :])
        gbT = p.sbuf([64, 2 * NH], F32, "gbT")
        p.load(gbT, gbT[:, :], io["gla_gate_bT"], io["gla_gate_bT"].ap[j])
        gn = p.sbuf([128, 1], F32, "gn")
        p.load(gn, gn[:, :], io["gla_norm_gT"], io["gla_norm_gT"].ap[j])
        rows = dict(q=OD["q"], k=OD["k"], v=OD["v"], g=OD["g"], z=(OD["lrf"], OD["lrb"]))
        qscale = 64 ** -0.5
    q_raw = p.sbuf([dk, Tb], F32, "q_raw")
    k_raw = None if hgrn else p.sbuf([dk, Tb], F32, "k_raw")
    v_raw = p.sbuf([dv, Tb], F32, "v_raw")
    g_raw = p.sbuf([dv, Tb], F32, "g_raw")
    z_raw = [p.sbuf([dk if hgrn else 16, Tb], F32, f"z_raw{i}") for i in range(2)]
    A1 = p.sbuf([dk, Tb], F32, "A1")
    A2 = p.sbuf([dk, Tb], F32, "A2")
    A3 = p.sbuf([dk, Tb], F32, "A3")
    A4 = p.sbuf([dk, Tb], F32, "A4")
    oacc = p.sbuf([dv, Tb], F32, "oacc")
    Qh = p.sbuf([dk, Tb], BF16, "Qh")
    Kh = p.sbuf([dk, Tb], BF16, "Kh")
    vb = p.sbuf([dv, Tb], BF16, "vb")
    lr_bf = None if hgrn else p.sbuf([16, Tb], BF16, "lr_bf")
    outb = p.sbuf([dv, Tb], BF16, "outb")
    Ktok = [p.sbuf([128, dk], BF16, f"Ktok{i}") for i in range(2)]
    Vtok = [p.sbuf([128, dv], BF16, f"Vtok{i}") for i in range(2)]
    Ktok3 = [p.sbuf([128, dk], BF16, f"Ktok3{i}") for i in range(2)]
    att_sb = [p.sbuf([128, 128], BF16, f"att{i}") for i in range(2)]
    S = p.sbuf([dk, dv], F32, "S")
    Stmp = p.sbuf([dk, dv], F32, "Stmp")
    Sbf = [p.sbuf([dk, dv], BF16, f"Sbf{i}") for i in range(2)]
    rmask = p.sbuf([128, Tb], F32, "rmask")
    p.load(rmask, rmask[:, :], io["rmask"], io["rmask"].ap[:, :])
    tabgen = peer_tables_gen(p, cfg, G, io, l)
    n_tile_iters = cfg.nb * NH * 2 * (Tb // 128)
    tab_per_iter = -(-(PEER_NKEYS * PEER_NKEYS // 128) // n_tile_iters)
    import os
    VCUT = int(os.environ.get("VCUT", "0"))
    if VCUT == 1:
        p.close_scope(); return
    nt = Tb // 128
    NCK = 128 // CH
    for b in range(cfg.nb):
        c0 = b * Tb
        for h in range(NH):
            p.load(q_raw, q_raw[:, :], YT, YT.ap[rows["q"] + h * dk:rows["q"] + (h + 1) * dk, c0:c0 + Tb], q="sp")
            if not hgrn:
                p.load(k_raw, k_raw[:, :], YT, YT.ap[rows["k"] + h * dk:rows["k"] + (h + 1) * dk, c0:c0 + Tb], q="act")
            p.load(v_raw, v_raw[:, :], YT, YT.ap[rows["v"] + h * dv:rows["v"] + (h + 1) * dv, c0:c0 + Tb], q="act")
            p.load(g_raw, g_raw[:, :], YT, YT.ap[rows["g"] + h * dv:rows["g"] + (h + 1) * dv, c0:c0 + Tb], q="sp")
            for d in range(2):
                zr = z_raw[d]
                if hgrn:
                    p.load(zr, zr[:, :], YT, YT.ap[rows["z"][d] + h * dk:rows["z"][d] + (h + 1) * dk, c0:c0 + Tb], q="sp")
                else:
                    p.load(zr, zr[:, :], YT, YT.ap[rows["z"][d]:rows["z"][d] + 16, c0:c0 + Tb], q="sp")
                if VCUT == 2:
                    p.close_scope(); return
                if hgrn:
                    for (s0, s1) in segs:
                        p.act(A1, A1[:, s0:s1], zr, nat_ap(zr.ap, cfg, s0, s1, d), AF.Sigmoid)
                    p.ts(A2, A2[:, :], A1, A1[:, :], oml[:, d, h:h + 1], lbf[:, d, h:h + 1], ALU.mult, ALU.add,
                         reads=[oml, lbf])
                    p.ts(A4, A4[:, :], A1, A1[:, :], noml[:, d, h:h + 1], oml[:, d, h:h + 1], ALU.mult, ALU.add,
                         reads=[oml, noml], eng="pool")
                    p.act(A2, A2[:, :], A2, A2[:, :], AF.Ln)
                    esc = 1.0
                else:
                    for (s0, s1) in segs:
                        p.copy(lr_bf, lr_bf[:, s0:s1], zr, nat_ap(zr.ap, cfg, s0, s1, d), eng="pool")
                    for (t0, t1) in blocks(Tb, 512):
                        p.mm(pX, pX[0:dk, 0:t1 - t0], gw_bf, gw_bf[:, d, h * dk:(h + 1) * dk], lr_bf, lr_bf[:, t0:t1])
                        p.act(A2, A2[:, t0:t1], pX, pX[0:dk, 0:t1 - t0], AF.Sigmoid,
                              bias=gbT[:, d * NH + h:d * NH + h + 1], reads=[gbT])
                    p.act(A2, A2[:, :], A2, A2[:, :], AF.Ln)
                    esc = 1.0 / 16.0
                p.op("dve", lambda e: e.tensor_tensor_scan(A3[:, :], rmask[0:dk, :], A2[:, :], 0.0, ALU.mult, ALU.add),
                     reads=[rmask, A2], writes=[A3])
                p.act(A1, A1[:, :], A3, A3[:, :], AF.Exp, scale=-esc)
                p.act(A3, A3[:, :], A3, A3[:, :], AF.Exp, scale=esc)
                if hgrn:
                    for (s0, s1) in segs:
                        p.act(A2, A2[:, s0:s1], q_raw, nat_ap(q_raw.ap, cfg, s0, s1, d), AF.Silu)
                    p.stt(Qh, Qh[:, :], A2, A2[:, :], float(qscale), A3, A3[:, :], ALU.mult, ALU.mult)
                    p.tt(Kh, Kh[:, :], A4, A4[:, :], A1, A1[:, :], ALU.mult, eng="pool")
                else:
                    for (s0, s1) in segs:
                        p.stt(Qh, Qh[:, s0:s1], q_raw, nat_ap(q_raw.ap, cfg, s0, s1, d), float(qscale),
                              A3, A3[:, s0:s1], ALU.mult, ALU.mult)
                        p.tt(Kh, Kh[:, s0:s1], k_raw, nat_ap(k_raw.ap, cfg, s0, s1, d), A1, A1[:, s0:s1], ALU.mult)
                for (s0, s1) in segs:
                    p.copy(vb, vb[:, s0:s1], v_raw, nat_ap(v_raw.ap, cfg, s0, s1, d), eng="pool")
                if VCUT == 3:
                    p.close_scope(); return
                p.memset(S, S[:, :], 0.0)
                p.memset(Sbf[0], Sbf[0][:, :], 0.0)
                cur = 0
                dprev = A3[:, 0:1]
                for ti in range(nt):
                    cs = slice(ti * 128, (ti + 1) * 128)
                    kt, vt = Ktok[ti % 2], Vtok[ti % 2]
                    kt3 = Ktok3[ti % 2]
                    p.transpose(pTrK, pTrK[:, 0:dk], Kh, Kh[:, cs], identb, identb[0:dk, 0:dk])
                    p.transpose(pTrV, pTrV[:, 0:dv], vb, vb[:, cs], identb, identb[:, :])
                    p.copy(kt, kt[:, :], pTrK, pTrK[:, 0:dk], eng="act")
                    p.copy(vt, vt[:, :], pTrV, pTrV[:, 0:dv], eng="dve")
                    p.ts(kt3, kt3[:, :], kt, kt[:, :], G["rm3"][:, 0:1], None, ALU.mult, reads=[G["rm3"]], eng="pool")
                    if VCUT == 4:
                        p.close_scope(); return
                    pa = pAtt[0]
                    p.mm(pa, pa[:, 0:128], Kh, Kh[:, cs], Qh, Qh[:, cs])
                    at = att_sb[ti % 2]
                    p.tt(at, at[:, :], pa, pa[:, 0:128], cmask, cmask[:, :], ALU.mult)
                    if VCUT == 5:
                        p.close_scope(); return
                    po = pO[ti % 2]
                    p.mm(po, po[0:dv, 0:128], vt, vt[:, :], at, at[:, :], start=True, stop=False)
                    for c in range(NCK):
                        cc = slice(ti * 128 + c * CH, ti * 128 + (c + 1) * CH)
                        p.mm(po, po[0:dv, c * CH:(c + 1) * CH], Sbf[cur], Sbf[cur][:, :], Qh, Qh[:, cc],
                             start=False, stop=(c == NCK - 1))
                        pk = pKV[c % 2]
                        if c * CH < 96:
                            p.mm(pk, pk[0:dk, 0:dv], kt, kt[c * CH:(c + 1) * CH, :], vt, vt[c * CH:(c + 1) * CH, :])
                        else:
                            p.mm(pk, pk[0:dk, 0:dv], kt3, kt3[64:128, :], vt, vt[64:128, :])
                        dcol = A3[:, ti * 128 + (c + 1) * CH - 1:ti * 128 + (c + 1) * CH]
                        p.stt(S, S[:, :], S, S[:, :], dprev, pk, pk[0:dk, 0:dv], ALU.mult, ALU.add, reads=[A3])
                        nxt = 1 - cur
                        p.act(Sbf[nxt], Sbf[nxt][:, :], S, S[:, :], AF.Identity, scale=dcol, reads=[A3])
                        cur = nxt
                        dprev = dcol
                    if VCUT == 6:
                        p.close_scope(); return
                    tabgen = drain_gen(tabgen, tab_per_iter)
                    sl, rev = nat_slice(cfg, ti * 128, (ti + 1) * 128, d)
                    if d == 0:
                        p.copy(oacc, oacc[:, sl], po, po[0:dv, 0:128], eng="act")
                    else:
                        oa = oacc.ap[:, sl][:, ::-1]
                        p.tt(oacc, oa, oacc, oa, po, po[0:dv, 0:128], ALU.add)
            if VCUT == 7:
                p.close_scope(); return
            if hgrn:
                p.act(g_raw, g_raw[:, :], g_raw, g_raw[:, :], AF.Sigmoid)
                p.tt(oacc, oacc[:, :], oacc, oacc[:, :], g_raw, g_raw[:, :], ALU.mult)
            else:
                p.act(g_raw, g_raw[:, :], g_raw, g_raw[:, :], AF.Silu)
            p.act(v_raw, v_raw[:, :], oacc, oacc[:, :], AF.Square)
            for (t0, t1) in blocks(Tb, 512):
                p.mm(pX, pX[:, 0:t1 - t0], onesm, onesm[:, :], v_raw, v_raw[:, t0:t1])
                p.act(v_raw, v_raw[:, t0:t1], pX, pX[:, 0:t1 - t0], AF.Ln, bias=float(NORM_EPS))
            p.act(v_raw, v_raw[:, :], v_raw, v_raw[:, :], AF.Exp, scale=-0.5)
            if hgrn:
                p.stt(outb, outb[:, :], oacc, oacc[:, :], gn[:, 0:1], v_raw, v_raw[:, :], ALU.mult, ALU.mult, reads=[gn])
            else:
                p.stt(oacc, oacc[:, :], oacc, oacc[:, :], gn[:, 0:1], v_raw, v_raw[:, :], ALU.mult, ALU.mult, reads=[gn])
                p.tt(outb, outb[:, :], oacc, oacc[:, :], g_raw, g_raw[:, :], ALU.mult)
            p.store(G["MIXT"], G["MIXT"].ap[h * dv:(h + 1) * dv, c0:c0 + Tb], outb, outb[:, :], q="pool")
    drain_gen(tabgen)
    p.close_scope()


def stage_attn(p, cfg, G, io, l):
    Tb, CTX, SEQ = cfg.tb, cfg.ctx, cfg.seq
    j = l // 2
    YT = G["YT"]
    p.open_scope()
    identb = G["ident_bf"]
    onesm = G["ones_mean"]
    cosT = p.sbuf([128, Tb], F32, "cosT")
    sinT = p.sbuf([128, Tb], F32, "sinT")
    p.load(cosT, cosT[:, :], io["cosT"], io["cosT"].ap[:, :])
    p.load(sinT, sinT[:, :], io["sinT"], io["sinT"].ap[:, :])
    onesb = p.sbuf([128, 128], BF16, "onesb")
    p.memset(onesb, onesb[:, :], 1.0)
    gq = p.sbuf([128, 4], F32, "gq")
    p.load(gq, gq[:, :], io["at_normT"], io["at_normT"].ap[j])
    gab = p.sbuf([128, 2], F32, "gab")
    p.act(gab, gab[:, 0:1], gq, gq[:, 0:1], AF.Abs)
    p.act(gab, gab[:, 1:2], gq, gq[:, 2:3], AF.Abs)
    pS = [p.psum([128, 512], F32, f"pS{i}") for i in range(3)]
    pOa = p.psum([128, 512], F32, "pOa")
    pSum = p.psum([128, 512], F32, "pSum")
    pM = p.psum([128, 512], F32, "pM")
    trs = [p.psum([128, 4, 256], BF16, f"pTrA{s}") for s in range(2)]
    ident = G["ident"]
    p.transpose(pM, pM[0:2, 0:128], gab, gab[:, 0:2], ident, ident[:, :])
    gmx = p.sbuf([2, 2], F32, "gmx")
    p.op("dve", lambda e: e.reduce_max(gmx[0:2, 0:1], pM[0:2, 0:128], axis=AX.X), reads=[pM], writes=[gmx])
    gmx2 = p.sbuf([2, 2], F32, "gmx2")
    p.memset(gmx2, gmx2[:, :], 0.0)
    p.tt(gmx2, gmx2[0:2, 0:2], ident, ident[0:2, 0:2], gmx, gmx.ap[0:2, 0:1].broadcast_to([2, 2]), ALU.mult)
    ones2 = p.sbuf([2, 128], F32, "ones2")
    p.memset(ones2, ones2[:, :], 1.0)
    p.mm(pM, pM[:, 0:2], ones2, ones2[:, :], gmx2, gmx2[:, :])
    nshift = p.sbuf([128, 1], F32, "nshift")
    p.copy(gab, gab[:, 0:2], pM, pM[:, 0:2])
    p.stt(nshift, nshift[:, 0:1], gab, gab[:, 0:1], float(-(128 ** 0.5)), gab, gab[:, 1:2], ALU.mult, ALU.mult)
    x_raw = p.sbuf([128, Tb], F32, "x_raw")
    xs_raw = p.sbuf([128, Tb], F32, "xs_raw")
    t1 = p.sbuf([128, Tb], F32, "t1")
    t2 = p.sbuf([128, Tb], F32, "t2")
    qr = p.sbuf([128, 4, Tb], BF16, "qr")
    kr = p.sbuf([128, 2, Tb], BF16, "kr")
    vbf = p.sbuf([128, Tb], BF16, "vbf")
    nkt = Tb // 128
    Vtok = p.sbuf([128, 2, nkt, 128], BF16, "Vtok")
    Pt = [p.sbuf([128, 512], BF16, f"Pt{i}") for i in range(3)]
    osb = p.sbuf([128, 512], F32, "osb")
    rs = p.sbuf([128, 512], F32, "rs")
    obf = [p.sbuf([128, 512], BF16, f"obf{i}") for i in range(2)]
    scale = 128 ** -0.5
    for b in range(cfg.nb):
        c0 = b * Tb
        heads = [("q", h, EV["aq"] + h * 128, EV["aqs"] + h * 128, 0) for h in range(4)] + \
                [("k", g, EV["ak"] + g * 128, EV["aks"] + g * 128, 2) for g in range(2)]
        for (kind, hi, r0, rs0, gc) in heads:
            p.load(x_raw, x_raw[:, :], YT, YT.ap[r0:r0 + 128, c0:c0 + Tb], q="sp")
            p.load(xs_raw, xs_raw[:, :], YT, YT.ap[rs0:rs0 + 128, c0:c0 + Tb], q="act")
            p.act(t1, t1[:, :], x_raw, x_raw[:, :], AF.Square)
            for (a0, a1) in blocks(Tb, 512):
                p.mm(pM, pM[:, 0:a1 - a0], onesm, onesm[:, :], t1, t1[:, a0:a1])
                p.act(t2, t2[:, a0:a1], pM, pM[:, 0:a1 - a0], AF.Ln, bias=float(NORM_EPS))
            p.act(t2, t2[:, :], t2, t2[:, :], AF.Exp, scale=-0.5)
            p.stt(t1, t1[:, :], x_raw, x_raw[:, :], gq[:, gc:gc + 1], cosT, cosT[:, :], ALU.mult, ALU.mult, reads=[gq])
            p.stt(x_raw, x_raw[:, :], xs_raw, xs_raw[:, :], gq[:, gc + 1:gc + 2], sinT, sinT[:, :], ALU.mult, ALU.mult,
                  reads=[gq])
            p.tt(t1, t1[:, :], t1, t1[:, :], x_raw, x_raw[:, :], ALU.add, eng="pool")
            dst = qr if kind == "q" else kr
            p.tt(dst, dst[:, hi, :], t1, t1[:, :], t2, t2[:, :], ALU.mult)
        for g in range(2):
            p.load(x_raw, x_raw[:, :], YT, YT.ap[EV["av"] + g * 128:EV["av"] + (g + 1) * 128, c0:c0 + Tb], q="sp")
            p.copy(vbf, vbf[:, :], x_raw, x_raw[:, :], eng="pool")
            for kt in range(nkt):
                tr = trs[kt % 2]
                p.transpose(tr, tr[:, 0, 0:128], vbf, vbf[:, kt * 128:(kt + 1) * 128], identb, identb[:, :])
                p.copy(Vtok, Vtok[:, g, kt, :], tr, tr[:, 0, 0:128], eng="act" if kt % 2 else "dve")
        it = 0
        for h in range(4):
            g = h // 2
            qblocks = [(a0, a1, 0, CTX // 128) for (a0, a1) in blocks(CTX, 512)] + \
                      [(CTX + a0, CTX + a1, 0, nkt) for (a0, a1) in blocks(SEQ, 512)]
            for (q0, q1, k0, k1) in qblocks:
                w = q1 - q0
                for kt in range(k0, k1):
                    ps = pS[it % 3]
                    pt = Pt[it % 3]
                    it += 1
                    p.mm(ps, ps[:, 0:w], kr, kr[:, g, kt * 128:(kt + 1) * 128], qr, qr[:, h, q0:q1])
                    p.act(pt, pt[:, 0:w], ps, ps[:, 0:w], AF.Exp, bias=nshift[:, 0:1], scale=float(scale), reads=[nshift])
                    p.mm(pOa, pOa[:, 0:w], Vtok, Vtok[:, g, kt, :], pt, pt[:, 0:w], start=(kt == k0), stop=(kt == k1 - 1))
                    p.mm(pSum, pSum[:, 0:w], onesb, onesb[:, :], pt, pt[:, 0:w], start=(kt == k0), stop=(kt == k1 - 1))
                p.op("dve", lambda e, w=w: e.reciprocal(rs[:, 0:w], pSum[:, 0:w]), reads=[pSum], writes=[rs])
                ob = obf[(it // 1) % 2]
                p.tt(ob, ob[:, 0:w], pOa, pOa[:, 0:w], rs, rs[:, 0:w], ALU.mult)
                p.store(G["MIXT"], G["MIXT"].ap[512 + h * 128:512 + (h + 1) * 128, c0 + q0:c0 + q1], ob, ob[:, 0:w], q="pool")
    p.close_scope()


NEG = -1.0e30

def peer_tables_gen(p, cfg, G, io, l):
    NE = PEER_NKEYS * PEER_NKEYS
    su = [p.sbuf([128, D], F32, f"tsu{i}") for i in range(2)]
    sv = [p.sbuf([128, D], F32, f"tsv{i}") for i in range(2)]
    ob = [p.sbuf([128, 2 * D], BF16, f"tob{i}") for i in range(2)]
    u_tab = io[f"peer_u{l}"].ap
    v_tab = io[f"peer_v{l}"].ap
    for rb in range(NE // 128):
        a, b_, o = su[rb % 2], sv[rb % 2], ob[rb % 2]
        p.load(a, a[:, :], G["ext"], u_tab[rb * 128:(rb + 1) * 128, :], q="sp")
        p.load(b_, b_[:, :], G["ext"], v_tab[rb * 128:(rb + 1) * 128, :], q="act")
        p.copy(o, o[:, 0:D], a, a[:, :], eng="act")
        p.copy(o, o[:, D:2 * D], b_, b_[:, :], eng="dve" if rb % 2 else "pool")
        p.store(G["UVB"], G["UVB"].ap[rb * 128:(rb + 1) * 128, :], o, o[:, :], q="sp")
        yield


def drain_gen(g, n=None):
    if g is None:
        return None
    try:
        if n is None:
            while True:
                next(g)
        for _ in range(n):
            next(g)
    except StopIteration:
        return None
    return g


def stage_peer(p, cfg, G, io, l):
    import os
    T = cfg.T
    H8, NK, TK = PEER_HEADS, PEER_NKEYS, PEER_TOPK
    NQ = H8 * 2 * NK
    p.open_scope()
    ident = G["ident"]
    wq_bf = p.sbuf([128, KC, NQ], BF16, "wq_bf")
    p.open_scope()
    wst = [p.sbuf([128, NQ], F32, f"wqst{i}") for i in range(2)]
    for kc in range(KC):
        st = wst[kc % 2]
        p.load(st, st[:, :], G["ext"], io["peer_wq"].ap[l, kc * 128:(kc + 1) * 128, :], q="sp" if kc % 2 else "act")
        p.copy(wq_bf, wq_bf[:, kc, :], st, st[:, :], eng="pool" if kc % 2 else "dve")
    sk = p.sbuf([128, 16 * NK], F32, "sk")
    p.load(sk, sk[:, :], G["ext"], io["peer_subkT"].ap[l])
    p.close_scope()
    sk_bf = p.sbuf([128, 16, NK], BF16, "sk_bf")
    p.open_scope()
    sk2 = p.sbuf([128, 16 * NK], F32, "sk2")
    p.load(sk2, sk2[:, :], G["ext"], io["peer_subkT"].ap[l])
    p.copy(sk_bf, sk_bf.ap[:, :, :].rearrange("p a b -> p (a b)"), sk2, sk2[:, :])
    p.close_scope()
    scb, shb = [], []
    for r in range(cfg.R):
        a = p.sbuf([128, D], F32, f"sc2b{r}")
        b_ = p.sbuf([128, D], F32, f"sh2b{r}")
        p.load(a, a[:, :], G["modtok"], G["modtok"].ap[l, r:r + 1, 4 * D:5 * D].partition_broadcast(128))
        p.load(b_, b_[:, :], G["modtok"], G["modtok"].ap[l, r:r + 1, 3 * D:4 * D].partition_broadcast(128))
        p.ts(a, a[:, :], a, a[:, :], 1.0, None, ALU.add)
        scb.append(a)
        shb.append(b_)
    TB = 2
    hst = [p.sbuf([128, D], F32, f"phst{i}") for i in range(2)]
    hm = p.sbuf([128, 4, D], F32, "hm_tok")
    hmT = p.sbuf([128, KC, TB * 128], BF16, "hmT")
    qT = p.sbuf([128, 16, TB * 128], BF16, "qT")
    pT = [p.psum([128, 4, 128], F32, "ppT")] * 2
    pQ = [p.psum([128, 512], F32, "ppQ")] * 2
    pS = [p.psum([128, 4, 128], F32, f"ppS{i}") for i in range(2)] * 2
    pHM = p.psum([128, 1024], F32, "pHM")
    pACC = [p.psum([128, 512], F32, f"pACC{i}") for i in range(2)]
    s_sb = p.sbuf([128, 16, NK], F32, "s_sb")
    s_wk = p.sbuf([128, NK], F32, "s_wk")
    ts16 = p.sbuf([128, 16, TK], F32, "ts16")
    ti16 = p.sbuf([128, 16, TK], U32, "ti16")
    ti16f = p.sbuf([128, 16, TK], F32, "ti16f")
    cand = p.sbuf([128, H8, TK * TK], F32, "cand")
    cwk = p.sbuf([128, TK * TK], F32, "cwk")
    eid = p.sbuf([128, H8, TK * TK], F32, "eid")
    best = p.sbuf([128, H8, TK], F32, "best")
    gate = p.sbuf([128, H8, TK], F32, "gate")
    gsum = p.sbuf([128, H8], F32, "gsum")
    idxf = p.sbuf([128, H8 * TK], F32, "idxf")
    idxu = p.sbuf([128, H8 * TK], U32, "idxu")
    aval = p.sbuf([128, H8 * TK], F32, "aval")
    coef = p.sbuf([128, H8 * TK], F32, "coef")
    tmpa = p.sbuf([128, H8 * TK], F32, "tmpa")
    junk = p.sbuf([128, D], BF16, "junk")
    NUB = int(os.environ.get("NUB", "12"))
    GRP = 4
    ub = [p.sbuf([128, 2 * D], BF16, f"ub{i}") for i in range(NUB)]
    dgs = [p.sbuf([128, 128], BF16, f"dg{i}") for i in range(8)]
    osb = p.sbuf([128, D], F32, "posb")
    identb = G["ident_bf"]
    UVB = G["UVB"]
    nslot = H8 * TK
    ntiles = T // 128
    idxus = [idxu, p.sbuf([128, H8 * TK], U32, "idxu_b")]
    gates = [gate, p.sbuf([128, H8, TK], F32, "gate_b")]
    posu = p.sbuf([128, H8, TK], U32, "posu")
    xg = p.sbuf([128, H8 * TK], F32, "xg")
    abu = p.sbuf([128, 2, H8 * TK], U32, "abu")
    abf = p.sbuf([128, 2, H8 * TK], F32, "abf")
    ijf = p.sbuf([128, 2, H8 * TK], F32, "ijf")
    iota16 = p.sbuf([128, TK], F32, "iota16")
    p.load(iota16, iota16[:, :], G["ext"], io["iota16"].ap[:, :])

    def producer(ti):
        t0 = (ti // TB) * TB * 128
        t1 = min(t0 + TB * 128, T)
        ntl = (t1 - t0) // 128
        w = t1 - t0
        tl = ti % TB
        if tl == 0:
            for tl2 in range(ntl):
                tj = ti + tl2
                r = cfg.seg_of_tile(tj)
                hs = hst[tj % 2]
                slot = tj % 4
                p.load(hs, hs[:, :], G["H"], G["H"].ap[tj * 128:(tj + 1) * 128, :], q="sp")
                p.tt(hm, hm[:, slot, :], hs, hs[:, :], scb[r], scb[r][:, :], ALU.mult)
                p.tt(hm, hm[:, slot, :], hm, hm[:, slot, :], shb[r], shb[r][:, :], ALU.add)
                yield
                for half in range(2):
                    pt = pT[half]
                    for kk in range(4):
                        kc = half * 4 + kk
                        p.transpose(pt, pt[:, kk, :], hm, hm[:, slot, kc * 128:(kc + 1) * 128], ident, ident[:, :])
                    p.copy(hmT, hmT[:, half * 4:(half + 1) * 4, tl2 * 128:(tl2 + 1) * 128], pt, pt[:, :, :],
                           eng="act")
                    yield
            for hsx in range(16):
                pq = pQ[hsx % 2]
                for kc in range(KC):
                    p.mm(pq, pq[:, 0:w], wq_bf, wq_bf[:, kc, hsx * NK:(hsx + 1) * NK], hmT, hmT[:, kc, 0:w],
                         start=(kc == 0), stop=(kc == KC - 1))
                p.copy(qT, qT[:, hsx, 0:w], pq, pq[:, 0:w], eng="act")
                yield
        idxu_, gate_ = idxus[ti % 2], gates[ti % 2]
        for q4 in range(4):
            ps = pS[q4]
            for k4 in range(4):
                hsx = q4 * 4 + k4
                p.mm(ps, ps[:, k4, :], qT, qT[:, hsx, tl * 128:(tl + 1) * 128], sk_bf, sk_bf[:, hsx, :])
            p.copy(s_sb, s_sb[:, q4 * 4:(q4 + 1) * 4, :], ps, ps[:, :, :], eng="act")
            yield
        for hsx in range(16):
            src_ = s_sb[:, hsx, :]
            p.op("dve", lambda e, hsx=hsx, src_=src_: e.max(out=ts16[:, hsx, 0:8], in_=src_), reads=[s_sb], writes=[ts16])
            p.op("dve", lambda e, hsx=hsx, src_=src_: e.max_index(ti16[:, hsx, 0:8], ts16[:, hsx, 0:8], src_),
                 reads=[s_sb, ts16], writes=[ti16])
            p.op("dve", lambda e, hsx=hsx, src_=src_: e.match_replace(out=s_wk[:, :], in_to_replace=ts16[:, hsx, 0:8],
                                                                      in_values=src_, imm_value=NEG),
                 reads=[s_sb, ts16], writes=[s_wk])
            p.op("dve", lambda e, hsx=hsx: e.max(out=ts16[:, hsx, 8:16], in_=s_wk[:, :]), reads=[s_wk], writes=[ts16])
            p.op("dve", lambda e, hsx=hsx: e.max_index(ti16[:, hsx, 8:16], ts16[:, hsx, 8:16], s_wk[:, :]),
                 reads=[s_wk, ts16], writes=[ti16])
            yield
        p.copy(ti16f, ti16f.ap[:, :, :].rearrange("p a b -> p (a b)"), ti16, ti16.ap[:, :, :].rearrange("p a b -> p (a b)"))
        ts4 = ts16.ap[:, :, :].rearrange("p (h s) k -> p h s k", s=2)
        ti4 = ti16f.ap[:, :, :].rearrange("p (h s) k -> p h s k", s=2)
        c4 = cand.ap[:, :, :].rearrange("p h (a b) -> p h a b", a=TK)
        e4 = eid.ap[:, :, :].rearrange("p h (a b) -> p h a b", a=TK)
        for h in range(H8):
            a_b = ts4[:, h, 0, :].unsqueeze(2).broadcast_to([128, TK, TK])
            b_b = ts4[:, h, 1, :].unsqueeze(1).broadcast_to([128, TK, TK])
            p.tt(cand, c4[:, h, :, :], ts16, a_b, ts16, b_b, ALU.add)
            yield
        for h in range(H8):
            src_ = cand[:, h, :]
            p.op("dve", lambda e, h=h, src_=src_: e.max(out=best[:, h, 0:8], in_=src_), reads=[cand], writes=[best])
            p.op("dve", lambda e, h=h, src_=src_: e.match_replace(out=cwk[:, :], in_to_replace=best[:, h, 0:8],
                                                                  in_values=src_, imm_value=NEG),
                 reads=[cand, best], writes=[cwk])
            p.op("dve", lambda e, h=h, src_=src_: e.max_index(posu[:, h, 0:8], best[:, h, 0:8], src_),
                 reads=[cand, best], writes=[posu])
            p.op("dve", lambda e, h=h: e.max(out=best[:, h, 8:16], in_=cwk[:, :]), reads=[cwk], writes=[best])
            p.op("dve", lambda e, h=h: e.max_index(posu[:, h, 8:16], best[:, h, 8:16], cwk[:, :]),
                 reads=[cwk, best], writes=[posu])
            yield
        mx_b = best.ap[:, :, 0:1].broadcast_to([128, H8, TK])
        p.tt(gate_, gate_[:, :, :], best, best[:, :, :], best, mx_b, ALU.subtract)
        p.act(gate_, gate_[:, :, :], gate_, gate_[:, :, :], AF.Exp)
        p.op("dve", lambda e: e.tensor_reduce(out=gsum[:, :], in_=gate_[:, :, :], axis=AX.X, op=ALU.add),
             reads=[gate_], writes=[gsum])
        p.op("dve", lambda e: e.reciprocal(gsum[:, :], gsum[:, :]), reads=[gsum], writes=[gsum])
        p.tt(gate_, gate_[:, :, :], gate_, gate_[:, :, :], gsum, gsum.ap[:, :].unsqueeze(2).broadcast_to([128, H8, TK]), ALU.mult)
        yield
        pflat = posu.ap[:, :, :].rearrange("p a b -> p (a b)")
        p.op("dve", lambda e: e.tensor_single_scalar(abu[:, 0, :], pflat, 4, op=ALU.logical_shift_right),
             reads=[posu], writes=[abu])
        p.op("dve", lambda e: e.tensor_single_scalar(abu[:, 1, :], pflat, 15, op=ALU.bitwise_and),
             reads=[posu], writes=[abu])
        p.copy(abf, abf.ap[:, :, :].rearrange("p a b -> p (a b)"), abu, abu.ap[:, :, :].rearrange("p a b -> p (a b)"))
        yield
        oh4 = eid.ap[:, :, :].rearrange("p h (k a) -> p h k a", k=TK)
        oh3 = eid.ap[:, :, :].rearrange("p h (k a) -> p (h k) a", k=TK)
        io16 = iota16.ap[:, :].unsqueeze(1).unsqueeze(1).broadcast_to([128, H8, TK, TK])
        for half in range(2):
            sel_ = abf.ap[:, half, :].rearrange("p (h k) -> p h k", h=H8).unsqueeze(3).broadcast_to([128, H8, TK, TK])
            tab_ = ti4[:, :, half, :].unsqueeze(2).broadcast_to([128, H8, TK, TK])
            p.tt(eid, oh4, abf, sel_, iota16, io16, ALU.is_equal)
            p.tt(eid, oh4, eid, oh4, ti16f, tab_, ALU.mult)
            yield
            p.op("dve", lambda e, half=half: e.tensor_reduce(out=ijf[:, half, :], in_=oh3, axis=AX.X, op=ALU.add),
                 reads=[eid], writes=[ijf])
            yield
        p.stt(idxf, idxf[:, :], ijf, ijf[:, 0, :], float(NK), ijf, ijf[:, 1, :], ALU.mult, ALU.add)
        p.ts(idxf, idxf[:, :], idxf, idxf[:, :], 0.0, float(NK * NK - 1), ALU.max, ALU.min)
        p.copy(idxu_, idxu_[:, :], idxf, idxf[:, :])
        yield

    def drain(g, n=None):
        if g is None:
            return None
        try:
            if n is None:
                while True:
                    next(g)
            for _ in range(n):
                next(g)
        except StopIteration:
            return None
        return g

    gi = 0
    drain(producer(0))
    for ti in range(ntiles):
        nxt = producer(ti + 1) if ti + 1 < ntiles else None
        idxu_, gate_ = idxus[ti % 2], gates[ti % 2]
        slot = ti % 4
        for hf in range(2):
            p.mm(pHM, pHM[:, hf * 512:(hf + 1) * 512], ident, ident[:, :], hm, hm[:, slot, hf * 512:(hf + 1) * 512])
        gflat = gate_.ap[:, :, :].rearrange("p a b -> p (a b)")
        for s0 in range(0, nslot, GRP):
            bufs = []
            for s in range(s0, s0 + GRP):
                u = ub[gi % NUB]
                gi += 1
                bufs.append(u)
                p.dma("pool", u, u[:, :], UVB, None, extra_reads=[idxu_],
                      fn=lambda e, u=u, s=s, idxu_=idxu_: e.indirect_dma_start(
                          out=u[:, :], out_offset=None, in_=UVB.ap,
                          in_offset=bass.IndirectOffsetOnAxis(ap=idxu_[:, s:s + 1], axis=0)))
                p.stt(junk, junk[:, :], u, u[:, 0:D], 1.0, pHM, pHM[:, :], ALU.mult, ALU.mult,
                      accum=aval[:, s:s + 1], accum_b=aval)
            sl = slice(s0, s0 + GRP)
            p.stt(tmpa, tmpa[:, sl], aval, aval[:, sl], 0.044715, aval, aval[:, sl], ALU.mult, ALU.mult)
            p.stt(tmpa, tmpa[:, sl], tmpa, tmpa[:, sl], 1.0, aval, aval[:, sl], ALU.add, ALU.mult)
            p.act(tmpa, tmpa[:, sl], tmpa, tmpa[:, sl], AF.Sigmoid, scale=float(2.0 * (2.0 / np.pi) ** 0.5))
            p.tt(xg, xg[:, sl], aval, aval[:, sl], gate_, gflat[:, sl], ALU.mult)
            p.tt(coef, coef[:, sl], tmpa, tmpa[:, sl], xg, xg[:, sl], ALU.mult)
            for k_, s in enumerate(range(s0, s0 + GRP)):
                u = bufs[k_]
                dg = dgs[s % 8]
                p.act(dg, dg[:, :], identb, identb[:, :], AF.Identity, scale=coef[:, s:s + 1], reads=[coef])
                for hf in range(2):
                    p.mm(pACC[hf], pACC[hf][:, :], dg, dg[:, :], u, u[:, D + hf * 512:D + (hf + 1) * 512],
                         start=(s == 0), stop=(s == nslot - 1))
            nxt = drain(nxt, 4)
        drain(nxt)
        p.copy(osb, osb[:, 0:512], pACC[0], pACC[0][:, :], eng="act")
        p.copy(osb, osb[:, 512:1024], pACC[1], pACC[1][:, :], eng="act")
        p.store(G["PEERO"], G["PEERO"].ap[ti * 128:(ti + 1) * 128, :], osb, osb[:, :], q="sp")
    p.close_scope()


def stage_ssd(p, cfg, G, io, l):
    Tb, CTX = cfg.tb, cfg.ctx
    j = l // 2
    YT = G["YT"]
    XSF = G["XSF"]
    YPRE = G["YPRE"]
    nt = Tb // 128
    segs = [(0, CTX), (CTX, Tb)]
    p.open_scope()
    identb, ident = G["ident_bf"], G["ident"]
    cw = p.sbuf([128, 8, 5], F32, "convw")
    p.load(cw, cw.ap[:, :, :].rearrange("p a b -> p (a b)"), G["ext"], io["ssd_conv_wT"].ap[j])
    cb = p.sbuf([128, 8], F32, "convb")
    p.load(cb, cb[:, :], G["ext"], io["ssd_conv_bT"].ap[j])
    dtb = p.sbuf([8, 2], F32, "dtb")
    p.load(dtb, dtb[:, :], G["ext"], io["ssd_dt_biasT"].ap[j])
    nA = p.sbuf([8, 2], F32, "nA")
    p.load(nA, nA[:, :], G["ext"], io["ssd_a_logT"].ap[j])
    p.act(nA, nA[:, :], nA, nA[:, :], AF.Exp)
    p.ts(nA, nA[:, :], nA, nA[:, :], -1.0, None, ALU.mult)
    dsk = p.sbuf([128, 4], F32, "dskip")
    p.load(dsk, dsk[:, :], G["ext"], io["ssd_dT"].ap[j])
    gnm = p.sbuf([128, 4], F32, "ssd_g")
    p.load(gnm, gnm[:, :], G["ext"], io["ssd_norm_gT"].ap[j])
    sel = p.sbuf([8, 8, 128], F32, "sel")
    p.load(sel, sel.ap[:, :, :].rearrange("p a b -> p (a b)"), G["ext"], io["selT"].ap[:, :])
    mbias = p.sbuf([128, 128], F32, "mbias")
    p.load(mbias, mbias[:, :], G["ext"], io["mbias"].ap[:, :])
    rm128 = p.sbuf([8, Tb], F32, "rm128")
    p.load(rm128, rm128[:, :], G["ext"], io["rmask128"].ap[:, :])
    ones256 = p.sbuf([128, 128], F32, "ones256")
    p.memset(ones256, ones256[:, :], 1.0 / 256.0)
    pTrX = p.psum([128, 1024], BF16, "pTrX")
    pTrB = p.psum([128, 1024], BF16, "pTrB")
    pTrD = p.psum([128, 512], F32, "pTrD")
    pCR = p.psum([128, 2, 256], F32, "pCR")
    pSc = p.psum([128, 512], F32, "pSc")
    pY = p.psum([128, 512], F32, "pY")
    pKV = p.psum([128, 512], F32, "pKV")
    pN = p.psum([128, 512], F32, "pN")
    XCb = p.sbuf([128, 8, Tb], BF16, "XCb")
    raw = [p.sbuf([128, Tb], F32, f"sraw{i}") for i in range(2)]
    cacc = p.sbuf([128, Tb], F32, "cacc")
    dtT = [p.sbuf([8, Tb], F32, f"dtT{d}") for d in range(2)]
    cumT = [p.sbuf([8, Tb], F32, f"cumT{d}") for d in range(2)]
    t8a = p.sbuf([8, Tb], F32, "t8a")
    t8b = p.sbuf([8, Tb], F32, "t8b")
    Xd = p.sbuf([128, 3, Tb], BF16, "Xd")
    yacc = p.sbuf([128, Tb], F32, "yacc")
    xs_tok = [p.sbuf([128, 128], BF16, f"xs_tok{i}") for i in range(2)]
    B_tok = [p.sbuf([128, 128], BF16, f"B_tok{i}") for i in range(2)]
    dc_tok = [p.sbuf([128, 16], F32, f"dc_tok{i}") for i in range(2)]
    crow = [p.sbuf([128, 2, 128], F32, f"crow{i}") for i in range(2)]
    sc_sb = [p.sbuf([128, 128], F32, f"sc_sb{i}") for i in range(2)]
    D1 = [p.sbuf([128, 128], F32, f"D1{i}") for i in range(2)]
    attT = [p.sbuf([128, 128], BF16, f"attT{i}") for i in range(2)]
    ec = [p.sbuf([128, 128], F32, f"ec{i}") for i in range(2)]
    Chat = [p.sbuf([128, 128], BF16, f"Chat{i}") for i in range(2)]
    w2 = p.sbuf([128, 2], F32, "w2")
    el2 = p.sbuf([128, 2], F32, "el2")
    xhat = [p.sbuf([128, 2, 64], BF16, f"xhat{i}") for i in range(2)]
    S2 = p.sbuf([128, 2, 64], F32, "S2")
    Sbf = [p.sbuf([128, 2, 64], BF16, f"S2bf{i}") for i in range(2)]
    obf = p.sbuf([128, Tb], BF16, "ssd_obf")
    for b in range(cfg.nb):
        c0 = b * Tb
        for ch in range(8):
            r0 = OD["xs"] + ch * 128
            rw = raw[ch % 2]
            p.load(rw, rw[:, :], YT, YT.ap[r0:r0 + 128, c0:c0 + Tb], q="sp" if ch % 2 else "act")
            for (s0, s1) in segs:
                p.ts(cacc, cacc[:, s0:s1], rw, rw[:, s0:s1], cw[:, ch, 2:3], None, ALU.mult, reads=[cw])
                for k in (0, 1, 3, 4):
                    sh = k - 2
                    o0, o1 = max(s0, s0 - sh), min(s1, s1 - sh)
                    p.stt(cacc, cacc[:, o0:o1], rw, rw[:, o0 + sh:o1 + sh], cw[:, ch, k:k + 1], cacc, cacc[:, o0:o1],
                          ALU.mult, ALU.add, reads=[cw])
            if ch < 4:
                p.act(cacc, cacc[:, :], cacc, cacc[:, :], AF.Silu, bias=cb[:, ch:ch + 1], reads=[cb])
                p.copy(XCb, XCb[:, ch, :], cacc, cacc[:, :], eng="pool")
                p.store(XSF, XSF.ap[ch * 128:(ch + 1) * 128, c0:c0 + Tb], cacc, cacc[:, :], q="sp")
            else:
                p.act(XCb, XCb[:, ch, :], cacc, cacc[:, :], AF.Silu, bias=cb[:, ch:ch + 1], reads=[cb])
        for d in range(2):
            r0 = OD["dtf"] if d == 0 else OD["dtb"]
            p.load(t8a, t8a[:, :], YT, YT.ap[r0:r0 + 8, c0:c0 + Tb], q="sp")
            for (s0, s1) in segs:
                p.ts(t8b, t8b[:, s0:s1], t8a, nat_ap(t8a.ap, cfg, s0, s1, d), dtb[:, d:d + 1], None, ALU.add, reads=[dtb])
            p.act(t8a, t8a[:, :], t8b, t8b[:, :], AF.Abs)
            p.act(t8a, t8a[:, :], t8a, t8a[:, :], AF.Exp, scale=-1.0)
            p.act(t8a, t8a[:, :], t8a, t8a[:, :], AF.Ln, bias=1.0)
            p.ts(t8b, t8b[:, :], t8b, t8b[:, :], 0.0, None, ALU.max)
            p.tt(dtT[d], dtT[d][:, :], t8a, t8a[:, :], t8b, t8b[:, :], ALU.add)
            p.ts(t8a, t8a[:, :], dtT[d], dtT[d][:, :], nA[:, d:d + 1], None, ALU.mult, reads=[nA])
            p.op("dve", lambda e, d=d: e.tensor_tensor_scan(cumT[d][:, :], rm128[:, :], t8a[:, :], 0.0, ALU.mult, ALU.add),
                 reads=[rm128, t8a], writes=[cumT[d]])
        for cc in range(4):
            g = cc // 2
            for d in range(2):
                for (s0, s1) in segs:
                    for k_, src_ch in enumerate((cc, 4 + g, 6 + g)):
                        p.copy(Xd, Xd[:, k_, s0:s1], XCb, nat_ap(XCb.ap[:, src_ch, :], cfg, s0, s1, d),
                               eng=("pool", "act", "dve")[k_])
                p.memset(S2, S2.ap[:, :, :].rearrange("p a b -> p (a b)"), 0.0)
                p.memset(Sbf[0], Sbf[0].ap[:, :, :].rearrange("p a b -> p (a b)"), 0.0)
                cur = 0
                for ti in range(nt):
                    cs = slice(ti * 128, (ti + 1) * 128)
                    i2 = ti % 2
                    p.transpose(pTrX, pTrX[:, 0:128], Xd, Xd[:, 0, cs], identb, identb[:, :])
                    p.transpose(pTrB, pTrB[:, 0:128], Xd, Xd[:, 1, cs], identb, identb[:, :])
                    p.copy(xs_tok[i2], xs_tok[i2][:, :], pTrX, pTrX[:, 0:128], eng="act")
                    p.copy(B_tok[i2], B_tok[i2][:, :], pTrB, pTrB[:, 0:128], eng="dve")
                    p.transpose(pTrD, pTrD[:, 0:8], dtT[d], dtT[d][:, cs], ident, ident[0:8, 0:8])
                    p.transpose(pTrD, pTrD[:, 8:16], cumT[d], cumT[d][:, cs], ident, ident[0:8, 0:8])
                    dc = dc_tok[i2]
                    p.copy(dc, dc[:, :], pTrD, pTrD[:, 0:16], eng="dve")
                    for hh in range(2):
                        p.mm(pCR, pCR[:, hh, 0:128], sel, sel[:, 2 * cc + hh, :], cumT[d], cumT[d][:, cs])
                    cr = crow[i2]
                    p.copy(cr, cr[:, :, :], pCR, pCR[:, :, 0:128], eng="act")
                    p.mm(pSc, pSc[:, 0:128], Xd, Xd[:, 1, cs], Xd, Xd[:, 2, cs])
                    sc = sc_sb[i2]
                    p.copy(sc, sc[:, :], pSc, pSc[:, 0:128], eng="dve")
                    for hh in range(2):
                        h = 2 * cc + hh
                        p.stt(D1[hh], D1[hh][:, :], cr, cr[:, hh, :], dc[:, 8 + h:9 + h], mbias, mbias[:, :],
                              ALU.subtract, ALU.add, reads=[dc])
                        p.act(D1[hh], D1[hh][:, :], D1[hh], D1[hh][:, :], AF.Exp)
                        p.stt(attT[hh], attT[hh][:, :], D1[hh], D1[hh][:, :], dc[:, h:h + 1], sc, sc[:, :],
                              ALU.mult, ALU.mult, reads=[dc])
                        p.act(ec[hh], ec[hh][:, :], cr, cr[:, hh, :], AF.Exp)
                        p.tt(Chat[hh], Chat[hh][:, :], Xd, Xd[:, 2, cs], ec[hh], ec[hh][:, :], ALU.mult, eng="pool")
                        p.mm(pY, pY[hh * 64:(hh + 1) * 64, 0:128], xs_tok[i2], xs_tok[i2][:, hh * 64:(hh + 1) * 64],
                             attT[hh], attT[hh][:, :], start=True, stop=False)
                        p.mm(pY, pY[hh * 64:(hh + 1) * 64, 0:128], Sbf[cur], Sbf[cur][:, hh, :],
                             Chat[hh], Chat[hh][:, :], start=False, stop=True)
                    lastb = cr[:, :, 127]
                    p.tt(w2, w2[:, :], cr, lastb, dc, dc[:, 8 + 2 * cc:10 + 2 * cc], ALU.subtract)
                    p.act(w2, w2[:, :], w2, w2[:, :], AF.Exp)
                    p.tt(w2, w2[:, :], w2, w2[:, :], dc, dc[:, 2 * cc:2 * cc + 2], ALU.mult)
                    p.act(el2, el2[:, :], cr, lastb, AF.Exp)
                    xh = xhat[i2]
                    p.tt(xh, xh[:, :, :], xs_tok[i2], xs_tok[i2].ap[:, :].rearrange("p (a b) -> p a b", a=2),
                         w2, w2.ap[:, :].unsqueeze(2).broadcast_to([128, 2, 64]), ALU.mult)
                    p.mm(pKV, pKV[:, 0:128], B_tok[i2], B_tok[i2][:, :], xh, xh.ap[:, :, :].rearrange("p a b -> p (a b)"))
                    p.tt(S2, S2[:, :, :], S2, S2[:, :, :], el2, el2.ap[:, :].unsqueeze(2).broadcast_to([128, 2, 64]), ALU.mult)
                    S2f = S2.ap[:, :, :].rearrange("p a b -> p (a b)")
                    p.tt(S2, S2f, S2, S2f, pKV, pKV[:, 0:128], ALU.add)
                    nxt = 1 - cur
                    p.copy(Sbf[nxt], Sbf[nxt].ap[:, :, :].rearrange("p a b -> p (a b)"), S2, S2f, eng="act")
                    cur = nxt
                    sl, rev = nat_slice(cfg, ti * 128, (ti + 1) * 128, d)
                    if d == 0:
                        p.copy(yacc, yacc[:, sl], pY, pY[:, 0:128], eng="act")
                    else:
                        ya = yacc.ap[:, sl][:, ::-1]
                        p.tt(yacc, ya, yacc, ya, pY, pY[:, 0:128], ALU.add)
            p.load(raw[0], raw[0][:, :], XSF, XSF.ap[cc * 128:(cc + 1) * 128, c0:c0 + Tb], q="sp")
            p.load(raw[1], raw[1][:, :], YT, YT.ap[OD["z"] + cc * 128:OD["z"] + (cc + 1) * 128, c0:c0 + Tb], q="act")
            p.stt(yacc, yacc[:, :], raw[0], raw[0][:, :], dsk[:, cc:cc + 1], yacc, yacc[:, :], ALU.mult, ALU.add, reads=[dsk])
            p.act(raw[1], raw[1][:, :], raw[1], raw[1][:, :], AF.Silu)
            p.tt(yacc, yacc[:, :], yacc, yacc[:, :], raw[1], raw[1][:, :], ALU.mult)
            p.store(YPRE, YPRE.ap[cc * 128:(cc + 1) * 128, c0:c0 + Tb], yacc, yacc[:, :], q="sp")
        for g in range(2):
            for k_ in range(2):
                p.load(raw[k_], raw[k_][:, :], YPRE, YPRE.ap[(2 * g + k_) * 128:(2 * g + k_ + 1) * 128, c0:c0 + Tb],
                       q="sp" if k_ else "act")
            p.act(cacc, cacc[:, :], raw[0], raw[0][:, :], AF.Square)
            p.act(yacc, yacc[:, :], raw[1], raw[1][:, :], AF.Square)
            for (a0, a1) in blocks(Tb, 512):
                p.mm(pN, pN[:, 0:a1 - a0], ones256, ones256[:, :], cacc, cacc[:, a0:a1], start=True, stop=False)
                p.mm(pN, pN[:, 0:a1 - a0], ones256, ones256[:, :], yacc, yacc[:, a0:a1], start=False, stop=True)
                p.act(cacc, cacc[:, a0:a1], pN, pN[:, 0:a1 - a0], AF.Ln, bias=float(NORM_EPS))
            p.act(cacc, cacc[:, :], cacc, cacc[:, :], AF.Exp, scale=-0.5)
            for k_ in range(2):
                p.stt(obf, obf[:, :], raw[k_], raw[k_][:, :], gnm[:, 2 * g + k_:2 * g + k_ + 1], cacc, cacc[:, :],
                      ALU.mult, ALU.mult, reads=[gnm])
                p.store(G["MIXT"], G["MIXT"].ap[512 + (2 * g + k_) * 128:512 + (2 * g + k_ + 1) * 128, c0:c0 + Tb],
                        obf, obf[:, :], q="pool")
    p.close_scope()


def kernel(**inputs):
    x = np.asarray(inputs["x"])
    B, SEQ, _ = x.shape
    CTX = np.asarray(inputs["ctx"]).shape[1]
    depth = np.asarray(inputs["mod_w"]).shape[0]
    ncores = NCORES if B % NCORES == 0 else 1
    cfg = Cfg(B // ncores, SEQ, CTX, depth)
    p = build_program(cfg)
    nc = p.finish()
    maps = prep_inputs(inputs, cfg, ncores)
    res = run_bass_kernel_spmd(nc, maps, core_ids=list(range(ncores))).results
    out = np.empty((B, SEQ, D), np.float32)
    for ci in range(ncores):
        o = res[ci]["out"].reshape(cfg.nb, cfg.tb, D)
        for bl in range(cfg.nb):
            out[ci * cfg.nb + bl] = o[bl, CTX:, :]
    return out
```
